# Optimizing a Trainium2 kernel written in Bass

```python
import math
import jax, jax.numpy as jnp
from jax import lax
import numpy as np

D_MODEL = 1024
BATCH = 2
SEQ = 8192
DEPTH = 1

GLA_HEADS = 4
GLA_DK = 128
GLA_DV = 256
GLA_RANK = 16
GLA_TAU = 16.0
GLA_CHUNK = 64
GLA_K_W = GLA_HEADS * GLA_DK
GLA_V_W = GLA_HEADS * GLA_DV
DIFF_HEADS = 8
DIFF_DH = 64
DIFF_QK_W = DIFF_HEADS * 2 * DIFF_DH
DIFF_V_W = DIFF_HEADS * 2 * DIFF_DH
ROT_DIM = DIFF_DH // 4
ROPE_THETA = 500000.0
Q_BLOCK = 128
N_EXPERTS = 32
TOP_K = 4
D_FF = 1024
SWIGLU_LIMIT = 7.0
SWIGLU_ALPHA = 1.702
MOE_BLOCK = 128
DN_ALPHA = (2 * DEPTH) ** 0.25
DN_BETA = (8 * DEPTH) ** -0.25
EPS = 1e-5

IN_SPLITS = (GLA_K_W, GLA_K_W, GLA_V_W, GLA_V_W, GLA_RANK, GLA_RANK,
             DIFF_QK_W, DIFF_QK_W, DIFF_V_W, D_MODEL, D_MODEL)
VALUE_BLOCKS = (2, 8)
N_IN = sum(IN_SPLITS)

kernel_name = "hybrid_gla_diffattn_moe_deepnorm"


def _layer_norm(x, g, b):
    xf = x.astype(jnp.float32)
    mu = jnp.mean(xf, -1, keepdims=True)
    var = jnp.mean(jnp.square(xf - mu), -1, keepdims=True)
    return ((xf - mu) * lax.rsqrt(var + EPS) * g + b).astype(x.dtype)


def _rms_norm(x, g):
    xf = x.astype(jnp.float32)
    return (xf * lax.rsqrt(jnp.mean(jnp.square(xf), -1, keepdims=True) + EPS) * g).astype(x.dtype)


def _apply_partial_rope(t, cos, sin):
    half = ROT_DIM // 2
    x1 = t[..., :half].astype(jnp.float32)
    x2 = t[..., half:ROT_DIM].astype(jnp.float32)
    rot = jnp.concatenate([x1 * cos - x2 * sin, x2 * cos + x1 * sin], -1).astype(t.dtype)
    return jnp.concatenate([rot, t[..., ROT_DIM:]], -1)


def _gla_chunked(q, k, v, logf):
    B, S, H, DK = q.shape
    DV = v.shape[-1]
    N, C = S // GLA_CHUNK, GLA_CHUNK

    def blk(t):
        return t.astype(jnp.float32).reshape(B, N, C, H, t.shape[-1]).transpose(0, 3, 1, 2, 4)

    q, k, v, logf = blk(q), blk(k), blk(v), blk(logf)
    b = jnp.cumsum(logf, axis=3)
    b_last = b[:, :, :, -1:, :]
    q_in = q * jnp.exp(b)
    k_in = k * jnp.exp(-b)
    k_st = k * jnp.exp(b_last - b)
    mask = jnp.tril(jnp.ones((C, C), dtype=bool))
    att = jnp.einsum('bhncd,bhnmd->bhncm', q_in, k_in)
    att = jnp.where(mask, att, 0.0)
    o_intra = jnp.einsum('bhncm,bhnme->bhnce', att, v)
    u = jnp.einsum('bhncd,bhnce->bhnde', k_st, v)
    decay = jnp.exp(b_last[:, :, :, 0, :])

    def step(state, inp):
        dec, un = inp
        return dec[..., None] * state + un, state

    _, s_prev = lax.scan(step, jnp.zeros((B, H, DK, DV), jnp.float32),
                         (decay.transpose(2, 0, 1, 3), u.transpose(2, 0, 1, 3, 4)))
    o_inter = jnp.einsum('bhncd,nbhde->bhnce', q_in, s_prev)
    o = o_intra + o_inter
    return o.transpose(0, 2, 3, 1, 4).reshape(B, S, H, DV)


def _diff_attention(q, k, v, lam):
    B, S, H, _, DH = q.shape
    nq = S // Q_BLOCK
    scale = DH ** -0.5
    qb = q.reshape(B, nq, Q_BLOCK, H, 2, DH).transpose(1, 0, 2, 3, 4, 5)

    def one_block(qi):
        s = jnp.einsum('bqhpd,bkhpd->bhpqk', qi, k).astype(jnp.float32) * scale
        p = jax.nn.softmax(s, axis=-1)
        pw = p[:, :, 0] - lam * p[:, :, 1]
        return jnp.einsum('bhqk,bkhe->bqhe', pw.astype(v.dtype), v)

    o = lax.map(one_block, qb)
    return o.transpose(1, 0, 2, 3, 4).reshape(B, S, H, 2 * DH)


def _token_mixer(h, w_in, wf_fwd, bf_fwd, wf_bwd, bf_bwd, gla_norm_g,
                 lq1, lk1, lq2, lk2, diff_norm_g, w_br_gla, w_br_diff, w_out,
                 cos, sin, lam_init):
    B, S, D = h.shape
    proj = h @ w_in
    cuts = np.cumsum(IN_SPLITS)[:-1].tolist()
    (qg, kg, vg, rg, lr_f, lr_b, qd, kd, vd, gate_g, gate_d) = jnp.split(proj, cuts, axis=-1)

    qg = qg.reshape(B, S, GLA_HEADS, GLA_DK) * (GLA_DK ** -0.5)
    kg = kg.reshape(B, S, GLA_HEADS, GLA_DK)
    vg = vg.reshape(B, S, GLA_HEADS, GLA_DV)
    logf_f = (jax.nn.log_sigmoid((lr_f @ wf_fwd + bf_fwd).astype(jnp.float32)) / GLA_TAU
              ).reshape(B, S, GLA_HEADS, GLA_DK)
    logf_b = (jax.nn.log_sigmoid((lr_b @ wf_bwd + bf_bwd).astype(jnp.float32)) / GLA_TAU
              ).reshape(B, S, GLA_HEADS, GLA_DK)
    o_f = _gla_chunked(qg, kg, vg, logf_f)
    flip = lambda t: jnp.flip(t, axis=1)
    o_b = flip(_gla_chunked(flip(qg), flip(kg), flip(vg), flip(logf_b)))
    o_gla = _rms_norm(o_f + o_b, gla_norm_g).astype(h.dtype).reshape(B, S, GLA_V_W)
    o_gla = o_gla * jax.nn.silu(rg)

    qd = _apply_partial_rope(qd.reshape(B, S, DIFF_HEADS, 2, DIFF_DH), cos, sin)
    kd = _apply_partial_rope(kd.reshape(B, S, DIFF_HEADS, 2, DIFF_DH), cos, sin)
    vd = vd.reshape(B, S, DIFF_HEADS, 2 * DIFF_DH)
    lam = (jnp.exp(jnp.sum(lq1.astype(jnp.float32) * lk1.astype(jnp.float32)))
           - jnp.exp(jnp.sum(lq2.astype(jnp.float32) * lk2.astype(jnp.float32))) + lam_init)
    o_diff = _diff_attention(qd, kd, vd, lam)
    o_diff = (_rms_norm(o_diff, diff_norm_g) * (1.0 - lam_init)).reshape(B, S, DIFF_V_W)

    mixed = (jax.nn.sigmoid(gate_g) * (o_gla @ w_br_gla)
             + jax.nn.sigmoid(gate_d) * (o_diff @ w_br_diff))
    return mixed @ w_out


def _moe(h, router_w, router_b, w_gate, b_gate, w_up, b_up, w_down, b_down):
    B, S, D = h.shape
    T = B * S
    A = T * TOP_K
    xf = h.reshape(T, D)
    logits = (xf @ router_w + router_b).astype(jnp.float32)
    top_v, top_i = lax.top_k(logits, TOP_K)
    gates = jax.nn.softmax(top_v, axis=-1)

    e_flat = top_i.reshape(A).astype(jnp.int32)
    tok = jnp.arange(A, dtype=jnp.int32) // TOP_K
    w_flat = gates.reshape(A)
    order = jnp.argsort(e_flat)
    e_sorted = e_flat[order]
    counts = jnp.bincount(e_flat, length=N_EXPERTS)
    padded = ((counts + MOE_BLOCK - 1) // MOE_BLOCK) * MOE_BLOCK
    pend = jnp.cumsum(padded)
    pstart = pend - padded
    ustart = jnp.cumsum(counts) - counts
    dest = pstart[e_sorted] + jnp.arange(A, dtype=jnp.int32) - ustart[e_sorted]
    P = A + N_EXPERTS * MOE_BLOCK
    NB = P // MOE_BLOCK
    row_tok = jnp.full((P,), T, jnp.int32).at[dest].set(tok[order])
    row_w = jnp.zeros((P,), xf.dtype).at[dest].set(w_flat[order].astype(xf.dtype))
    blk_e = jnp.minimum(jnp.searchsorted(pend, jnp.arange(NB, dtype=jnp.int32) * MOE_BLOCK,
                                         side='right'), N_EXPERTS - 1).astype(jnp.int32)
    x_pad = jnp.concatenate([xf, jnp.zeros((1, D), xf.dtype)], axis=0)
    xb = x_pad[row_tok].reshape(NB, MOE_BLOCK, D)

    def expert_block(args):
        xe, e = args
        g = jnp.minimum(xe @ w_gate[e] + b_gate[e], SWIGLU_LIMIT)
        u = jnp.clip(xe @ w_up[e] + b_up[e], -SWIGLU_LIMIT, SWIGLU_LIMIT)
        a = g * jax.nn.sigmoid(SWIGLU_ALPHA * g) * (u + 1.0)
        return a @ w_down[e] + b_down[e]

    yb = lax.map(expert_block, (xb, blk_e)).reshape(P, D)
    y = jnp.zeros((T + 1, D), yb.dtype).at[row_tok].add(yb * row_w[:, None])[:T]
    return y.reshape(B, S, D)


def setup_inputs(seed: int = 0) -> dict:
    key = jax.random.key(seed)
    ks = iter(jax.random.split(key, 32))
    L = DEPTH

    def nrm(shape, scale):
        return jax.random.normal(next(ks), shape, jnp.float32) * scale

    col_scale = np.concatenate([np.full((n,), DN_BETA if i in VALUE_BLOCKS else 1.0, np.float32)
                                for i, n in enumerate(IN_SPLITS)])
    return {
        "x": nrm((BATCH, SEQ, D_MODEL), 1.0),
        "ln0_g": 1.0 + nrm((D_MODEL,), 0.02),
        "ln0_b": nrm((D_MODEL,), 0.02),
        "w_in": nrm((L, D_MODEL, N_IN), D_MODEL ** -0.5) * jnp.asarray(col_scale),
        "gla_wf_fwd": nrm((L, GLA_RANK, GLA_K_W), GLA_RANK ** -0.5),
        "gla_bf_fwd": nrm((L, GLA_K_W), 0.1),
        "gla_wf_bwd": nrm((L, GLA_RANK, GLA_K_W), GLA_RANK ** -0.5),
        "gla_bf_bwd": nrm((L, GLA_K_W), 0.1),
        "gla_norm_g": 1.0 + nrm((L, GLA_DV), 0.02),
        "diff_lq1": nrm((L, DIFF_DH), 0.1),
        "diff_lk1": nrm((L, DIFF_DH), 0.1),
        "diff_lq2": nrm((L, DIFF_DH), 0.1),
        "diff_lk2": nrm((L, DIFF_DH), 0.1),
        "diff_norm_g": 1.0 + nrm((L, 2 * DIFF_DH), 0.02),
        "w_br_gla": nrm((L, GLA_V_W, D_MODEL), GLA_V_W ** -0.5 * DN_BETA),
        "w_br_diff": nrm((L, DIFF_V_W, D_MODEL), DIFF_V_W ** -0.5 * DN_BETA),
        "w_out": nrm((L, D_MODEL, D_MODEL), D_MODEL ** -0.5 * DN_BETA),
        "ln1_g": 1.0 + nrm((L, D_MODEL), 0.02),
        "ln1_b": nrm((L, D_MODEL), 0.02),
        "router_w": nrm((L, D_MODEL, N_EXPERTS), D_MODEL ** -0.5),
        "router_b": nrm((L, N_EXPERTS), 0.01),
        "exp_w_gate": nrm((L, N_EXPERTS, D_MODEL, D_FF), D_MODEL ** -0.5),
        "exp_b_gate": nrm((L, N_EXPERTS, D_FF), 0.02),
        "exp_w_up": nrm((L, N_EXPERTS, D_MODEL, D_FF), D_MODEL ** -0.5),
        "exp_b_up": nrm((L, N_EXPERTS, D_FF), 0.02),
        "exp_w_down": nrm((L, N_EXPERTS, D_FF, D_MODEL), D_FF ** -0.5 * DN_BETA),
        "exp_b_down": nrm((L, N_EXPERTS, D_MODEL), 0.02),
        "ln2_g": 1.0 + nrm((L, D_MODEL), 0.02),
        "ln2_b": nrm((L, D_MODEL), 0.02),
    }


def reference(x, ln0_g, ln0_b, w_in, gla_wf_fwd, gla_bf_fwd, gla_wf_bwd, gla_bf_bwd,
              gla_norm_g, diff_lq1, diff_lk1, diff_lq2, diff_lk2, diff_norm_g,
              w_br_gla, w_br_diff, w_out, ln1_g, ln1_b, router_w, router_b,
              exp_w_gate, exp_b_gate, exp_w_up, exp_b_up, exp_w_down, exp_b_down,
              ln2_g, ln2_b):
    S = x.shape[1]
    pos = jnp.arange(S, dtype=jnp.float32)
    inv_freq = jnp.power(ROPE_THETA, -jnp.arange(0, ROT_DIM, 2, dtype=jnp.float32) / ROT_DIM)
    ang = pos[:, None] * inv_freq[None, :]
    cos = jnp.cos(ang)[None, :, None, None, :]
    sin = jnp.sin(ang)[None, :, None, None, :]

    h = _layer_norm(x, ln0_g, ln0_b)
    for l in range(DEPTH):
        lam_init = 0.8 - 0.6 * math.exp(-0.3 * l)
        mix = _token_mixer(h, w_in[l], gla_wf_fwd[l], gla_bf_fwd[l], gla_wf_bwd[l], gla_bf_bwd[l],
                           gla_norm_g[l], diff_lq1[l], diff_lk1[l], diff_lq2[l], diff_lk2[l],
                           diff_norm_g[l], w_br_gla[l], w_br_diff[l], w_out[l],
                           cos, sin, lam_init)
        h = _layer_norm(DN_ALPHA * h + mix, ln1_g[l], ln1_b[l])
        ffn = _moe(h, router_w[l], router_b[l], exp_w_gate[l], exp_b_gate[l],
                   exp_w_up[l], exp_b_up[l], exp_w_down[l], exp_b_down[l])
        h = _layer_norm(DN_ALPHA * h + ffn, ln2_g[l], ln2_b[l])
    return h
```

```python
import numpy as np
from contextlib import ExitStack
import concourse.bass as bass
import concourse.mybir as mybir
from concourse.bass_utils import run_bass_kernel_spmd

F32 = mybir.dt.float32
BF16 = mybir.dt.bfloat16
I32 = mybir.dt.int32
AF = mybir.ActivationFunctionType
ALU = mybir.AluOpType

D = 1024
NOWN = 2048
NOTH = 6144
NALL = 8192
CAP = 384
NE = 32
EPS = 1e-5
DN_ALPHA = 2.0 ** 0.25
LAM_INIT = 0.2
SAME_ENG_SYNC = True
P1_TILES = list(range(64))
P1_LEVEL = 9
OWN_N = 16
P4_H = 8
P4_QB = 4
P4_KT = 64
P5_TB = 4
P5_TT = 4
P7_E = 32
P8_T = 16
STORE_Q = 'pool'
NO_ROPE = False
DEBUG = False

C_QG, C_KG, C_VG, C_RG, C_LRF, C_LRB, C_QD, C_KD, C_VD, C_GG, C_GD = (
    0, 512, 1024, 2048, 3072, 3088, 3104, 4128, 5152, 6176, 7200)

ENG_ATTR = {"pe": "tensor", "act": "scalar", "dve": "vector", "pool": "gpsimd", "sp": "sync"}
NRING = 12


class Op:
    __slots__ = ("eng", "fn", "deps", "signal", "sem", "val", "dma", "real")


class Sched:
    def __init__(self, nc, es):
        self.nc = nc
        self.engs = ["pe", "act", "dve", "pool", "sp"]
        self.csem = {e: es.enter_context(nc.semaphore("c_" + e)) for e in ["pe", "act", "dve", "pool"]}
        self.ccnt = {e: 0 for e in self.csem}
        self.ring = {q: [es.enter_context(nc.semaphore("d_%s%d" % (q, i))) for i in range(NRING)]
                     for q in ["sp", "pool"]}
        self.rcnt = {q: 0 for q in self.ring}
        self.rlast = {q: [None] * NRING for q in self.ring}
        self.ops = {e: [] for e in self.engs}
        self.lw = {}
        self.rd = {}
        self.seen = {e: {} for e in self.engs}
        self.nops = 0

    def add(self, eng, fn, r=(), w=(), dma=False):
        op = Op()
        op.eng = eng; op.fn = fn; op.dma = dma; op.signal = False; op.deps = []
        op.sem = None; op.val = None; op.real = True
        deps = []
        for k in r:
            o = self.lw.get(k)
            if o is not None:
                deps.append(o)
        for k in w:
            o = self.lw.get(k)
            if o is not None:
                deps.append(o)
            deps.extend(self.rd.get(k, ()))
        if dma:
            q = eng
            i = self.rcnt[q]
            slot = i % NRING
            prev = self.rlast[q][slot]
            if prev is not None:
                deps.append(prev)
            op.sem = self.ring[q][slot]
            op.val = 16 * (i // NRING + 1)
            self.rcnt[q] = i + 1
            self.rlast[q][slot] = op
        sd = set()
        for d in deps:
            if id(d) in sd:
                continue
            sd.add(id(d))
            if (not d.dma) and (not dma) and d.eng == eng:
                if eng == "pe" or not SAME_ENG_SYNC:
                    continue
            if not d.dma:
                d.signal = True
            op.deps.append(d)
        for k in r:
            self.rd.setdefault(k, []).append(op)
        for k in w:
            self.lw[k] = op
            self.rd[k] = []
        self.ops[eng].append(op)
        self.nops += 1
        return op

    def waitall(self, eng, keys):
        op = Op()
        op.eng = eng; op.fn = (lambda e: None); op.dma = False; op.signal = False; op.sem = None; op.val = None
        op.deps = []
        op.real = False
        for k in keys:
            o = self.lw.get(k)
            if o is not None:
                if not o.dma:
                    o.signal = True
                op.deps.append(o)
        self.ops[eng].append(op)

    def barrier(self):
        lasts = []
        for eng in self.engs:
            real = [o for o in self.ops[eng] if not o.dma and o.deps is not None and getattr(o, "real", True)]
            if real:
                o = real[-1]
                o.signal = True
                lasts.append(o)
        dmas = []
        for q in self.ring:
            for o in self.rlast[q]:
                if o is not None:
                    dmas.append(o)
        for eng in self.engs:
            op = Op()
            op.eng = eng; op.fn = (lambda e: None); op.dma = False; op.signal = False; op.sem = None; op.val = None
            op.deps = [o for o in lasts if o.eng != eng] + dmas
            self.ops[eng].append(op)

    def flush(self):
        nc = self.nc
        self.barrier()
        for o in self.lw.values():
            if not o.dma and o.val is None:
                o.signal = True
        for lst in self.rd.values():
            for o in lst:
                if not o.dma and o.val is None:
                    o.signal = True
        for eng in self.engs:
            for op in self.ops[eng]:
                if not op.dma and op.signal:
                    self.ccnt[eng] += 1
                    op.val = self.ccnt[eng]
                    op.sem = self.csem[eng]
                elif not op.dma:
                    op.val = -1
        with nc.Block() as blk:
            for eng in self.engs:
                ops = self.ops[eng]
                if not ops:
                    continue

                def body(e, ops=ops, eng=eng):
                    seen = self.seen[eng]
                    for op in ops:
                        for d in op.deps:
                            assert d.val is not None and d.val > 0, "dep not signalled"
                            key = id(d.sem)
                            if seen.get(key, 0) < d.val:
                                e.wait_ge(d.sem, d.val)
                                seen[key] = d.val
                        ins = op.fn(e)
                        if ins is None:
                            continue
                        if op.dma:
                            ins.then_inc(op.sem, 16)
                        elif op.signal:
                            ins.then_inc(op.sem, 1)

                getattr(blk, ENG_ATTR[eng])(body)
        self.ops = {e: [] for e in self.engs}


def build(stop=99, debug=False):
    global DEBUG
    DEBUG = debug
    nc = bass.Bass("TRN2", target_bir_lowering=False)

    def din(name, shape, dt=F32):
        return nc.dram_tensor(name, list(shape), dt, kind="ExternalInput").ap()

    def dscr(name, shape, dt):
        kind = "ExternalOutput" if DEBUG else "Internal"
        return nc.dram_tensor(name, list(shape), dt, kind=kind).ap()

    xa = din("xa", [NALL, D])
    csa = din("csa", [NALL, 130])
    cst = din("cst", [128, 8, 128])
    cst2 = din("cst2", [128, 32])
    ln0_g = din("ln0_g", [D]); ln0_b = din("ln0_b", [D])
    w_in = din("w_in", [D, 8224])
    wf_f = din("gla_wf_fwd", [16, 512]); bf_f = din("gla_bf_fwd", [512])
    wf_b = din("gla_wf_bwd", [16, 512]); bf_b = din("gla_bf_bwd", [512])
    gla_g = din("gla_norm_g", [256])
    lq1 = din("diff_lq1", [64]); lk1 = din("diff_lk1", [64])
    lq2 = din("diff_lq2", [64]); lk2 = din("diff_lk2", [64])
    diff_g = din("diff_norm_g", [128])
    w_bra = din("w_br_gla", [D, D]); w_brb = din("w_br_diff", [D, D]); w_out = din("w_out", [D, D])
    ln1_g = din("ln1_g", [D]); ln1_b = din("ln1_b", [D])
    router_w = din("router_w", [D, NE]); router_b = din("router_b", [NE])
    ew_g = din("exp_w_gate", [NE, D, D]); eb_g = din("exp_b_gate", [NE, D])
    ew_u = din("exp_w_up", [NE, D, D]); eb_u = din("exp_b_up", [NE, D])
    ew_d = din("exp_w_down", [NE, D, D]); eb_d = din("exp_b_down", [NE, D])
    ln2_g = din("ln2_g", [D]); ln2_b = din("ln2_b", [D])
    out = nc.dram_tensor("out", [NOWN, D], F32, kind="ExternalOutput").ap()

    KT_d = dscr("KT_d", [8, 128, NALL], BF16)
    V_d = dscr("V_d", [NALL, D], BF16)
    QT_d = dscr("QT_d", [8, 128, NOWN], BF16)
    H_d = dscr("H_d", [NOWN, D], F32)
    HT_d = dscr("HT_d", [D, NOWN], BF16)
    OF_d = dscr("OF_d", [NOWN, D], F32)
    AT_d = dscr("AT_d", [D, NOWN], BF16)
    BT_d = dscr("BT_d", [D, NOWN], BF16)
    H1_d = dscr("H1_d", [NOWN, D], F32)
    XG_d = dscr("XG_d", [NE * CAP, D], BF16)
    YG_d = dscr("YG_d", [NE * CAP, D], F32)

    es0 = ExitStack()
    S = Sched(nc, es0)

    def MM(out_, lhsT, rhs, start=True, stop=True, r=(), w=()):
        return S.add("pe", lambda e: e.matmul(out_, lhsT, rhs, start=start, stop=stop, skip_group_check=True), r, w)

    def TR(out_, in_, ident, r=(), w=()):
        return S.add("pe", lambda e: e.transpose(out_, in_, ident), r, w)

    def ACT(out_, in_, func, r=(), w=(), bias=0.0, scale=1.0, accum_out=None):
        if accum_out is None:
            return S.add("act", lambda e: e.activation(out_, in_, func, bias=bias, scale=scale), r, w)
        return S.add("act", lambda e: e.activation(out_, in_, func, bias=bias, scale=scale, accum_out=accum_out), r, w)

    def TS_(eng, out_, in0, s1, s2, op0, op1=None, r=(), w=(), accum_out=None):
        if op1 is None:
            return S.add(eng, lambda e: e.tensor_scalar(out_, in0, s1, None, op0), r, w)
        if accum_out is not None:
            return S.add(eng, lambda e: e.tensor_scalar(out_, in0, s1, s2, op0, op1, accum_out), r, w)
        return S.add(eng, lambda e: e.tensor_scalar(out_, in0, s1, s2, op0, op1), r, w)

    def TT(eng, out_, in0, in1, op, r=(), w=()):
        return S.add(eng, lambda e: e.tensor_tensor(out_, in0, in1, op), r, w)

    def STT(out_, in0, scalar, in1, op0, op1, r=(), w=(), accum_out=None):
        if accum_out is None:
            return S.add("dve", lambda e: e.scalar_tensor_tensor(out_, in0, scalar, in1, op0, op1), r, w)
        return S.add("dve", lambda e: e.scalar_tensor_tensor(out_, in0, scalar, in1, op0, op1, accum_out), r, w)

    def CP(eng, out_, in_, r=(), w=()):
        if eng == "act":
            return S.add("act", lambda e: e.activation(out_, in_, AF.Copy), r, w)
        return S.add(eng, lambda e: e.tensor_copy(out_, in_), r, w)

    def MEMSET(eng, ap, val, w=()):
        return S.add(eng, lambda e: e.memset(ap, val), (), w)

    def DMA(q, out_, in_, r=(), w=(), slow=False):
        if slow:
            return S.add(q, lambda e: e.dma_start(out=out_, in_=in_, allow_slow_non_contiguous=True), r, w, dma=True)
        return S.add(q, lambda e: e.dma_start(out=out_, in_=in_), r, w, dma=True)

    uid = [0]

    class Pool_:
        def __init__(self, es, name, n, shape, dt, psum=False):
            self.t = []
            uid[0] += 1
            for i in range(n):
                nm = "%s_%d_u%d" % (name, i, uid[0])
                if psum:
                    self.t.append((es.enter_context(nc.psum_tensor(nm, shape, dt)), nm))
                else:
                    self.t.append((es.enter_context(nc.sbuf_tensor(nm, shape, dt)), nm))
            self.i = 0

        def next(self):
            t = self.t[self.i % len(self.t)]
            self.i += 1
            return t

    def sb(es, name, shape, dt):
        uid[0] += 1
        return es.enter_context(nc.sbuf_tensor("%s_u%d" % (name, uid[0]), shape, dt))

    cstf = sb(es0, "cstf", [128, 8, 128], F32)
    cstb = sb(es0, "cstb", [128, 8, 128], BF16)
    ioe = sb(es0, "ioe", [128, 32], F32)
    Sst = sb(es0, "Sst", [128, 2, 4, 256], F32)
    Pb = sb(es0, "Pb", [128, 4], F32)
    IDX = sb(es0, "IDX", [128, 16, 4], I32)
    GATE = sb(es0, "GATE", [128, 16, 4], F32)
    DMA("sp", cstf[:, :, :], cst, w=["cstf"])
    DMA("sp", ioe[:, :], cst2, w=["ioe"])
    CP("dve", cstb[:, :, :], cstf[:, :, :], r=["cstf"], w=["cstb"])
    MEMSET("dve", Sst[:, :, :, :], 0.0, w=["Sf0", "Sf1", "Sf2", "Sf3", "Sb0", "Sb1", "Sb2", "Sb3"])
    MEMSET("dve", Pb[:, :], 1.0, w=["Pb"])
    identb = cstb[:, 0, :]
    identf = cstf[:, 0, :]
    TIb, TItb, TSb, TPb, LTb, ONEb = (cstb[:, j, :] for j in range(1, 7))
    CIb = cstb[:, 7, 0:2]
    CK = ["cstb"]

    stg_es = ExitStack()
    wst = {"pool": None, "n": 128}

    def new_stage(es, n, cols):
        wst["pool"] = Pool_(es, "wstage", n, [128, 8, cols], F32)
        wst["n"] = cols
    cast_rr = [0]
    es_ht = ExitStack()
    HT_own = sb(es_ht, "HT_own", [128, 8, NOWN], BF16)
    Wkv = sb(es_ht, "Wkv", [128, 8, 1536], BF16)

    def load_w(dst, dkey, wd, c0, ncols, dcol0=0, engs=("pool", "act")):
        PW = wst["n"]
        for p0 in range(0, ncols, PW):
            n = min(PW, ncols - p0)
            st, sk = wst["pool"].next()
            DMA("sp", st[:, :, 0:n], wd[:, c0 + p0:c0 + p0 + n].rearrange("(kc p) n -> p kc n", p=128), w=[sk])
            eng = engs[cast_rr[0] % len(engs)]
            cast_rr[0] += 1
            CP(eng, dst[:, :, dcol0 + p0:dcol0 + p0 + n], st[:, :, 0:n], r=[sk], w=[dkey])

    def load_w_rows(dst, dkey, wd, engs):
        for kc0 in range(0, 8, 2):
            st, sk = wst["pool"].next()
            stv = st[:, :, :].rearrange("p a b -> p (a b)").rearrange("p (k n) -> p k n", k=2)
            DMA("sp", stv, wd[kc0 * 128:(kc0 + 2) * 128, :].rearrange("(kc p) n -> p kc n", p=128), w=[sk])
            eng = engs[cast_rr[0] % len(engs)]
            cast_rr[0] += 1
            CP(eng, dst[:, kc0:kc0 + 2, :], stv, r=[sk], w=[dkey])

    def bcast_load(es, name, vec, n):
        t = sb(es, name, [128, n], F32)
        DMA("sp", t[:, :], vec.partition_broadcast(128), w=[name])
        return t

    def layernorm(es_tmp, pref, x_t, xkey, g_t, gkey, b_t, bkey, out_t, okey, small):
        st, stk = small.next()
        S.add("dve", lambda e: e.bn_stats(st[:, 0:6], x_t[:, 0:512]), [xkey], [stk])
        S.add("dve", lambda e: e.bn_stats(st[:, 6:12], x_t[:, 512:1024]), [xkey], [stk])
        S.add("dve", lambda e: e.bn_aggr(st[:, 12:14], st[:, 0:12]), [stk], [stk])
        ACT(st[:, 14:15], st[:, 13:14], AF.Ln, r=[stk], w=[stk], bias=EPS)
        ACT(st[:, 15:16], st[:, 14:15], AF.Exp, r=[stk], w=[stk], scale=-0.5)
        TS_("dve", out_t[:, :], x_t[:, :], st[:, 12:13], st[:, 15:16], ALU.subtract, ALU.mult, r=[xkey, stk], w=[okey])
        TT("dve", out_t[:, :], out_t[:, :], g_t[:, :], ALU.mult, r=[okey, gkey], w=[okey])
        TT("dve", out_t[:, :], out_t[:, :], b_t[:, :], ALU.add, r=[okey, bkey], w=[okey])

    NW1 = 3072
    with ExitStack() as es:
        new_stage(es, 2, 128)
        W1 = sb(es, "W1", [128, 8, NW1], BF16)
        Wlr = sb(es, "Wlr", [128, 8, 32], BF16)
        wfs = sb(es, "wfs", [17, 2, 512], F32)
        wfa = sb(es, "wfa", [17, 2, 512], BF16)
        Lt = sb(es, "Lt", [32, 2, 128], BF16)
        g0t = bcast_load(es, "g0t", ln0_g, D)
        b0t = bcast_load(es, "b0t", ln0_b, D)
        load_w(W1, "W1", w_in, C_KD, 1024, 0)
        load_w(W1, "W1", w_in, C_VD, 1024, 1024)
        load_w(W1, "W1", w_in, C_QD, 1024, 2048)
        load_w(Wkv, "Wkv", w_in, C_KG, 512, 0)
        load_w(Wkv, "Wkv", w_in, C_VG, 1024, 512)
        load_w(Wlr, "Wlr", w_in, C_LRF, 32, 0)
        DMA("sp", wfs[0:16, 0, :], wf_f, w=["wfs"])
        DMA("sp", wfs[16:17, 0, :], bf_f.rearrange("(a n) -> a n", a=1), w=["wfs"])
        DMA("sp", wfs[0:16, 1, :], wf_b, w=["wfs"])
        DMA("sp", wfs[16:17, 1, :], bf_b.rearrange("(a n) -> a n", a=1), w=["wfs"])
        CP("dve", wfa[:, :, :], wfs[:, :, :], r=["wfs"], w=["wfa"])
        MEMSET("dve", Lt[:, :, :], 1.0, w=["Lt0", "Lt1"])

        xr = Pool_(es, "xr", 2, [128, D], F32)
        csr = Pool_(es, "csr", 2, [128, 130], F32)
        small = Pool_(es, "small", 3, [128, 16], F32)
        hfr = Pool_(es, "hfr", 2, [128, D], F32)
        hbr = Pool_(es, "hbr", 2, [128, D], BF16)
        hTr = Pool_(es, "hTr", 2, [128, 8, 128], BF16)
        kbr = Pool_(es, "kbr", 2, [128, D], BF16)
        ktr = Pool_(es, "ktr", 2, [128, 8, 128], BF16)
        vbr = Pool_(es, "vbr", 2, [128, D], BF16)
        ropet = Pool_(es, "ropet", 2, [128, 4, 8, 8], F32)
        kgr = Pool_(es, "kgr", 2, [128, 512], BF16)
        vgr = Pool_(es, "vgr", 2, [128, D], BF16)
        f32r = Pool_(es, "f32r", 4, [128, 512], F32)
        lfr = Pool_(es, "lfr", 4, [128, 512], BF16)
        kstr = Pool_(es, "kstr", 4, [128, 512], BF16)
        decr = Pool_(es, "decr", 2, [128, 16], F32)
        nmr = Pool_(es, "nmr", 2, [128, 2], F32)
        ptp = Pool_(es, "ptp", 1, [128, 512], F32, psum=True)
        ppj = Pool_(es, "ppj", 2, [128, 512], F32, psum=True)
        pg = Pool_(es, "pg", 2, [128, 512], F32, psum=True)
        pu = Pool_(es, "pu", 2, [128, 512], F32, psum=True)
        psm = Pool_(es, "psm", 1, [128, 512], F32, psum=True)
        uslot = [0]

        def rope_and_store(pj, pjk, kb, kbk, cs, csk, half):
            dst = kb[:, half * 512:(half + 1) * 512]
            CP("act", dst, pj[:, :], r=[pjk], w=[kbk])
            if NO_ROPE:
                return
            d3 = dst.rearrange("p (g d) -> p g d", d=64)
            x1 = d3[:, :, 0:8]; x2 = d3[:, :, 8:16]
            cosb = cs[:, 0:64].rearrange("p (g d) -> p g d", d=8)
            sinb = cs[:, 64:128].rearrange("p (g d) -> p g d", d=8)
            rt, rk = ropet.next()
            TT("dve", rt[:, 0], x1, cosb, ALU.mult, r=[kbk, csk], w=[rk])
            TT("dve", rt[:, 1], x2, sinb, ALU.mult, r=[kbk, csk], w=[rk])
            TT("dve", rt[:, 2], x2, cosb, ALU.mult, r=[kbk, csk], w=[rk])
            TT("dve", rt[:, 3], x1, sinb, ALU.mult, r=[kbk, csk], w=[rk])
            TT("dve", d3[:, :, 0:8], rt[:, 0], rt[:, 1], ALU.subtract, r=[rk], w=[kbk])
            TT("dve", d3[:, :, 8:16], rt[:, 2], rt[:, 3], ALU.add, r=[rk], w=[kbk])

        pend = [iter(())]

        def proj_tok(hT, hTk, W, wkey, c0, pj, pjk):
            for kc in range(8):
                MM(pj[:, :], hT[:, kc, :], W[:, kc, c0:c0 + 512], start=(kc == 0), stop=(kc == 7),
                   r=[hTk, wkey], w=[pjk])
            next(pend[0], None)
            next(pend[0], None)

        def p1A(i):
                own = i >= 48
                xt, xk = xr.next()
                cs, csk = csr.next()
                DMA("sp", xt[:, :], xa[i * 128:(i + 1) * 128, :], w=[xk])
                DMA("sp", cs[:, :], csa[i * 128:(i + 1) * 128, :], w=[csk])
                if P1_LEVEL < 2:
                    return None
                hf, hfk = hfr.next()
                layernorm(es, "ln0", xt, xk, g0t, "g0t", b0t, "b0t", hf, hfk, small)
                hb, hbk = hbr.next()
                CP("act", hb[:, :], hf[:, :], r=[hfk], w=[hbk])
                if own:
                    DMA(STORE_Q, H_d[(i - 48) * 128:(i - 47) * 128, :], hf[:, :], r=[hfk], w=["H_d"])
                tp, tpk = ptp.next()
                tpb = tp[:, :].bitcast(BF16)
                for kc in range(8):
                    TR(tpb[:, kc * 128:(kc + 1) * 128], hb[:, kc * 128:(kc + 1) * 128], identb, r=[hbk] + CK, w=[tpk])
                hT, hTk = hTr.next()
                CP("act", hT[:, :, :], tpb[:, 0:1024].rearrange("p (k t) -> p k t", k=8), r=[tpk], w=[hTk])
                if own:
                    CP("pool", HT_own[:, :, (i - 48) * 128:(i - 47) * 128], hT[:, :, :], r=[hTk], w=["HT_own"])
                return dict(own=own, cs=cs, csk=csk, hT=hT, hTk=hTk)

        def p1B(i, c):
                if c is None:
                    return
                own = c["own"]; cs = c["cs"]; csk = c["csk"]; hT = c["hT"]; hTk = c["hTk"]
                if P1_LEVEL < 3:
                    return
                gla = (not own) and P1_LEVEL >= 4
                if gla:
                    kg, kgk = kgr.next()
                    pj, pjk = ppj.next()
                    proj_tok(hT, hTk, Wkv, "Wkv", 0, pj, pjk)
                    CP("act", kg[:, :], pj[:, :], r=[pjk], w=[kgk])
                    nm, nmk = nmr.next()
                    TS_("dve", nm[:, :], cs[:, 128:130], -1.0 / 16.0, None, ALU.mult, r=[csk], w=[nmk])
                    sm, smk = psm.next()
                    for d in range(2):
                        for kc in range(8):
                            MM(sm[0:16, d * 128:(d + 1) * 128], Wlr[:, kc, d * 16:(d + 1) * 16], hT[:, kc, :],
                               start=(kc == 0), stop=(kc == 7), r=["Wlr", hTk], w=[smk])
                    for d in range(2):
                        CP("act", Lt[0:16, d, :], sm[0:16, d * 128:(d + 1) * 128], r=[smk], w=["Lt%d" % d])
                    e1s = []
                    for d in range(2):
                        g1, g1k = pg.next()
                        MM(g1[:, :], Lt[0:17, d, :], wfa[0:17, d, :], r=["Lt%d" % d, "wfa"], w=[g1k])
                        e1, e1k = f32r.next()
                        ACT(e1[:, :], g1[:, :], AF.Exp, r=[g1k], w=[e1k], scale=-1.0)
                        e1s.append((e1, e1k))
                    lfs = []
                    for d in range(2):
                        e1, e1k = e1s[d]
                        ACT(e1[:, :], e1[:, :], AF.Ln, r=[e1k], w=[e1k], bias=1.0)
                        lf, lfk = lfr.next()
                        TS_("dve", lf[:, :], e1[:, :], nm[:, d:d + 1], None, ALU.mult, r=[e1k, nmk], w=[lfk])
                        lfs.append((lf, lfk))
                    vg, vgk = vgr.next()
                    for half in range(2):
                        pj, pjk = ppj.next()
                        proj_tok(hT, hTk, Wkv, "Wkv", 512 + half * 512, pj, pjk)
                        CP("act", vg[:, half * 512:(half + 1) * 512], pj[:, :], r=[pjk], w=[vgk])
                kb, kbk = kbr.next()
                for half in range(2):
                    pj, pjk = ppj.next()
                    proj_tok(hT, hTk, W1, "W1", half * 512, pj, pjk)
                    rope_and_store(pj, pjk, kb, kbk, cs, csk, half)
                if gla:
                    ksts = []
                    e2s = []
                    for d in range(2):
                        lf, lfk = lfs[d]
                        g2, g2k = pg.next()
                        MM(g2[:, :], (TSb if d == 0 else TPb), lf[:, :], r=CK + [lfk], w=[g2k])
                        e2, e2k = f32r.next()
                        ACT(e2[:, :], g2[:, :], AF.Exp, r=[g2k], w=[e2k])
                        e2s.append((e2, e2k))
                    for d in range(2):
                        e2, e2k = e2s[d]
                        kst, kstk = kstr.next()
                        STT(kst[:, :], e2[:, :], cs[:, 128 + d:129 + d], kg[:, :], ALU.mult, ALU.mult, r=[e2k, csk, kgk], w=[kstk])
                        ksts.append((kst, kstk))
                tp, tpk = ptp.next()
                tpb = tp[:, :].bitcast(BF16)
                for h in range(8):
                    TR(tpb[:, h * 128:(h + 1) * 128], kb[:, h * 128:(h + 1) * 128], identb, r=[kbk] + CK, w=[tpk])
                kt, ktk = ktr.next()
                CP("dve", kt[:, :, :], tpb[:, 0:1024].rearrange("p (k t) -> p k t", k=8), r=[tpk], w=[ktk])
                DMA(STORE_Q, KT_d[:, :, i * 128:(i + 1) * 128].rearrange("h p t -> p h t"), kt[:, :, :], r=[ktk], w=["KT_d"])
                vb, vbk = vbr.next()
                for half in range(2):
                    pj, pjk = ppj.next()
                    proj_tok(hT, hTk, W1, "W1", 1024 + half * 512, pj, pjk)
                    CP("act", vb[:, half * 512:(half + 1) * 512], pj[:, :], r=[pjk], w=[vbk])
                DMA(STORE_Q, V_d[i * 128:(i + 1) * 128, :], vb[:, :], r=[vbk], w=["V_d"])
                if own:
                    kb, kbk = kbr.next()
                    for half in range(2):
                        pj, pjk = ppj.next()
                        proj_tok(hT, hTk, W1, "W1", 2048 + half * 512, pj, pjk)
                        rope_and_store(pj, pjk, kb, kbk, cs, csk, half)
                    tp, tpk = ptp.next()
                    tpb = tp[:, :].bitcast(BF16)
                    for h in range(8):
                        TR(tpb[:, h * 128:(h + 1) * 128], kb[:, h * 128:(h + 1) * 128], identb, r=[kbk] + CK, w=[tpk])
                    kt, ktk = ktr.next()
                    CP("dve", kt[:, :, :], tpb[:, 0:1024].rearrange("p (k t) -> p k t", k=8), r=[tpk], w=[ktk])
                    DMA(STORE_Q, QT_d[:, :, (i - 48) * 128:(i - 47) * 128].rearrange("h p t -> p h t"), kt[:, :, :],
                        r=[ktk], w=["QT_d"])
                    return
                if not gla:
                    return
                for d in range(2):
                    lf, lfk = lfs[d]
                    for h in range(4):
                        col = 256 + d * 4 + h
                        MM(sm[:, col:col + 1], lf[:, h * 128:(h + 1) * 128], ONEb[:, 0:1], r=[lfk] + CK, w=[smk])
                dec, deck = decr.next()
                ACT(dec[:, 0:8], sm[:, 256:264], AF.Exp, r=[smk], w=[deck])

                def cgen(ksts=ksts, vg=vg, vgk=vgk, dec=dec, deck=deck):
                    for d in range(2):
                        kst, kstk = ksts[d]
                        for h in range(4):
                            ub, ubk = pu.next()
                            uap = ub[:, 0:256]
                            MM(uap, kst[:, h * 128:(h + 1) * 128], vg[:, h * 256:(h + 1) * 256], r=[kstk, vgk], w=[ubk])
                            if d == 0:
                                sk_ = "Sf%d" % h
                                STT(Sst[:, 0, h, :], Sst[:, 0, h, :], dec[:, h:h + 1], uap, ALU.mult, ALU.add,
                                    r=[sk_, deck, ubk], w=[sk_])
                            else:
                                sk_ = "Sb%d" % h
                                STT(Sst[:, 1, h, :], uap, Pb[:, h:h + 1], Sst[:, 1, h, :], ALU.mult, ALU.add,
                                    r=[sk_, "Pb", ubk], w=[sk_])
                            yield
                    TT("dve", Pb[:, :], Pb[:, :], dec[:, 4:8], ALU.mult, r=["Pb", deck], w=["Pb"])
                    yield
                for _ in pend[0]:
                    pass
                pend[0] = cgen()

        p1tiles = (P1_TILES if P1_LEVEL >= 1 else [])
        ctx = p1A(p1tiles[0]) if p1tiles else None
        for n_, i in enumerate(p1tiles):
            nxt = p1A(p1tiles[n_ + 1]) if n_ + 1 < len(p1tiles) else None
            p1B(i, ctx)
            ctx = nxt
        for _ in pend[0]:
            pass
        if stop == 1:
            DMA("sp", out[0:128, 0:1024].rearrange("p (a b c) -> p a b c", a=2, b=4), Sst[:, :, :, 0:128], r=["Sf0", "Sf1", "Sf2", "Sf3", "Sb0", "Sb1", "Sb2", "Sb3"], w=["dbg"])
            S.waitall("sp", ["dbg", "KT_d", "V_d", "QT_d", "H_d"])
        S.flush()
    if stop == 1:
        return nc

    NW2 = 2048
    with ExitStack() as es:
        for d in range(2):
            if d == 0:
                new_stage(es, 2, 128)
                Wq = sb(es, "Wq", [128, 8, 512], BF16)
                load_w(Wq, "Wq", w_in, C_QG, 512, 0)
                Wrg = sb(es, "Wrg", [128, 8, 1024], BF16)
                load_w(Wrg, "Wrg", w_in, C_RG, 1024, 0, engs=("pool",))
                ggt = bcast_load(es, "ggt", gla_g, 256)
            Wl2 = sb(es, "Wl2_%d" % d, [128, 8, 16], BF16)
            wfs2 = sb(es, "wfs2_%d" % d, [17, 512], F32)
            wfa2 = sb(es, "wfa2_%d" % d, [17, 512], BF16)
            Lt2 = sb(es, "Lt2_%d" % d, [32, 128], BF16)
            mask4 = sb(es, "mask4_%d" % d, [128, 4, 128], F32)
            load_w(Wl2, "Wl2", w_in, C_LRF if d == 0 else C_LRB, 16, 0)
            DMA("sp", wfs2[0:16, :], wf_f if d == 0 else wf_b, w=["wfs2"])
            DMA("sp", wfs2[16:17, :], (bf_f if d == 0 else bf_b).rearrange("(a n) -> a n", a=1), w=["wfs2"])
            CP("dve", wfa2[:, :], wfs2[:, :], r=["wfs2"], w=["wfa2"])
            MEMSET("dve", Lt2[:, :], 1.0, w=["Lt2"])
            for h in range(4):
                CP("dve", mask4[:, h, :], cstf[:, 1 if d == 0 else 2, :], r=["cstf"], w=["mask4"])
            if d == 0:
                ofr = Pool_(es, "ofr", 2, [128, D], F32)
                osr = Pool_(es, "osr", 2, [128, D], F32)
                abr = Pool_(es, "abr", 2, [128, D], BF16)
                atr = Pool_(es, "atr", 2, [128, 8, 128], BF16)
                srr = Pool_(es, "srr", 2, [128, 512], F32)
                small = Pool_(es, "small3", 2, [128, 16], F32)
                kgr = Pool_(es, "kgr2", 2, [128, 512], BF16)
                vgr = Pool_(es, "vgr2", 2, [128, D], BF16)
                f32r = Pool_(es, "f32r2", 6, [128, 512], F32)
                lfr = Pool_(es, "lfr2", 2, [128, 512], BF16)
                kstr = Pool_(es, "kstr2", 2, [128, 512], BF16)
                eqr = Pool_(es, "eqr", 3, [128, 512], F32)
                qinr = Pool_(es, "qinr", 2, [128, 512], BF16)
                kinr = Pool_(es, "kinr", 2, [128, 512], BF16)
                attr = Pool_(es, "attr", 2, [128, 512], BF16)
                ppj = Pool_(es, "ppj2", 1, [128, 512], F32, psum=True)
                pg = Pool_(es, "pg2", 3, [128, 512], F32, psum=True)
                pu = Pool_(es, "pu2", 2, [128, 512], F32, psum=True)
                po = Pool_(es, "po2", 2, [128, 512], F32, psum=True)
            Sbf = [Pool_(es, "Sbf%d_%d" % (h, d), 2, [128, 256], BF16) for h in range(4)]
            uslot = [0]
            oslot = [0]
            skey = ["S%s%d" % ("f" if d == 0 else "b", h) for h in range(4)]
            cur = []
            for h in range(4):
                t, tk = Sbf[h].next()
                CP("pool", t[:, :], Sst[:, d, h, :], r=[skey[h]], w=[tk])
                cur.append((t, tk))
            tiles = list(range(OWN_N)) if d == 0 else list(range(OWN_N - 1, -1, -1))
            chunks = (0,)
            def stageA(i):
                    hT = HT_own[:, :, i * 128:(i + 1) * 128]
                    hTk = "HT_own"
                    kg, kgk = kgr.next()
                    pj, pjk = ppj.next()
                    for kc in range(8):
                        MM(pj[:, :], hT[:, kc, :], Wkv[:, kc, 0:512], start=(kc == 0), stop=(kc == 7), r=["Wkv", hTk], w=[pjk])
                    CP("act", kg[:, :], pj[:, :], r=[pjk], w=[kgk])
                    vg, vgk = vgr.next()
                    for half in range(2):
                        pj, pjk = ppj.next()
                        for kc in range(8):
                            MM(pj[:, :], hT[:, kc, :], Wkv[:, kc, 512 + half * 512:1024 + half * 512],
                               start=(kc == 0), stop=(kc == 7), r=["Wkv", hTk], w=[pjk])
                        CP("act", vg[:, half * 512:(half + 1) * 512], pj[:, :], r=[pjk], w=[vgk])
                    g1, g1k = pg.next()
                    for kc in range(8):
                        MM(g1[0:16, 0:128], Wl2[:, kc, 0:16], hT[:, kc, :], start=(kc == 0), stop=(kc == 7),
                           r=["Wl2", hTk], w=[g1k])
                    CP("act", Lt2[0:16, :], g1[0:16, 0:128], r=[g1k], w=["Lt2"])
                    MM(g1[:, :], Lt2[0:17, :], wfa2[0:17, :], r=["Lt2", "wfa2"], w=[g1k])
                    e1, e1k = f32r.next()
                    ACT(e1[:, :], g1[:, :], AF.Exp, r=[g1k], w=[e1k], scale=-1.0)
                    ACT(e1[:, :], e1[:, :], AF.Ln, r=[e1k], w=[e1k], bias=1.0)
                    lf, lfk = lfr.next()
                    TS_("dve", lf[:, :], e1[:, :], -1.0 / 16.0, None, ALU.mult, r=[e1k], w=[lfk])
                    gb, gbk = pg.next()
                    for h in range(4):
                        MM(gb[:, h * 128:(h + 1) * 128], lf[:, h * 128:(h + 1) * 128], (TIb if d == 0 else TItb),
                           r=[lfk] + CK, w=[gbk])
                    Eq, Eqk = eqr.next()
                    Ek, Ekk = f32r.next()
                    ACT(Eq[:, :], gb[:, :], AF.Exp, r=[gbk], w=[Eqk])
                    ACT(Ek[:, :], gb[:, :], AF.Exp, r=[gbk], w=[Ekk], scale=-1.0)
                    g2, g2k = pg.next()
                    MM(g2[:, :], (TSb if d == 0 else TPb), lf[:, :], r=CK + [lfk], w=[g2k])
                    e2, e2k = f32r.next()
                    ACT(e2[:, :], g2[:, :], AF.Exp, r=[g2k], w=[e2k])
                    kst, kstk = kstr.next()
                    TT("dve", kst[:, :], e2[:, :], kg[:, :], ALU.mult, r=[e2k, kgk], w=[kstk])
                    gq, gqk = pg.next()
                    gk, gkk = pg.next()
                    for h in range(4):
                        for kc in range(8):
                            MM(gq[:, h * 128:(h + 1) * 128], Wq[:, kc, h * 128:(h + 1) * 128], hT[:, kc, :],
                               start=(kc == 0), stop=(kc == 7), r=["Wq", hTk], w=[gqk])
                    for h in range(4):
                        for kc in range(8):
                            MM(gk[:, h * 128:(h + 1) * 128], Wkv[:, kc, h * 128:(h + 1) * 128], hT[:, kc, :],
                               start=(kc == 0), stop=(kc == 7), r=["Wkv", hTk], w=[gkk])
                    qin, qink = qinr.next()
                    kin, kink = kinr.next()
                    STT(qin[:, :], gq[:, :], 128.0 ** -0.5, Eq[:, :], ALU.mult, ALU.mult, r=[gqk, Eqk], w=[qink])
                    TT("dve", kin[:, :], gk[:, :], Ek[:, :], ALU.mult, r=[gkk, Ekk], w=[kink])
                    ga, gak = pg.next()
                    for h in range(4):
                        MM(ga[:, h * 128:(h + 1) * 128], kin[:, h * 128:(h + 1) * 128], qin[:, h * 128:(h + 1) * 128],
                           r=[kink, qink], w=[gak])
                    att, attk = attr.next()
                    TT("dve", att[:, :], ga[:, :], mask4[:, :, :].rearrange("p h t -> p (h t)"), ALU.mult,
                       r=[gak, "mask4"], w=[attk])
                    if d == 1:
                        of, ofk = ofr.next()
                        DMA("sp", of[:, :], OF_d[i * 128:(i + 1) * 128, :], r=["OF_d"], w=[ofk])
                        osum, osk = osr.next()
                    else:
                        of, ofk = ofr.next()
                    return dict(hT=hT, hTk=hTk, vg=vg, vgk=vgk, kst=kst, kstk=kstk, qin=qin, qink=qink, att=att, attk=attk,
                                Eq=Eq, Eqk=Eqk, of=of, ofk=ofk, osum=(osum if d == 1 else None), osk=(osk if d == 1 else None))

            def stageB(i, c):
                    hT = c["hT"]; hTk = c["hTk"]; vg = c["vg"]; vgk = c["vgk"]; kst = c["kst"]; kstk = c["kstk"]
                    qin = c["qin"]; qink = c["qink"]; att = c["att"]; attk = c["attk"]; Eq = c["Eq"]; Eqk = c["Eqk"]
                    of = c["of"]; ofk = c["ofk"]; osum = c["osum"]; osk = c["osk"]
                    obs = []
                    for h in range(4):
                        ob_, obk_ = po.t[h % 2]
                        c0 = (h // 2) * 256
                        obs.append((ob_, obk_, c0))
                        MM(ob_[:, c0:c0 + 256], att[:, h * 128:(h + 1) * 128], vg[:, h * 256:(h + 1) * 256],
                           start=(h < 2), stop=False, r=[attk, vgk], w=[obk_])
                    for ci, ch in enumerate(chunks):
                        r0 = ch * 64
                        for h in range(4):
                            ob_, obk_, c0 = obs[h]
                            st_bf, st_k = cur[h]
                            MM(ob_[:, c0:c0 + 256], qin[:, h * 128:(h + 1) * 128], st_bf[:, :],
                               start=False, stop=True, r=[qink, st_k], w=[obk_])
                        for h in range(4):
                            ub, ubk = pu.t[h % 2]
                            uc0 = (h // 2) * 256
                            uap = ub[:, uc0:uc0 + 256]
                            MM(uap, kst[:, h * 128:(h + 1) * 128], vg[:, h * 256:(h + 1) * 256],
                               r=[kstk, vgk], w=[ubk])
                            if d == 0:
                                dcol = h * 128 + 127
                            else:
                                dcol = h * 128
                            STT(Sst[:, d, h, :], Sst[:, d, h, :], Eq[:, dcol:dcol + 1], uap, ALU.mult, ALU.add,
                                r=[skey[h], Eqk, ubk], w=[skey[h]])
                            t, tk = Sbf[h].next()
                            CP("pool", t[:, :], Sst[:, d, h, :], r=[skey[h]], w=[tk])
                            cur[h] = (t, tk)
                    for h in range(4):
                        ob_, obk_, c0 = obs[h]
                        ob = ob_[:, c0:c0 + 256]
                        if d == 0:
                            CP("act", of[:, h * 256:(h + 1) * 256], ob, r=[obk_], w=[ofk])
                        else:
                            TT("dve", osum[:, h * 256:(h + 1) * 256], ob, of[:, h * 256:(h + 1) * 256], ALU.add,
                               r=[obk_, ofk], w=[osk])
                    if d == 0:
                        DMA(STORE_Q, OF_d[i * 128:(i + 1) * 128, :], of[:, :], r=[ofk], w=["OF_d"])
                        return
                    st, stk = small.next()
                    junk, junkk = f32r.next()
                    for h in range(4):
                        STT(junk[:, 0:256], osum[:, h * 256:(h + 1) * 256], 1.0, osum[:, h * 256:(h + 1) * 256],
                            ALU.mult, ALU.mult, r=[osk], w=[junkk, stk], accum_out=st[:, h:h + 1])
                    ACT(st[:, 4:8], st[:, 0:4], AF.Ln, r=[stk], w=[stk], bias=EPS, scale=1.0 / 256.0)
                    ACT(st[:, 8:12], st[:, 4:8], AF.Exp, r=[stk], w=[stk], scale=-0.5)
                    for h in range(4):
                        STT(osum[:, h * 256:(h + 1) * 256], osum[:, h * 256:(h + 1) * 256], st[:, 8 + h:9 + h], ggt[:, :],
                            ALU.mult, ALU.mult, r=[osk, stk, "ggt"], w=[osk])
                    ab, abk = abr.next()
                    for half in range(2):
                        pj, pjk = ppj.next()
                        for kc in range(8):
                            MM(pj[:, :], hT[:, kc, :], Wrg[:, kc, half * 512:(half + 1) * 512], start=(kc == 0), stop=(kc == 7),
                               r=["Wrg", hTk], w=[pjk])
                        sr, srk = srr.next()
                        ACT(sr[:, :], pj[:, :], AF.Silu, r=[pjk], w=[srk])
                        TT("dve", ab[:, half * 512:(half + 1) * 512], osum[:, half * 512:(half + 1) * 512], sr[:, :], ALU.mult,
                           r=[osk, srk], w=[abk])
                    tp, tpk = pg.next()
                    tpb = tp[:, :].bitcast(BF16)
                    for kc in range(8):
                        TR(tpb[:, kc * 128:(kc + 1) * 128], ab[:, kc * 128:(kc + 1) * 128], identb, r=[abk] + CK, w=[tpk])
                    at, atk = atr.next()
                    CP("act", at[:, :, :], tpb[:, 0:1024].rearrange("p (k t) -> p k t", k=8), r=[tpk], w=[atk])
                    DMA(STORE_Q, AT_d[:, i * 128:(i + 1) * 128].rearrange("(kc p) t -> p kc t", p=128), at[:, :, :],
                        r=[atk], w=["AT_d"])

            ctx = stageA(tiles[0])
            for n_, i in enumerate(tiles):
                nxt = stageA(tiles[n_ + 1]) if n_ + 1 < len(tiles) else None
                stageB(i, ctx)
                ctx = nxt
            if d == 1:
                DMA(STORE_Q, HT_d[:, 0:OWN_N * 128].rearrange("(kc p) t -> p kc t", p=128), HT_own[:, :, 0:OWN_N * 128], r=["HT_own"], w=["HT_d"])
        if stop in (2, 3):
            S.waitall("sp", ["OF_d", "AT_d", "HT_d"])
        S.flush()
    if stop in (2, 3):
        return nc
    es_ht.close()

    es5 = ExitStack()
    stage5 = Pool_(es5, "wstage5", 2, [128, 8, 128], F32)
    Wa = sb(es5, "Wa", [128, 8, D], BF16)
    Wb = sb(es5, "Wb", [128, 8, D], BF16)
    Wo = sb(es5, "Wo", [128, 8, D], BF16)
    Wgg = sb(es5, "Wgg", [128, 8, D], BF16)
    Wgd = sb(es5, "Wgd", [128, 8, D], BF16)

    def prefetch_p5():
        wst["pool"] = stage5
        wst["n"] = 128
        load_w(Wa, "Wa", w_bra, 0, D, engs=("pool", "dve"))
        load_w(Wb, "Wb", w_brb, 0, D, engs=("pool", "dve"))
        load_w(Wo, "Wo", w_out, 0, D, engs=("pool", "dve"))
        load_w(Wgg, "Wgg", w_in, C_GG, D, engs=("pool", "dve"))
        load_w(Wgd, "Wgd", w_in, C_GD, D, engs=("pool", "dve"))

    with ExitStack() as es:
        ktp = Pool_(es, "ktp", 2, [128, NALL], BF16)
        vtp = Pool_(es, "vtp", 2, [128, 64, 129], BF16)
        qtp = Pool_(es, "qtp", 2, [128, NOWN], BF16)
        ptr = [Pool_(es, "ptr%d" % m, 3, [128, 512], BF16) for m in range(2)]
        bthr = Pool_(es, "bthr", 2, [128, NOWN], BF16)
        lv = sb(es, "lv", [128, 4, 64], F32)
        lsm = sb(es, "lsm", [128, 16], F32)
        gdt = bcast_load(es, "gdt", diff_g, 128)
        fin = Pool_(es, "fin", 8, [128, 16], F32)
        t1r = Pool_(es, "t1r", 2, [128, 128], F32)
        o1r = Pool_(es, "o1r", 8, [128, 128], F32)
        jr = Pool_(es, "jr", 2, [128, 128], F32)
        bbr = Pool_(es, "bbr", 2, [128, 128], BF16)
        accs = Pool_(es, "accs", 6, [128, 388], F32)
        pst = [Pool_(es, "pst%d" % m, 2, [128, 512], F32, psum=True) for m in range(2)]
        pacc = Pool_(es, "pacc", 3, [128, 512], F32, psum=True)
        ptp = Pool_(es, "ptp4", 1, [128, 512], F32, psum=True)
        for j, v in enumerate((lq1, lk1, lq2, lk2)):
            DMA("sp", lv[:, j, :], v.partition_broadcast(128), w=["lv"])
        junk, junkk = jr.next()
        STT(junk[:, 0:64], lv[:, 0, :], 1.0, lv[:, 1, :], ALU.mult, ALU.mult, r=["lv"], w=[junkk, "lsm"], accum_out=lsm[:, 0:1])
        STT(junk[:, 0:64], lv[:, 2, :], 1.0, lv[:, 3, :], ALU.mult, ALU.mult, r=["lv"], w=[junkk, "lsm"], accum_out=lsm[:, 1:2])
        ACT(lsm[:, 2:4], lsm[:, 0:2], AF.Exp, r=["lsm"], w=["lsm"])
        TT("dve", lsm[:, 4:5], lsm[:, 3:4], lsm[:, 2:3], ALU.subtract, r=["lsm"], w=["lsm"])
        TS_("dve", lsm[:, 5:6], lsm[:, 4:5], -LAM_INIT, None, ALU.add, r=["lsm"], w=["lsm"])
        TS_("dve", gdt[:, :], gdt[:, :], 1.0 - LAM_INIT, None, ALU.mult, r=["gdt"], w=["gdt"])
        for t, tk in vtp.t:
            MEMSET("pool", t[:, :, 128:129], 1.0, w=[tk])
        accpos = {}
        n = 0
        for j in range(4):
            for m in range(2):
                accpos[(j, m)] = (n // 3, (n % 3) * 129)
                n += 1
        deferred = [None, None]

        def make_finalize(h, qb, sacc, bth, bthk, last):
            st = {}

            def part1():
                for j in range(4):
                    b0_, c00 = accpos[(j, 0)]
                    b1_, c01 = accpos[(j, 1)]
                    a0, a0k = sacc[b0_]
                    a1, a1k = sacc[b1_]
                    f, fk = fin.next()
                    S.add("dve", lambda e, f=f, a0=a0, c00=c00: e.reciprocal(f[:, 0:1], a0[:, c00 + 128:c00 + 129]), [a0k], [fk])
                    S.add("dve", lambda e, f=f, a1=a1, c01=c01: e.reciprocal(f[:, 1:2], a1[:, c01 + 128:c01 + 129]), [a1k], [fk])
                    TT("dve", f[:, 2:3], f[:, 1:2], lsm[:, 5:6], ALU.mult, r=[fk, "lsm"], w=[fk])
                    t1, t1k = t1r.next()
                    TS_("dve", t1[:, :], a1[:, c01:c01 + 128], f[:, 2:3], None, ALU.mult, r=[a1k, fk], w=[t1k])
                    o1, o1k = o1r.next()
                    STT(o1[:, :], a0[:, c00:c00 + 128], f[:, 0:1], t1[:, :], ALU.mult, ALU.add, r=[a0k, fk, t1k], w=[o1k])
                    junk, junkk = jr.next()
                    STT(junk[:, :], o1[:, :], 1.0, o1[:, :], ALU.mult, ALU.mult, r=[o1k], w=[junkk, fk], accum_out=f[:, 3:4])
                    st[j] = (f, fk, o1, o1k)

            def part2():
                for j in range(4):
                    f, fk, o1, o1k = st[j]
                    ACT(f[:, 4:5], f[:, 3:4], AF.Ln, r=[fk], w=[fk], bias=EPS, scale=1.0 / 128.0)
                    ACT(f[:, 5:6], f[:, 4:5], AF.Exp, r=[fk], w=[fk], scale=-0.5)
                for j in range(4):
                    f, fk, o1, o1k = st[j]
                    bb, bbk = bbr.next()
                    STT(bb[:, :], o1[:, :], f[:, 5:6], gdt[:, :], ALU.mult, ALU.mult, r=[o1k, fk, "gdt"], w=[bbk])
                    tp, tpk = ptp.t[0]
                    tpb = tp[:, :].bitcast(BF16)
                    TR(tpb[:, 0:128], bb[:, :], identb, r=[bbk] + CK, w=[tpk])
                    tok0 = qb * 512 + j * 128
                    CP("dve", bth[:, tok0:tok0 + 128], tpb[:, 0:128], r=[tpk], w=[bthk])
                if last:
                    DMA(STORE_Q, BT_d[h * 128:(h + 1) * 128, 0:P4_QB * 512], bth[:, 0:P4_QB * 512], r=[bthk], w=["BT_d"])
            return part1, part2

        for h in range(P4_H):
            kT, kTk = ktp.next()
            vt, vtk = vtp.next()
            qT, qTk = qtp.next()
            DMA("sp", kT[:, :], KT_d[h, :, :], r=["KT_d"], w=[kTk])
            DMA("sp", qT[:, :], QT_d[h, :, :], r=["QT_d"], w=[qTk])
            for q4 in range(4):
                DMA("sp", vt[:, q4 * 16:(q4 + 1) * 16, 0:128],
                    V_d[q4 * 2048:(q4 + 1) * 2048, h * 128:(h + 1) * 128].rearrange("(kt p) c -> p kt c", p=128),
                    r=["V_d"], w=[vtk])
            if h == 0:
                prefetch_p5()
            bth, bthk = bthr.next()
            for qb in range(P4_QB):
                banks = [pacc.t[b] for b in range(3)]
                started = [False, False, False]
                pts_of = {}

                def qk_exp(kt_, m):
                    st_, stk_ = pst[m].next()
                    MM(st_[:, :], kT[m * 64:(m + 1) * 64, kt_ * 128:(kt_ + 1) * 128],
                       qT[m * 64:(m + 1) * 64, qb * 512:(qb + 1) * 512], r=[kTk, qTk], w=[stk_])
                    pt, ptk = ptr[m].next()
                    ACT(pt[:, :], st_[:, :], AF.Exp, r=[stk_], w=[ptk], scale=0.125)
                    pts_of[(kt_, m)] = (pt, ptk)

                for k0 in range(2):
                    for m in range(2):
                        qk_exp(k0, m)
                for kt_ in range(P4_KT):
                    if kt_ == 2 and deferred[0] is not None:
                        deferred[0]()
                    if kt_ == 12 and deferred[1] is not None:
                        deferred[1]()
                        deferred[0] = deferred[1] = None
                    for m in range(2):
                        pt, ptk = pts_of.pop((kt_, m))
                        for j in range(4):
                            b, c0 = accpos[(j, m)]
                            bt_, btk_ = banks[b]
                            first = (kt_ == 0 and not started[b])
                            if kt_ == 0:
                                started[b] = True
                            MM(bt_[:, c0:c0 + 129], pt[:, j * 128:(j + 1) * 128], vt[:, kt_, :],
                               start=first, stop=(kt_ == P4_KT - 1), r=[ptk, vtk], w=[btk_])
                    if kt_ + 2 < P4_KT:
                        qk_exp(kt_ + 2, 0)
                        qk_exp(kt_ + 2, 1)
                sacc = []
                for b in range(3):
                    a_, ak_ = accs.next()
                    ncol = 387 if b < 2 else 258
                    CP("dve", a_[:, 0:ncol], banks[b][0][:, 0:ncol], r=[banks[b][1]], w=[ak_])
                    sacc.append((a_, ak_))
                p1_, p2_ = make_finalize(h, qb, sacc, bth, bthk, qb == P4_QB - 1)
                deferred[0], deferred[1] = p1_, p2_
        if deferred[0] is not None:
            deferred[0]()
            deferred[1]()
        if stop == 4:
            S.waitall("sp", ["BT_d"])
        S.flush()
    if stop == 4:
        return nc

    with ExitStack() as es:
        Wr = sb(es, "Wr", [128, 8, NE], F32)
        DMA("sp", Wr[:, :, :], router_w.rearrange("(kc p) n -> p kc n", p=128), w=["Wr"])
        g1t = bcast_load(es, "g1t", ln1_g, D)
        b1t = bcast_load(es, "b1t", ln1_b, D)
        rbt = bcast_load(es, "rbt", router_b, NE)
        cum = sb(es, "cum", [128, NE], F32)
        MEMSET("dve", cum[:, :], 0.0, w=["cum"])
        blk = Pool_(es, "blk", 3, [128, 8, 512], BF16)
        mTt = sb(es, "mTt", [128, 8, 512], BF16)
        sgr = Pool_(es, "sgr", 4, [128, 512], F32)
        hr = Pool_(es, "hr5", 2, [128, D], F32)
        rr = Pool_(es, "rr5", 2, [128, D], F32)
        h1r = Pool_(es, "h1r", 2, [128, D], F32)
        h1br = Pool_(es, "h1br", 2, [128, D], BF16)
        h1Tr = Pool_(es, "h1Tr", 2, [128, 8, 128], F32)
        small = Pool_(es, "small5", 3, [128, 16], F32)
        rt = Pool_(es, "rt5", 2, [128, 8, 32], F32)
        mkb = Pool_(es, "mkb5", 2, [128, 32], BF16)
        pm = Pool_(es, "pm5", 4, [128, 512], F32, psum=True)
        pt2 = Pool_(es, "pt5", 2, [128, 512], F32, psum=True)
        pr = Pool_(es, "pr5", 2, [128, 512], F32, psum=True)
        for tb in range(P5_TB):
            at, atk = blk.next()
            bt, btk = blk.next()
            ht, htk = blk.next()
            cols = slice(tb * 512, (tb + 1) * 512)
            DMA("sp", at[:, :, :], AT_d[:, cols].rearrange("(kc p) t -> p kc t", p=128), r=["AT_d"], w=[atk])
            DMA("sp", bt[:, :, :], BT_d[:, cols].rearrange("(kc p) t -> p kc t", p=128), r=["BT_d"], w=[btk])
            DMA("sp", ht[:, :, :], HT_d[:, cols].rearrange("(kc p) t -> p kc t", p=128), r=["HT_d"], w=[htk])
            for oc in range(8):
                ocs = slice(oc * 128, (oc + 1) * 128)
                pa, pak = pm.next(); pga, pgak = pm.next(); pb, pbk = pm.next(); pgb, pgbk = pm.next()
                for (po_, pok_, W_, wk_, x_, xk_) in ((pa, pak, Wa, "Wa", at, atk), (pga, pgak, Wgg, "Wgg", ht, htk),
                                                    (pb, pbk, Wb, "Wb", bt, btk), (pgb, pgbk, Wgd, "Wgd", ht, htk)):
                    for kc in range(8):
                        MM(po_[:, :], W_[:, kc, ocs], x_[:, kc, :], start=(kc == 0), stop=(kc == 7), r=[wk_, xk_], w=[pok_])
                sa, sak = sgr.next(); sb_, sbk = sgr.next()
                ACT(sa[:, :], pga[:, :], AF.Sigmoid, r=[pgak], w=[sak])
                ACT(sb_[:, :], pgb[:, :], AF.Sigmoid, r=[pgbk], w=[sbk])
                TT("dve", sa[:, :], pa[:, :], sa[:, :], ALU.mult, r=[pak, sak], w=[sak])
                TT("dve", sb_[:, :], pb[:, :], sb_[:, :], ALU.mult, r=[pbk, sbk], w=[sbk])
                TT("dve", mTt[:, oc, :], sa[:, :], sb_[:, :], ALU.add, r=[sak, sbk], w=["mTt"])
            for tt in range(P5_TT):
                ti = tb * 4 + tt
                h0, h0k = hr.next()
                DMA("sp", h0[:, :], H_d[ti * 128:(ti + 1) * 128, :], r=["H_d"], w=[h0k])
                r_, rk_ = rr.next()
                for half in range(2):
                    po_, pok_ = pm.next()
                    for kc in range(8):
                        MM(po_[:, :], mTt[:, kc, tt * 128:(tt + 1) * 128], Wo[:, kc, half * 512:(half + 1) * 512],
                           start=(kc == 0), stop=(kc == 7), r=["mTt", "Wo"], w=[pok_])
                    STT(r_[:, half * 512:(half + 1) * 512], h0[:, half * 512:(half + 1) * 512], DN_ALPHA, po_[:, :],
                        ALU.mult, ALU.add, r=[h0k, pok_], w=[rk_])
                h1, h1k = h1r.next()
                layernorm(es, "ln1", r_, rk_, g1t, "g1t", b1t, "b1t", h1, h1k, small)
                DMA(STORE_Q, H1_d[ti * 128:(ti + 1) * 128, :], h1[:, :], r=[h1k], w=["H1_d"])
                h1b, h1bk = h1br.next()
                CP("act", h1b[:, :], h1[:, :], r=[h1k], w=[h1bk])
                h1T, h1Tk = h1Tr.next()
                for half in range(2):
                    tp, tpk = pt2.next()
                    for q in range(4):
                        kc = half * 4 + q
                        TR(tp[:, q * 128:(q + 1) * 128], h1[:, kc * 128:(kc + 1) * 128], identf, r=[h1k, "cstf"], w=[tpk])
                    CP("act", h1T[:, half * 4:(half + 1) * 4, :], tp[:, :].rearrange("p (k t) -> p k t", k=4), r=[tpk], w=[h1Tk])
                pl, plk = pr.next()
                for kc in range(8):
                    MM(pl[:, 0:NE], h1T[:, kc, :], Wr[:, kc, :], start=(kc == 0), stop=(kc == 7), r=[h1Tk, "Wr"], w=[plk])
                R, Rk = rt.next()
                sm, smk = small.next()
                TT("dve", R[:, 0, :], pl[:, 0:NE], rbt[:, :], ALU.add, r=[plk, "rbt"], w=[Rk])
                S.add("dve", lambda e, sm=sm, R=R: e.max(sm[:, 0:8], R[:, 0, :]), [Rk], [smk])
                TS_("dve", R[:, 1, :], R[:, 0, :], sm[:, 3:4], None, ALU.is_ge, r=[Rk, smk], w=[Rk])
                TS_("dve", sm[:, 8:9], sm[:, 0:1], -1.0, None, ALU.mult, r=[smk], w=[smk])
                ACT(R[:, 2, :], R[:, 0, :], AF.Exp, r=[Rk, smk], w=[Rk], bias=sm[:, 8:9])
                STT(R[:, 2, :], R[:, 2, :], 1.0, R[:, 1, :], ALU.mult, ALU.mult, r=[Rk], w=[Rk, smk], accum_out=sm[:, 9:10])
                S.add("dve", lambda e, sm=sm: e.reciprocal(sm[:, 10:11], sm[:, 9:10]), [smk], [smk])
                TS_("dve", R[:, 3, :], R[:, 2, :], sm[:, 10:11], None, ALU.mult, r=[Rk, smk], w=[Rk])
                mb, mbk = mkb.next()
                CP("dve", mb[:, :], R[:, 1, :], r=[Rk], w=[mbk])
                pc, pck = pr.next()
                MM(pc[:, 0:NE], LTb, mb[:, :], r=CK + [mbk], w=[pck])
                MM(pc[:, 64:64 + NE], ONEb, mb[:, :], r=CK + [mbk], w=[pck])
                TT("dve", R[:, 4, :], pc[:, 0:NE], cum[:, :], ALU.add, r=[pck, "cum"], w=[Rk])
                TS_("dve", R[:, 4, :], R[:, 4, :], float(CAP - 1), None, ALU.min, r=[Rk], w=[Rk])
                TT("dve", R[:, 4, :], R[:, 4, :], ioe[:, :], ALU.add, r=[Rk, "ioe"], w=[Rk])
                TT("dve", cum[:, :], cum[:, :], pc[:, 64:64 + NE], ALU.add, r=[pck, "cum"], w=["cum"])
                for k in range(4):
                    TS_("dve", R[:, 5, :], R[:, 0, :], sm[:, k:k + 1], None, ALU.is_equal, r=[Rk, smk], w=[Rk])
                    STT(R[:, 6, :], R[:, 5, :], 1.0, R[:, 4, :], ALU.mult, ALU.mult, r=[Rk], w=[Rk, smk],
                        accum_out=sm[:, 11 + k:12 + k])
                    STT(R[:, 6, :], R[:, 5, :], 1.0, R[:, 3, :], ALU.mult, ALU.mult, r=[Rk], w=[Rk, "GATE"],
                        accum_out=GATE[:, ti, k:k + 1])
                CP("dve", IDX[:, ti, :], sm[:, 11:15], r=[smk], w=["IDX"])
                for k in range(4):
                    S.add("pool", lambda e, h1b=h1b, ti=ti, k=k: e.indirect_dma_start(
                        out=XG_d[:, :], out_offset=bass.IndirectOffsetOnAxis(ap=IDX[:, ti, k:k + 1], axis=0),
                        in_=h1b[:, :], in_offset=None), r=[h1bk, "IDX"], w=["XG_d"], dma=True)
        if stop == 5:
            S.waitall("sp", ["XG_d", "H1_d"])
        S.flush()
    es5.close()
    if stop == 5:
        return nc

    with ExitStack() as es:
        new_stage(es, 4, 256)
        wgp = Pool_(es, "wgp", 2, [128, 8, D], BF16)
        wup = Pool_(es, "wup", 2, [128, 8, D], BF16)
        wdp = Pool_(es, "wdp", 2, [128, 8, D], BF16)
        bcol = sb(es, "bcol", [128, 2, 256], F32)
        bld = Pool_(es, "bld", 2, [128, 128], F32)
        bdf = Pool_(es, "bdf", 2, [1, D], F32)
        bdb = Pool_(es, "bdb", 2, [1, D], BF16)
        xgp = Pool_(es, "xgp", 2, [128, D], BF16)
        xTp = Pool_(es, "xTp", 2, [128, 8, CAP], BF16)
        aTp = Pool_(es, "aTp", 2, [128, 8, CAP], BF16)
        gr = Pool_(es, "gr7", 2, [128, CAP], F32)
        ur = Pool_(es, "ur7", 2, [128, CAP], F32)
        sr7 = Pool_(es, "sr7", 2, [128, CAP], F32)
        yr = Pool_(es, "yr7", 2, [128, D], F32)
        pgu = Pool_(es, "pgu7", 4, [128, 512], F32, psum=True)
        py = Pool_(es, "py7", 2, [128, 512], F32, psum=True)
        ptp = Pool_(es, "ptp7", 2, [128, 512], F32, psum=True)
        for gi, bsrc in enumerate((eb_g, eb_u)):
            bv = bsrc.rearrange("e (fc p) -> (e fc) p", p=128)
            for half in range(2):
                bl_, blk_ = bld.next()
                DMA("sp", bl_[:, :], bv[half * 128:(half + 1) * 128, :], w=[blk_])
                tp, tpk = ptp.next()
                TR(tp[:, 0:128], bl_[:, :], identf, r=[blk_, "cstf"], w=[tpk])
                CP("act", bcol[:, gi, half * 128:(half + 1) * 128], tp[:, 0:128], r=[tpk], w=["bcol"])
        CE = ("act", "dve", "act", "dve", "pool", "act", "dve", "act", "dve", "pool", "act", "dve")

        def expert_w(e_):
            wg, wgk = wgp.next(); wu, wuk = wup.next(); wd, wdk = wdp.next()

            def gen():
                n = 0
                for (dst, dkey, src) in ((wg, wgk, ew_g[e_]), (wu, wuk, ew_u[e_]), (wd, wdk, ew_d[e_])):
                    for kc0 in range(0, 8, 2):
                        st, sk = wst["pool"].next()
                        stv = st[:, :, :].rearrange("p a b -> p (a b)").rearrange("p (k n) -> p k n", k=2)
                        DMA("sp", stv, src[kc0 * 128:(kc0 + 2) * 128, :].rearrange("(kc p) n -> p kc n", p=128), w=[sk])
                        CP(CE[n % len(CE)], dst[:, kc0:kc0 + 2, :], stv, r=[sk], w=[dkey])
                        n += 1
                        yield
            return (wg, wgk, wu, wuk, wd, wdk), gen()

        wcur, g0 = expert_w(0)
        for _ in g0:
            pass

        def x_transposes(e_):
            xT, xTk = xTp.next()
            for st_ in range(3):
                xg, xgk = xgp.next()
                r0 = e_ * CAP + st_ * 128
                DMA("sp", xg[:, :], XG_d[r0:r0 + 128, :], r=["XG_d"], w=[xgk])
                tp, tpk = ptp.next()
                tpb = tp[:, :].bitcast(BF16)
                for kc in range(8):
                    TR(tpb[:, kc * 128:(kc + 1) * 128], xg[:, kc * 128:(kc + 1) * 128], identb, r=[xgk] + CK, w=[tpk])
                CP("dve", xT[:, :, st_ * 128:(st_ + 1) * 128], tpb[:, 0:1024].rearrange("p (k t) -> p k t", k=8),
                   r=[tpk], w=[xTk])
            return xT, xTk

        def gu(e_, fc, wg, wgk, wu, wuk, xT, xTk, aT, aTk):
            fcs = slice(fc * 128, (fc + 1) * 128)
            pgt, pgtk = pgu.next(); put, putk = pgu.next()
            for kc in range(8):
                MM(pgt[:, 0:CAP], wg[:, kc, fcs], xT[:, kc, :], start=(kc == 0), stop=(kc == 7), r=[wgk, xTk], w=[pgtk])
            for kc in range(8):
                MM(put[:, 0:CAP], wu[:, kc, fcs], xT[:, kc, :], start=(kc == 0), stop=(kc == 7), r=[wuk, xTk], w=[putk])
            bi = e_ * 8 + fc
            g_, gk_ = gr.next(); u_, uk_ = ur.next(); s_, sk_ = sr7.next()
            TS_("dve", g_[:, :], pgt[:, 0:CAP], bcol[:, 0, bi:bi + 1], 7.0, ALU.add, ALU.min, r=[pgtk, "bcol"], w=[gk_])
            TS_("dve", u_[:, :], put[:, 0:CAP], bcol[:, 1, bi:bi + 1], 7.0, ALU.add, ALU.min, r=[putk, "bcol"], w=[uk_])
            TS_("dve", u_[:, :], u_[:, :], -7.0, 1.0, ALU.max, ALU.add, r=[uk_], w=[uk_])
            ACT(s_[:, :], g_[:, :], AF.Sigmoid, r=[gk_], w=[sk_], scale=1.702)
            TT("dve", g_[:, :], g_[:, :], s_[:, :], ALU.mult, r=[gk_, sk_], w=[gk_])
            TT("dve", aT[:, fc, :], g_[:, :], u_[:, :], ALU.mult, r=[gk_, uk_], w=[aTk])

        def down(e_, aT, aTk, wd, wdk, bb_, bbk, gnext):
            for st_ in range(3):
                y_, yk_ = yr.next()
                for half in range(2):
                    pyt, pytk = py.next()
                    for fc in range(8):
                        MM(pyt[:, :], aT[:, fc, st_ * 128:(st_ + 1) * 128], wd[:, fc, half * 512:(half + 1) * 512],
                           start=(fc == 0), stop=False, r=[aTk, wdk], w=[pytk])
                    MM(pyt[:, :], ONEb[0:1, :], bb_[0:1, half * 512:(half + 1) * 512], start=False, stop=True,
                       r=CK + [bbk], w=[pytk])
                    CP("act", y_[:, half * 512:(half + 1) * 512], pyt[:, :], r=[pytk], w=[yk_])
                r0 = e_ * CAP + st_ * 128
                DMA(STORE_Q, YG_d[r0:r0 + 128, :], y_[:, :], r=[yk_], w=["YG_d"])
                next(gnext, None)
                next(gnext, None)

        xcur = x_transposes(0)
        prev = None
        for e_ in range(P7_E):
            wg, wgk, wu, wuk, wd, wdk = wcur
            if e_ + 1 < P7_E:
                wnext, gnext = expert_w(e_ + 1)
            else:
                wnext, gnext = None, iter(())
            bf_, bfk = bdf.next(); bb_, bbk = bdb.next()
            DMA("sp", bf_[:, :], eb_d[e_:e_ + 1, :], w=[bfk])
            CP("dve", bb_[:, :], bf_[:, :], r=[bfk], w=[bbk])
            xT, xTk = xcur
            aT, aTk = aTp.next()
            for fc in range(2):
                gu(e_, fc, wg, wgk, wu, wuk, xT, xTk, aT, aTk)
                next(gnext, None)
            if prev is not None:
                down(*prev, gnext)
            if e_ + 1 < P7_E:
                xcur = x_transposes(e_ + 1)
            for fc in range(2, 8):
                gu(e_, fc, wg, wgk, wu, wuk, xT, xTk, aT, aTk)
                next(gnext, None)
            for _ in gnext:
                pass
            prev = (e_, aT, aTk, wd, wdk, bb_, bbk)
            wcur = wnext
        down(*prev, iter(()))
        if stop == 7:
            S.waitall("sp", ["YG_d"])
        S.flush()
    if stop == 7:
        return nc

    with ExitStack() as es:
        g2t = bcast_load(es, "g2t", ln2_g, D)
        b2t = bcast_load(es, "b2t", ln2_b, D)
        h1r = Pool_(es, "h1r8", 2, [128, D], F32)
        ykr = Pool_(es, "ykr8", 4, [128, D], F32)
        accr = Pool_(es, "accr8", 2, [128, D], F32)
        outr = Pool_(es, "outr8", 2, [128, D], F32)
        small = Pool_(es, "small8", 2, [128, 16], F32)
        outk = []
        for ti in range(P8_T):
            h1, h1k = h1r.next()
            DMA("sp", h1[:, :], H1_d[ti * 128:(ti + 1) * 128, :], r=["H1_d"], w=[h1k])
            acc, acck = accr.next()
            TS_("dve", acc[:, :], h1[:, :], DN_ALPHA, None, ALU.mult, r=[h1k], w=[acck])
            for k in range(4):
                yk, ykk = ykr.next()
                S.add("pool", lambda e, yk=yk, ti=ti, k=k: e.indirect_dma_start(
                    out=yk[:, :], out_offset=None, in_=YG_d[:, :],
                    in_offset=bass.IndirectOffsetOnAxis(ap=IDX[:, ti, k:k + 1], axis=0)),
                    r=["YG_d", "IDX"], w=[ykk], dma=True)
                STT(acc[:, :], yk[:, :], GATE[:, ti, k:k + 1], acc[:, :], ALU.mult, ALU.add, r=[ykk, "GATE", acck], w=[acck])
            o_, ok_ = outr.next()
            layernorm(es, "ln2", acc, acck, g2t, "g2t", b2t, "b2t", o_, ok_, small)
            kk = "out%d" % ti
            DMA("sp", out[ti * 128:(ti + 1) * 128, :], o_[:, :], r=[ok_], w=[kk])
            outk.append(kk)
        S.waitall("sp", outk)
        S.flush()
    es0.close()
    return nc


def _consts():
    c = np.zeros((128, 8, 128), np.float32)
    idx = np.arange(128)
    same = np.ones((128, 128), bool)
    c[:, 0, :] = np.eye(128, dtype=np.float32)
    c[:, 1, :] = (same & (idx[:, None] <= idx[None, :])).astype(np.float32)
    c[:, 2, :] = (same & (idx[:, None] >= idx[None, :])).astype(np.float32)
    c[:, 3, :] = (same & (idx[:, None] > idx[None, :])).astype(np.float32)
    c[:, 4, :] = (same & (idx[:, None] < idx[None, :])).astype(np.float32)
    c[:, 5, :] = (idx[:, None] < idx[None, :]).astype(np.float32)
    c[:, 6, :] = 1.0
    c[:, 7, 0] = (idx < 64).astype(np.float32)
    c[:, 7, 1] = (idx >= 64).astype(np.float32)
    c2 = np.tile((np.arange(32, dtype=np.float32) * CAP)[None, :], (128, 1)).astype(np.float32)
    return c, c2


_NC_CACHE = {}


def kernel(**inp):
    x = np.asarray(inp["x"], np.float32)
    if "nc" not in _NC_CACHE:
        _NC_CACHE["nc"] = build()
    nc = _NC_CACHE["nc"]
    cst, cst2 = _consts()
    pos = np.arange(8192, dtype=np.float32)
    inv_freq = np.power(np.float32(500000.0), -np.arange(0, 16, 2, dtype=np.float32) / np.float32(16)).astype(np.float32)
    ang = (pos[:, None] * inv_freq[None, :]).astype(np.float32)
    cs_full = np.concatenate([np.tile(np.cos(ang), (1, 8)), np.tile(np.sin(ang), (1, 8))], axis=1).astype(np.float32)

    def sq(name):
        a = np.asarray(inp[name], np.float32)
        return np.ascontiguousarray(a[0]) if a.shape[0] == 1 and name not in ("ln0_g", "ln0_b") else np.ascontiguousarray(a)

    shared = {
        "cst": cst, "cst2": cst2,
        "ln0_g": np.ascontiguousarray(inp["ln0_g"], np.float32), "ln0_b": np.ascontiguousarray(inp["ln0_b"], np.float32),
    }
    for nm in ("w_in", "gla_wf_fwd", "gla_bf_fwd", "gla_wf_bwd", "gla_bf_bwd", "gla_norm_g", "diff_lq1", "diff_lk1",
               "diff_lq2", "diff_lk2", "diff_norm_g", "w_br_gla", "w_br_diff", "w_out", "ln1_g", "ln1_b", "router_w",
               "router_b", "exp_w_gate", "exp_b_gate", "exp_w_up", "exp_b_up", "exp_w_down", "exp_b_down", "ln2_g", "ln2_b"):
        shared[nm] = np.ascontiguousarray(np.asarray(inp[nm], np.float32)[0])
    in_maps = []
    for c in range(8):
        b, qd = c // 4, c % 4
        own = np.arange(qd * 2048, (qd + 1) * 2048)
        oth = np.concatenate([np.arange(0, qd * 2048), np.arange((qd + 1) * 2048, 8192)])
        order = np.concatenate([oth, own])
        mk = np.zeros((8192, 2), np.float32)
        mk[:6144, 0] = (oth < qd * 2048).astype(np.float32)
        mk[:6144, 1] = (oth >= (qd + 1) * 2048).astype(np.float32)
        m = dict(shared)
        m["xa"] = np.ascontiguousarray(x[b][order])
        m["csa"] = np.ascontiguousarray(np.concatenate([cs_full[order], mk], axis=1))
        in_maps.append(m)
    res = run_bass_kernel_spmd(nc, in_maps, core_ids=list(range(8)))
    _NC_CACHE["last"] = res
    outp = np.zeros((2, 8192, 1024), np.float32)
    for c in range(8):
        b, qd = c // 4, c % 4
        outp[b, qd * 2048:(qd + 1) * 2048] = res.results[c]["out"]
    return outp
```

```python
import numpy as np
from contextlib import ExitStack
import concourse.bass as bass
import concourse.mybir as mybir
from concourse.bass_utils import run_bass_kernel_spmd

F32 = mybir.dt.float32
BF16 = mybir.dt.bfloat16
I32 = mybir.dt.int32
AF = mybir.ActivationFunctionType
ALU = mybir.AluOpType

D = 1024
NOWN = 2048
NOTH = 6144
NALL = 8192
CAP = 384
NE = 32
EPS = 1e-5
DN_ALPHA = 2.0 ** 0.25
LAM_INIT = 0.2
SAME_ENG_SYNC = True
P1_TILES = list(range(64))
P1_LEVEL = 9
OWN_N = 16
P4_H = 8
P4_QB = 4
P4_KT = 64
P5_TB = 4
P5_TT = 4
P7_E = 32
P8_T = 16
STORE_Q = 'pool'
NO_ROPE = False
DEBUG = False

C_QG, C_KG, C_VG, C_RG, C_LRF, C_LRB, C_QD, C_KD, C_VD, C_GG, C_GD = (
    0, 512, 1024, 2048, 3072, 3088, 3104, 4128, 5152, 6176, 7200)

ENG_ATTR = {"pe": "tensor", "act": "scalar", "dve": "vector", "pool": "gpsimd", "sp": "sync"}
NRING = 12


class Op:
    __slots__ = ("eng", "fn", "deps", "signal", "sem", "val", "dma", "real")


class Sched:
    def __init__(self, nc, es):
        self.nc = nc
        self.engs = ["pe", "act", "dve", "pool", "sp"]
        self.csem = {e: es.enter_context(nc.semaphore("c_" + e)) for e in ["pe", "act", "dve", "pool"]}
        self.ccnt = {e: 0 for e in self.csem}
        self.ring = {q: [es.enter_context(nc.semaphore("d_%s%d" % (q, i))) for i in range(NRING)]
                     for q in ["sp", "pool"]}
        self.rcnt = {q: 0 for q in self.ring}
        self.rlast = {q: [None] * NRING for q in self.ring}
        self.ops = {e: [] for e in self.engs}
        self.lw = {}
        self.rd = {}
        self.seen = {e: {} for e in self.engs}
        self.nops = 0

    def add(self, eng, fn, r=(), w=(), dma=False):
        op = Op()
        op.eng = eng; op.fn = fn; op.dma = dma; op.signal = False; op.deps = []
        op.sem = None; op.val = None; op.real = True
        deps = []
        for k in r:
            o = self.lw.get(k)
            if o is not None:
                deps.append(o)
        for k in w:
            o = self.lw.get(k)
            if o is not None:
                deps.append(o)
            deps.extend(self.rd.get(k, ()))
        if dma:
            q = eng
            i = self.rcnt[q]
            slot = i % NRING
            prev = self.rlast[q][slot]
            if prev is not None:
                deps.append(prev)
            op.sem = self.ring[q][slot]
            op.val = 16 * (i // NRING + 1)
            self.rcnt[q] = i + 1
            self.rlast[q][slot] = op
        sd = set()
        for d in deps:
            if id(d) in sd:
                continue
            sd.add(id(d))
            if (not d.dma) and (not dma) and d.eng == eng:
                if eng == "pe" or not SAME_ENG_SYNC:
                    continue
            if not d.dma:
                d.signal = True
            op.deps.append(d)
        for k in r:
            self.rd.setdefault(k, []).append(op)
        for k in w:
            self.lw[k] = op
            self.rd[k] = []
        self.ops[eng].append(op)
        self.nops += 1
        return op

    def waitall(self, eng, keys):
        op = Op()
        op.eng = eng; op.fn = (lambda e: None); op.dma = False; op.signal = False; op.sem = None; op.val = None
        op.deps = []
        op.real = False
        for k in keys:
            o = self.lw.get(k)
            if o is not None:
                if not o.dma:
                    o.signal = True
                op.deps.append(o)
        self.ops[eng].append(op)

    def barrier(self):
        lasts = []
        for eng in self.engs:
            real = [o for o in self.ops[eng] if not o.dma and o.deps is not None and getattr(o, "real", True)]
            if real:
                o = real[-1]
                o.signal = True
                lasts.append(o)
        dmas = []
        for q in self.ring:
            for o in self.rlast[q]:
                if o is not None:
                    dmas.append(o)
        for eng in self.engs:
            op = Op()
            op.eng = eng; op.fn = (lambda e: None); op.dma = False; op.signal = False; op.sem = None; op.val = None
            op.deps = [o for o in lasts if o.eng != eng] + dmas
            self.ops[eng].append(op)

    def flush(self):
        nc = self.nc
        self.barrier()
        for o in self.lw.values():
            if not o.dma and o.val is None:
                o.signal = True
        for lst in self.rd.values():
            for o in lst:
                if not o.dma and o.val is None:
                    o.signal = True
        for eng in self.engs:
            for op in self.ops[eng]:
                if not op.dma and op.signal:
                    self.ccnt[eng] += 1
                    op.val = self.ccnt[eng]
                    op.sem = self.csem[eng]
                elif not op.dma:
                    op.val = -1
        with nc.Block() as blk:
            for eng in self.engs:
                ops = self.ops[eng]
                if not ops:
                    continue

                def body(e, ops=ops, eng=eng):
                    seen = self.seen[eng]
                    for op in ops:
                        for d in op.deps:
                            assert d.val is not None and d.val > 0, "dep not signalled"
                            key = id(d.sem)
                            if seen.get(key, 0) < d.val:
                                e.wait_ge(d.sem, d.val)
                                seen[key] = d.val
                        ins = op.fn(e)
                        if ins is None:
                            continue
                        if op.dma:
                            ins.then_inc(op.sem, 16)
                        elif op.signal:
                            ins.then_inc(op.sem, 1)

                getattr(blk, ENG_ATTR[eng])(body)
        self.ops = {e: [] for e in self.engs}


def build(stop=99, debug=False):
    global DEBUG
    DEBUG = debug
    nc = bass.Bass("TRN2", target_bir_lowering=False)

    def din(name, shape, dt=F32):
        return nc.dram_tensor(name, list(shape), dt, kind="ExternalInput").ap()

    def dscr(name, shape, dt):
        kind = "ExternalOutput" if DEBUG else "Internal"
        return nc.dram_tensor(name, list(shape), dt, kind=kind).ap()

    xa = din("xa", [NALL, D])
    csa = din("csa", [NALL, 130])
    cst = din("cst", [128, 8, 128])
    cst2 = din("cst2", [128, 32])
    ln0_g = din("ln0_g", [D]); ln0_b = din("ln0_b", [D])
    w_in = din("w_in", [D, 8224])
    wf_f = din("gla_wf_fwd", [16, 512]); bf_f = din("gla_bf_fwd", [512])
    wf_b = din("gla_wf_bwd", [16, 512]); bf_b = din("gla_bf_bwd", [512])
    gla_g = din("gla_norm_g", [256])
    lq1 = din("diff_lq1", [64]); lk1 = din("diff_lk1", [64])
    lq2 = din("diff_lq2", [64]); lk2 = din("diff_lk2", [64])
    diff_g = din("diff_norm_g", [128])
    w_bra = din("w_br_gla", [D, D]); w_brb = din("w_br_diff", [D, D]); w_out = din("w_out", [D, D])
    ln1_g = din("ln1_g", [D]); ln1_b = din("ln1_b", [D])
    router_w = din("router_w", [D, NE]); router_b = din("router_b", [NE])
    ew_g = din("exp_w_gate", [NE, D, D]); eb_g = din("exp_b_gate", [NE, D])
    ew_u = din("exp_w_up", [NE, D, D]); eb_u = din("exp_b_up", [NE, D])
    ew_d = din("exp_w_down", [NE, D, D]); eb_d = din("exp_b_down", [NE, D])
    ln2_g = din("ln2_g", [D]); ln2_b = din("ln2_b", [D])
    out = nc.dram_tensor("out", [NOWN, D], F32, kind="ExternalOutput").ap()

    KT_d = dscr("KT_d", [8, 128, NALL], BF16)
    V_d = dscr("V_d", [NALL, D], BF16)
    QT_d = dscr("QT_d", [8, 128, NOWN], BF16)
    H_d = dscr("H_d", [NOWN, D], F32)
    HT_d = dscr("HT_d", [D, NOWN], BF16)
    OF_d = dscr("OF_d", [NOWN, D], F32)
    AT_d = dscr("AT_d", [D, NOWN], BF16)
    BT_d = dscr("BT_d", [D, NOWN], BF16)
    H1_d = dscr("H1_d", [NOWN, D], F32)
    XG_d = dscr("XG_d", [NE * CAP, D], BF16)
    YG_d = dscr("YG_d", [NE * CAP, D], F32)

    es0 = ExitStack()
    S = Sched(nc, es0)

    def MM(out_, lhsT, rhs, start=True, stop=True, r=(), w=()):
        return S.add("pe", lambda e: e.matmul(out_, lhsT, rhs, start=start, stop=stop, skip_group_check=True), r, w)

    def TR(out_, in_, ident, r=(), w=()):
        return S.add("pe", lambda e: e.transpose(out_, in_, ident), r, w)

    def ACT(out_, in_, func, r=(), w=(), bias=0.0, scale=1.0, accum_out=None):
        if accum_out is None:
            return S.add("act", lambda e: e.activation(out_, in_, func, bias=bias, scale=scale), r, w)
        return S.add("act", lambda e: e.activation(out_, in_, func, bias=bias, scale=scale, accum_out=accum_out), r, w)

    def TS_(eng, out_, in0, s1, s2, op0, op1=None, r=(), w=(), accum_out=None):
        if op1 is None:
            return S.add(eng, lambda e: e.tensor_scalar(out_, in0, s1, None, op0), r, w)
        if accum_out is not None:
            return S.add(eng, lambda e: e.tensor_scalar(out_, in0, s1, s2, op0, op1, accum_out), r, w)
        return S.add(eng, lambda e: e.tensor_scalar(out_, in0, s1, s2, op0, op1), r, w)

    def TT(eng, out_, in0, in1, op, r=(), w=()):
        return S.add(eng, lambda e: e.tensor_tensor(out_, in0, in1, op), r, w)

    def STT(out_, in0, scalar, in1, op0, op1, r=(), w=(), accum_out=None):
        if accum_out is None:
            return S.add("dve", lambda e: e.scalar_tensor_tensor(out_, in0, scalar, in1, op0, op1), r, w)
        return S.add("dve", lambda e: e.scalar_tensor_tensor(out_, in0, scalar, in1, op0, op1, accum_out), r, w)

    def CP(eng, out_, in_, r=(), w=()):
        if eng == "act":
            return S.add("act", lambda e: e.activation(out_, in_, AF.Copy), r, w)
        return S.add(eng, lambda e: e.tensor_copy(out_, in_), r, w)

    def MEMSET(eng, ap, val, w=()):
        return S.add(eng, lambda e: e.memset(ap, val), (), w)

    def DMA(q, out_, in_, r=(), w=(), slow=False):
        if slow:
            return S.add(q, lambda e: e.dma_start(out=out_, in_=in_, allow_slow_non_contiguous=True), r, w, dma=True)
        return S.add(q, lambda e: e.dma_start(out=out_, in_=in_), r, w, dma=True)

    uid = [0]

    class Pool_:
        def __init__(self, es, name, n, shape, dt, psum=False):
            self.t = []
            uid[0] += 1
            for i in range(n):
                nm = "%s_%d_u%d" % (name, i, uid[0])
                if psum:
                    self.t.append((es.enter_context(nc.psum_tensor(nm, shape, dt)), nm))
                else:
                    self.t.append((es.enter_context(nc.sbuf_tensor(nm, shape, dt)), nm))
            self.i = 0

        def next(self):
            t = self.t[self.i % len(self.t)]
            self.i += 1
            return t

    def sb(es, name, shape, dt):
        uid[0] += 1
        return es.enter_context(nc.sbuf_tensor("%s_u%d" % (name, uid[0]), shape, dt))

    cstf = sb(es0, "cstf", [128, 8, 128], F32)
    cstb = sb(es0, "cstb", [128, 8, 128], BF16)
    ioe = sb(es0, "ioe", [128, 32], F32)
    Sst = sb(es0, "Sst", [128, 2, 4, 256], F32)
    Pb = sb(es0, "Pb", [128, 4], F32)
    IDX = sb(es0, "IDX", [128, 16, 4], I32)
    GATE = sb(es0, "GATE", [128, 16, 4], F32)
    DMA("sp", cstf[:, :, :], cst, w=["cstf"])
    DMA("sp", ioe[:, :], cst2, w=["ioe"])
    CP("dve", cstb[:, :, :], cstf[:, :, :], r=["cstf"], w=["cstb"])
    MEMSET("dve", Sst[:, :, :, :], 0.0, w=["Sf0", "Sf1", "Sf2", "Sf3", "Sb0", "Sb1", "Sb2", "Sb3"])
    MEMSET("dve", Pb[:, :], 1.0, w=["Pb"])
    identb = cstb[:, 0, :]
    identf = cstf[:, 0, :]
    TIb, TItb, TSb, TPb, LTb, ONEb = (cstb[:, j, :] for j in range(1, 7))
    CIb = cstb[:, 7, 0:2]
    CK = ["cstb"]

    stg_es = ExitStack()
    wst = {"pool": None, "n": 128}

    def new_stage(es, n, cols):
        wst["pool"] = Pool_(es, "wstage", n, [128, 8, cols], F32)
        wst["n"] = cols
    cast_rr = [0]
    es_ht = ExitStack()
    HT_own = sb(es_ht, "HT_own", [128, 8, NOWN], BF16)
    Wkv = sb(es_ht, "Wkv", [128, 8, 1536], BF16)

    def load_w(dst, dkey, wd, c0, ncols, dcol0=0, engs=("pool", "act")):
        PW = wst["n"]
        for p0 in range(0, ncols, PW):
            n = min(PW, ncols - p0)
            st, sk = wst["pool"].next()
            DMA("sp", st[:, :, 0:n], wd[:, c0 + p0:c0 + p0 + n].rearrange("(kc p) n -> p kc n", p=128), w=[sk])
            eng = engs[cast_rr[0] % len(engs)]
            cast_rr[0] += 1
            CP(eng, dst[:, :, dcol0 + p0:dcol0 + p0 + n], st[:, :, 0:n], r=[sk], w=[dkey])

    def load_w_rows(dst, dkey, wd, engs):
        for kc0 in range(0, 8, 2):
            st, sk = wst["pool"].next()
            stv = st[:, :, :].rearrange("p a b -> p (a b)").rearrange("p (k n) -> p k n", k=2)
            DMA("sp", stv, wd[kc0 * 128:(kc0 + 2) * 128, :].rearrange("(kc p) n -> p kc n", p=128), w=[sk])
            eng = engs[cast_rr[0] % len(engs)]
            cast_rr[0] += 1
            CP(eng, dst[:, kc0:kc0 + 2, :], stv, r=[sk], w=[dkey])

    def bcast_load(es, name, vec, n):
        t = sb(es, name, [128, n], F32)
        DMA("sp", t[:, :], vec.partition_broadcast(128), w=[name])
        return t

    def layernorm(es_tmp, pref, x_t, xkey, g_t, gkey, b_t, bkey, out_t, okey, small):
        st, stk = small.next()
        S.add("dve", lambda e: e.bn_stats(st[:, 0:6], x_t[:, 0:512]), [xkey], [stk])
        S.add("dve", lambda e: e.bn_stats(st[:, 6:12], x_t[:, 512:1024]), [xkey], [stk])
        S.add("dve", lambda e: e.bn_aggr(st[:, 12:14], st[:, 0:12]), [stk], [stk])
        ACT(st[:, 14:15], st[:, 13:14], AF.Ln, r=[stk], w=[stk], bias=EPS)
        ACT(st[:, 15:16], st[:, 14:15], AF.Exp, r=[stk], w=[stk], scale=-0.5)
        TS_("dve", out_t[:, :], x_t[:, :], st[:, 12:13], st[:, 15:16], ALU.subtract, ALU.mult, r=[xkey, stk], w=[okey])
        TT("dve", out_t[:, :], out_t[:, :], g_t[:, :], ALU.mult, r=[okey, gkey], w=[okey])
        TT("dve", out_t[:, :], out_t[:, :], b_t[:, :], ALU.add, r=[okey, bkey], w=[okey])

    NW1 = 3072
    with ExitStack() as es:
        new_stage(es, 2, 128)
        W1 = sb(es, "W1", [128, 8, NW1], BF16)
        Wlr = sb(es, "Wlr", [128, 8, 32], BF16)
        wfs = sb(es, "wfs", [17, 2, 512], F32)
        wfa = sb(es, "wfa", [17, 2, 512], BF16)
        Lt = sb(es, "Lt", [32, 2, 128], BF16)
        g0t = bcast_load(es, "g0t", ln0_g, D)
        b0t = bcast_load(es, "b0t", ln0_b, D)
        load_w(W1, "W1", w_in, C_KD, 1024, 0)
        load_w(W1, "W1", w_in, C_VD, 1024, 1024)
        load_w(W1, "W1", w_in, C_QD, 1024, 2048)
        load_w(Wkv, "Wkv", w_in, C_KG, 512, 0)
        load_w(Wkv, "Wkv", w_in, C_VG, 1024, 512)
        load_w(Wlr, "Wlr", w_in, C_LRF, 32, 0)
        DMA("sp", wfs[0:16, 0, :], wf_f, w=["wfs"])
        DMA("sp", wfs[16:17, 0, :], bf_f.rearrange("(a n) -> a n", a=1), w=["wfs"])
        DMA("sp", wfs[0:16, 1, :], wf_b, w=["wfs"])
        DMA("sp", wfs[16:17, 1, :], bf_b.rearrange("(a n) -> a n", a=1), w=["wfs"])
        CP("dve", wfa[:, :, :], wfs[:, :, :], r=["wfs"], w=["wfa"])
        MEMSET("dve", Lt[:, :, :], 1.0, w=["Lt0", "Lt1"])

        xr = Pool_(es, "xr", 2, [128, D], F32)
        csr = Pool_(es, "csr", 2, [128, 130], F32)
        small = Pool_(es, "small", 3, [128, 16], F32)
        hfr = Pool_(es, "hfr", 2, [128, D], F32)
        hbr = Pool_(es, "hbr", 2, [128, D], BF16)
        hTr = Pool_(es, "hTr", 2, [128, 8, 128], BF16)
        kbr = Pool_(es, "kbr", 2, [128, D], BF16)
        ktr = Pool_(es, "ktr", 2, [128, 8, 128], BF16)
        vbr = Pool_(es, "vbr", 2, [128, D], BF16)
        ropet = Pool_(es, "ropet", 2, [128, 4, 8, 8], F32)
        kgr = Pool_(es, "kgr", 2, [128, 512], BF16)
        vgr = Pool_(es, "vgr", 2, [128, D], BF16)
        f32r = Pool_(es, "f32r", 4, [128, 512], F32)
        lfr = Pool_(es, "lfr", 4, [128, 512], BF16)
        kstr = Pool_(es, "kstr", 4, [128, 512], BF16)
        decr = Pool_(es, "decr", 2, [128, 16], F32)
        nmr = Pool_(es, "nmr", 2, [128, 2], F32)
        ptp = Pool_(es, "ptp", 1, [128, 512], F32, psum=True)
        ppj = Pool_(es, "ppj", 2, [128, 512], F32, psum=True)
        pg = Pool_(es, "pg", 2, [128, 512], F32, psum=True)
        pu = Pool_(es, "pu", 2, [128, 512], F32, psum=True)
        psm = Pool_(es, "psm", 1, [128, 512], F32, psum=True)
        uslot = [0]

        def rope_and_store(pj, pjk, kb, kbk, cs, csk, half):
            dst = kb[:, half * 512:(half + 1) * 512]
            CP("act", dst, pj[:, :], r=[pjk], w=[kbk])
            if NO_ROPE:
                return
            d3 = dst.rearrange("p (g d) -> p g d", d=64)
            x1 = d3[:, :, 0:8]; x2 = d3[:, :, 8:16]
            cosb = cs[:, 0:64].rearrange("p (g d) -> p g d", d=8)
            sinb = cs[:, 64:128].rearrange("p (g d) -> p g d", d=8)
            rt, rk = ropet.next()
            TT("dve", rt[:, 0], x1, cosb, ALU.mult, r=[kbk, csk], w=[rk])
            TT("dve", rt[:, 1], x2, sinb, ALU.mult, r=[kbk, csk], w=[rk])
            TT("dve", rt[:, 2], x2, cosb, ALU.mult, r=[kbk, csk], w=[rk])
            TT("dve", rt[:, 3], x1, sinb, ALU.mult, r=[kbk, csk], w=[rk])
            TT("dve", d3[:, :, 0:8], rt[:, 0], rt[:, 1], ALU.subtract, r=[rk], w=[kbk])
            TT("dve", d3[:, :, 8:16], rt[:, 2], rt[:, 3], ALU.add, r=[rk], w=[kbk])

        pend = [iter(())]

        def proj_tok(hT, hTk, W, wkey, c0, pj, pjk):
            for kc in range(8):
                MM(pj[:, :], hT[:, kc, :], W[:, kc, c0:c0 + 512], start=(kc == 0), stop=(kc == 7),
                   r=[hTk, wkey], w=[pjk])
            next(pend[0], None)
            next(pend[0], None)

        def p1A(i):
                own = i >= 48
                xt, xk = xr.next()
                cs, csk = csr.next()
                DMA("sp", xt[:, :], xa[i * 128:(i + 1) * 128, :], w=[xk])
                DMA("sp", cs[:, :], csa[i * 128:(i + 1) * 128, :], w=[csk])
                if P1_LEVEL < 2:
                    return None
                hf, hfk = hfr.next()
                layernorm(es, "ln0", xt, xk, g0t, "g0t", b0t, "b0t", hf, hfk, small)
                hb, hbk = hbr.next()
                CP("act", hb[:, :], hf[:, :], r=[hfk], w=[hbk])
                if own:
                    DMA(STORE_Q, H_d[(i - 48) * 128:(i - 47) * 128, :], hf[:, :], r=[hfk], w=["H_d"])
                tp, tpk = ptp.next()
                tpb = tp[:, :].bitcast(BF16)
                for kc in range(8):
                    TR(tpb[:, kc * 128:(kc + 1) * 128], hb[:, kc * 128:(kc + 1) * 128], identb, r=[hbk] + CK, w=[tpk])
                hT, hTk = hTr.next()
                CP("act", hT[:, :, :], tpb[:, 0:1024].rearrange("p (k t) -> p k t", k=8), r=[tpk], w=[hTk])
                if own:
                    CP("pool", HT_own[:, :, (i - 48) * 128:(i - 47) * 128], hT[:, :, :], r=[hTk], w=["HT_own"])
                return dict(own=own, cs=cs, csk=csk, hT=hT, hTk=hTk)

        def p1B(i, c):
                if c is None:
                    return
                own = c["own"]; cs = c["cs"]; csk = c["csk"]; hT = c["hT"]; hTk = c["hTk"]
                if P1_LEVEL < 3:
                    return
                gla = (not own) and P1_LEVEL >= 4
                if gla:
                    kg, kgk = kgr.next()
                    pj, pjk = ppj.next()
                    proj_tok(hT, hTk, Wkv, "Wkv", 0, pj, pjk)
                    CP("act", kg[:, :], pj[:, :], r=[pjk], w=[kgk])
                    nm, nmk = nmr.next()
                    TS_("dve", nm[:, :], cs[:, 128:130], -1.0 / 16.0, None, ALU.mult, r=[csk], w=[nmk])
                    sm, smk = psm.next()
                    for d in range(2):
                        for kc in range(8):
                            MM(sm[0:16, d * 128:(d + 1) * 128], Wlr[:, kc, d * 16:(d + 1) * 16], hT[:, kc, :],
                               start=(kc == 0), stop=(kc == 7), r=["Wlr", hTk], w=[smk])
                    for d in range(2):
                        CP("act", Lt[0:16, d, :], sm[0:16, d * 128:(d + 1) * 128], r=[smk], w=["Lt%d" % d])
                    e1s = []
                    for d in range(2):
                        g1, g1k = pg.next()
                        MM(g1[:, :], Lt[0:17, d, :], wfa[0:17, d, :], r=["Lt%d" % d, "wfa"], w=[g1k])
                        e1, e1k = f32r.next()
                        ACT(e1[:, :], g1[:, :], AF.Exp, r=[g1k], w=[e1k], scale=-1.0)
                        e1s.append((e1, e1k))
                    lfs = []
                    for d in range(2):
                        e1, e1k = e1s[d]
                        ACT(e1[:, :], e1[:, :], AF.Ln, r=[e1k], w=[e1k], bias=1.0)
                        lf, lfk = lfr.next()
                        TS_("dve", lf[:, :], e1[:, :], nm[:, d:d + 1], None, ALU.mult, r=[e1k, nmk], w=[lfk])
                        lfs.append((lf, lfk))
                    vg, vgk = vgr.next()
                    for half in range(2):
                        pj, pjk = ppj.next()
                        proj_tok(hT, hTk, Wkv, "Wkv", 512 + half * 512, pj, pjk)
                        CP("act", vg[:, half * 512:(half + 1) * 512], pj[:, :], r=[pjk], w=[vgk])
                kb, kbk = kbr.next()
                for half in range(2):
                    pj, pjk = ppj.next()
                    proj_tok(hT, hTk, W1, "W1", half * 512, pj, pjk)
                    rope_and_store(pj, pjk, kb, kbk, cs, csk, half)
                if gla:
                    ksts = []
                    e2s = []
                    for d in range(2):
                        lf, lfk = lfs[d]
                        g2, g2k = pg.next()
                        MM(g2[:, :], (TSb if d == 0 else TPb), lf[:, :], r=CK + [lfk], w=[g2k])
                        e2, e2k = f32r.next()
                        ACT(e2[:, :], g2[:, :], AF.Exp, r=[g2k], w=[e2k])
                        e2s.append((e2, e2k))
                    for d in range(2):
                        e2, e2k = e2s[d]
                        kst, kstk = kstr.next()
                        STT(kst[:, :], e2[:, :], cs[:, 128 + d:129 + d], kg[:, :], ALU.mult, ALU.mult, r=[e2k, csk, kgk], w=[kstk])
                        ksts.append((kst, kstk))
                tp, tpk = ptp.next()
                tpb = tp[:, :].bitcast(BF16)
                for h in range(8):
                    TR(tpb[:, h * 128:(h + 1) * 128], kb[:, h * 128:(h + 1) * 128], identb, r=[kbk] + CK, w=[tpk])
                kt, ktk = ktr.next()
                CP("dve", kt[:, :, :], tpb[:, 0:1024].rearrange("p (k t) -> p k t", k=8), r=[tpk], w=[ktk])
                DMA(STORE_Q, KT_d[:, :, i * 128:(i + 1) * 128].rearrange("h p t -> p h t"), kt[:, :, :], r=[ktk], w=["KT_d"])
                vb, vbk = vbr.next()
                for half in range(2):
                    pj, pjk = ppj.next()
                    proj_tok(hT, hTk, W1, "W1", 1024 + half * 512, pj, pjk)
                    CP("act", vb[:, half * 512:(half + 1) * 512], pj[:, :], r=[pjk], w=[vbk])
                DMA(STORE_Q, V_d[i * 128:(i + 1) * 128, :], vb[:, :], r=[vbk], w=["V_d"])
                if own:
                    kb, kbk = kbr.next()
                    for half in range(2):
                        pj, pjk = ppj.next()
                        proj_tok(hT, hTk, W1, "W1", 2048 + half * 512, pj, pjk)
                        rope_and_store(pj, pjk, kb, kbk, cs, csk, half)
                    tp, tpk = ptp.next()
                    tpb = tp[:, :].bitcast(BF16)
                    for h in range(8):
                        TR(tpb[:, h * 128:(h + 1) * 128], kb[:, h * 128:(h + 1) * 128], identb, r=[kbk] + CK, w=[tpk])
                    kt, ktk = ktr.next()
                    CP("dve", kt[:, :, :], tpb[:, 0:1024].rearrange("p (k t) -> p k t", k=8), r=[tpk], w=[ktk])
                    DMA(STORE_Q, QT_d[:, :, (i - 48) * 128:(i - 47) * 128].rearrange("h p t -> p h t"), kt[:, :, :],
                        r=[ktk], w=["QT_d"])
                    return
                if not gla:
                    return
                for d in range(2):
                    lf, lfk = lfs[d]
                    for h in range(4):
                        col = 256 + d * 4 + h
                        MM(sm[:, col:col + 1], lf[:, h * 128:(h + 1) * 128], ONEb[:, 0:1], r=[lfk] + CK, w=[smk])
                dec, deck = decr.next()
                ACT(dec[:, 0:8], sm[:, 256:264], AF.Exp, r=[smk], w=[deck])

                def cgen(ksts=ksts, vg=vg, vgk=vgk, dec=dec, deck=deck):
                    for d in range(2):
                        kst, kstk = ksts[d]
                        for h in range(4):
                            ub, ubk = pu.next()
                            uap = ub[:, 0:256]
                            MM(uap, kst[:, h * 128:(h + 1) * 128], vg[:, h * 256:(h + 1) * 256], r=[kstk, vgk], w=[ubk])
                            if d == 0:
                                sk_ = "Sf%d" % h
                                STT(Sst[:, 0, h, :], Sst[:, 0, h, :], dec[:, h:h + 1], uap, ALU.mult, ALU.add,
                                    r=[sk_, deck, ubk], w=[sk_])
                            else:
                                sk_ = "Sb%d" % h
                                STT(Sst[:, 1, h, :], uap, Pb[:, h:h + 1], Sst[:, 1, h, :], ALU.mult, ALU.add,
                                    r=[sk_, "Pb", ubk], w=[sk_])
                            yield
                    TT("dve", Pb[:, :], Pb[:, :], dec[:, 4:8], ALU.mult, r=["Pb", deck], w=["Pb"])
                    yield
                for _ in pend[0]:
                    pass
                pend[0] = cgen()

        p1tiles = (P1_TILES if P1_LEVEL >= 1 else [])
        ctx = p1A(p1tiles[0]) if p1tiles else None
        for n_, i in enumerate(p1tiles):
            nxt = p1A(p1tiles[n_ + 1]) if n_ + 1 < len(p1tiles) else None
            p1B(i, ctx)
            ctx = nxt
        for _ in pend[0]:
            pass
        if stop == 1:
            DMA("sp", out[0:128, 0:1024].rearrange("p (a b c) -> p a b c", a=2, b=4), Sst[:, :, :, 0:128], r=["Sf0", "Sf1", "Sf2", "Sf3", "Sb0", "Sb1", "Sb2", "Sb3"], w=["dbg"])
            S.waitall("sp", ["dbg", "KT_d", "V_d", "QT_d", "H_d"])
        S.flush()
    if stop == 1:
        return nc

    NW2 = 2048
    with ExitStack() as es:
        for d in range(2):
            if d == 0:
                new_stage(es, 2, 128)
                Wq = sb(es, "Wq", [128, 8, 512], BF16)
                load_w(Wq, "Wq", w_in, C_QG, 512, 0)
                Wrg = sb(es, "Wrg", [128, 8, 1024], BF16)
                load_w(Wrg, "Wrg", w_in, C_RG, 1024, 0, engs=("pool",))
                ggt = bcast_load(es, "ggt", gla_g, 256)
            Wl2 = sb(es, "Wl2_%d" % d, [128, 8, 16], BF16)
            wfs2 = sb(es, "wfs2_%d" % d, [17, 512], F32)
            wfa2 = sb(es, "wfa2_%d" % d, [17, 512], BF16)
            Lt2 = sb(es, "Lt2_%d" % d, [32, 128], BF16)
            mask4 = sb(es, "mask4_%d" % d, [128, 4, 128], F32)
            load_w(Wl2, "Wl2", w_in, C_LRF if d == 0 else C_LRB, 16, 0)
            DMA("sp", wfs2[0:16, :], wf_f if d == 0 else wf_b, w=["wfs2"])
            DMA("sp", wfs2[16:17, :], (bf_f if d == 0 else bf_b).rearrange("(a n) -> a n", a=1), w=["wfs2"])
            CP("dve", wfa2[:, :], wfs2[:, :], r=["wfs2"], w=["wfa2"])
            MEMSET("dve", Lt2[:, :], 1.0, w=["Lt2"])
            for h in range(4):
                CP("dve", mask4[:, h, :], cstf[:, 1 if d == 0 else 2, :], r=["cstf"], w=["mask4"])
            if d == 0:
                ofr = Pool_(es, "ofr", 2, [128, D], F32)
                osr = Pool_(es, "osr", 2, [128, D], F32)
                abr = Pool_(es, "abr", 2, [128, D], BF16)
                atr = Pool_(es, "atr", 2, [128, 8, 128], BF16)
                srr = Pool_(es, "srr", 2, [128, 512], F32)
                small = Pool_(es, "small3", 2, [128, 16], F32)
                kgr = Pool_(es, "kgr2", 2, [128, 512], BF16)
                vgr = Pool_(es, "vgr2", 2, [128, D], BF16)
                f32r = Pool_(es, "f32r2", 6, [128, 512], F32)
                lfr = Pool_(es, "lfr2", 2, [128, 512], BF16)
                kstr = Pool_(es, "kstr2", 2, [128, 512], BF16)
                eqr = Pool_(es, "eqr", 3, [128, 512], F32)
                qinr = Pool_(es, "qinr", 2, [128, 512], BF16)
                kinr = Pool_(es, "kinr", 2, [128, 512], BF16)
                attr = Pool_(es, "attr", 2, [128, 512], BF16)
                ppj = Pool_(es, "ppj2", 1, [128, 512], F32, psum=True)
                pg = Pool_(es, "pg2", 3, [128, 512], F32, psum=True)
                pu = Pool_(es, "pu2", 2, [128, 512], F32, psum=True)
                po = Pool_(es, "po2", 2, [128, 512], F32, psum=True)
            Sbf = [Pool_(es, "Sbf%d_%d" % (h, d), 2, [128, 256], BF16) for h in range(4)]
            uslot = [0]
            oslot = [0]
            skey = ["S%s%d" % ("f" if d == 0 else "b", h) for h in range(4)]
            cur = []
            for h in range(4):
                t, tk = Sbf[h].next()
                CP("pool", t[:, :], Sst[:, d, h, :], r=[skey[h]], w=[tk])
                cur.append((t, tk))
            tiles = list(range(OWN_N)) if d == 0 else list(range(OWN_N - 1, -1, -1))
            chunks = (0,)
            def stageA(i):
                    hT = HT_own[:, :, i * 128:(i + 1) * 128]
                    hTk = "HT_own"
                    kg, kgk = kgr.next()
                    pj, pjk = ppj.next()
                    for kc in range(8):
                        MM(pj[:, :], hT[:, kc, :], Wkv[:, kc, 0:512], start=(kc == 0), stop=(kc == 7), r=["Wkv", hTk], w=[pjk])
                    CP("act", kg[:, :], pj[:, :], r=[pjk], w=[kgk])
                    vg, vgk = vgr.next()
                    for half in range(2):
                        pj, pjk = ppj.next()
                        for kc in range(8):
                            MM(pj[:, :], hT[:, kc, :], Wkv[:, kc, 512 + half * 512:1024 + half * 512],
                               start=(kc == 0), stop=(kc == 7), r=["Wkv", hTk], w=[pjk])
                        CP("act", vg[:, half * 512:(half + 1) * 512], pj[:, :], r=[pjk], w=[vgk])
                    g1, g1k = pg.next()
                    for kc in range(8):
                        MM(g1[0:16, 0:128], Wl2[:, kc, 0:16], hT[:, kc, :], start=(kc == 0), stop=(kc == 7),
                           r=["Wl2", hTk], w=[g1k])
                    CP("act", Lt2[0:16, :], g1[0:16, 0:128], r=[g1k], w=["Lt2"])
                    MM(g1[:, :], Lt2[0:17, :], wfa2[0:17, :], r=["Lt2", "wfa2"], w=[g1k])
                    e1, e1k = f32r.next()
                    ACT(e1[:, :], g1[:, :], AF.Exp, r=[g1k], w=[e1k], scale=-1.0)
                    ACT(e1[:, :], e1[:, :], AF.Ln, r=[e1k], w=[e1k], bias=1.0)
                    lf, lfk = lfr.next()
                    TS_("dve", lf[:, :], e1[:, :], -1.0 / 16.0, None, ALU.mult, r=[e1k], w=[lfk])
                    gb, gbk = pg.next()
                    for h in range(4):
                        MM(gb[:, h * 128:(h + 1) * 128], lf[:, h * 128:(h + 1) * 128], (TIb if d == 0 else TItb),
                           r=[lfk] + CK, w=[gbk])
                    Eq, Eqk = eqr.next()
                    Ek, Ekk = f32r.next()
                    ACT(Eq[:, :], gb[:, :], AF.Exp, r=[gbk], w=[Eqk])
                    ACT(Ek[:, :], gb[:, :], AF.Exp, r=[gbk], w=[Ekk], scale=-1.0)
                    g2, g2k = pg.next()
                    MM(g2[:, :], (TSb if d == 0 else TPb), lf[:, :], r=CK + [lfk], w=[g2k])
                    e2, e2k = f32r.next()
                    ACT(e2[:, :], g2[:, :], AF.Exp, r=[g2k], w=[e2k])
                    kst, kstk = kstr.next()
                    TT("dve", kst[:, :], e2[:, :], kg[:, :], ALU.mult, r=[e2k, kgk], w=[kstk])
                    gq, gqk = pg.next()
                    gk, gkk = pg.next()
                    for h in range(4):
                        for kc in range(8):
                            MM(gq[:, h * 128:(h + 1) * 128], Wq[:, kc, h * 128:(h + 1) * 128], hT[:, kc, :],
                               start=(kc == 0), stop=(kc == 7), r=["Wq", hTk], w=[gqk])
                    for h in range(4):
                        for kc in range(8):
                            MM(gk[:, h * 128:(h + 1) * 128], Wkv[:, kc, h * 128:(h + 1) * 128], hT[:, kc, :],
                               start=(kc == 0), stop=(kc == 7), r=["Wkv", hTk], w=[gkk])
                    qin, qink = qinr.next()
                    kin, kink = kinr.next()
                    STT(qin[:, :], gq[:, :], 128.0 ** -0.5, Eq[:, :], ALU.mult, ALU.mult, r=[gqk, Eqk], w=[qink])
                    TT("dve", kin[:, :], gk[:, :], Ek[:, :], ALU.mult, r=[gkk, Ekk], w=[kink])
                    ga, gak = pg.next()
                    for h in range(4):
                        MM(ga[:, h * 128:(h + 1) * 128], kin[:, h * 128:(h + 1) * 128], qin[:, h * 128:(h + 1) * 128],
                           r=[kink, qink], w=[gak])
                    att, attk = attr.next()
                    TT("dve", att[:, :], ga[:, :], mask4[:, :, :].rearrange("p h t -> p (h t)"), ALU.mult,
                       r=[gak, "mask4"], w=[attk])
                    if d == 1:
                        of, ofk = ofr.next()
                        DMA("sp", of[:, :], OF_d[i * 128:(i + 1) * 128, :], r=["OF_d"], w=[ofk])
                        osum, osk = osr.next()
                    else:
                        of, ofk = ofr.next()
                    return dict(hT=hT, hTk=hTk, vg=vg, vgk=vgk, kst=kst, kstk=kstk, qin=qin, qink=qink, att=att, attk=attk,
                                Eq=Eq, Eqk=Eqk, of=of, ofk=ofk, osum=(osum if d == 1 else None), osk=(osk if d == 1 else None))

            def stageB(i, c):
                    hT = c["hT"]; hTk = c["hTk"]; vg = c["vg"]; vgk = c["vgk"]; kst = c["kst"]; kstk = c["kstk"]
                    qin = c["qin"]; qink = c["qink"]; att = c["att"]; attk = c["attk"]; Eq = c["Eq"]; Eqk = c["Eqk"]
                    of = c["of"]; ofk = c["ofk"]; osum = c["osum"]; osk = c["osk"]
                    obs = []
                    for h in range(4):
                        ob_, obk_ = po.t[h % 2]
                        c0 = (h // 2) * 256
                        obs.append((ob_, obk_, c0))
                        MM(ob_[:, c0:c0 + 256], att[:, h * 128:(h + 1) * 128], vg[:, h * 256:(h + 1) * 256],
                           start=(h < 2), stop=False, r=[attk, vgk], w=[obk_])
                    for ci, ch in enumerate(chunks):
                        r0 = ch * 64
                        for h in range(4):
                            ob_, obk_, c0 = obs[h]
                            st_bf, st_k = cur[h]
                            MM(ob_[:, c0:c0 + 256], qin[:, h * 128:(h + 1) * 128], st_bf[:, :],
                               start=False, stop=True, r=[qink, st_k], w=[obk_])
                        for h in range(4):
                            ub, ubk = pu.t[h % 2]
                            uc0 = (h // 2) * 256
                            uap = ub[:, uc0:uc0 + 256]
                            MM(uap, kst[:, h * 128:(h + 1) * 128], vg[:, h * 256:(h + 1) * 256],
                               r=[kstk, vgk], w=[ubk])
                            if d == 0:
                                dcol = h * 128 + 127
                            else:
                                dcol = h * 128
                            STT(Sst[:, d, h, :], Sst[:, d, h, :], Eq[:, dcol:dcol + 1], uap, ALU.mult, ALU.add,
                                r=[skey[h], Eqk, ubk], w=[skey[h]])
                            t, tk = Sbf[h].next()
                            CP("pool", t[:, :], Sst[:, d, h, :], r=[skey[h]], w=[tk])
                            cur[h] = (t, tk)
                    for h in range(4):
                        ob_, obk_, c0 = obs[h]
                        ob = ob_[:, c0:c0 + 256]
                        if d == 0:
                            CP("act", of[:, h * 256:(h + 1) * 256], ob, r=[obk_], w=[ofk])
                        else:
                            TT("dve", osum[:, h * 256:(h + 1) * 256], ob, of[:, h * 256:(h + 1) * 256], ALU.add,
                               r=[obk_, ofk], w=[osk])
                    if d == 0:
                        DMA(STORE_Q, OF_d[i * 128:(i + 1) * 128, :], of[:, :], r=[ofk], w=["OF_d"])
                        return
                    st, stk = small.next()
                    junk, junkk = f32r.next()
                    for h in range(4):
                        STT(junk[:, 0:256], osum[:, h * 256:(h + 1) * 256], 1.0, osum[:, h * 256:(h + 1) * 256],
                            ALU.mult, ALU.mult, r=[osk], w=[junkk, stk], accum_out=st[:, h:h + 1])
                    ACT(st[:, 4:8], st[:, 0:4], AF.Ln, r=[stk], w=[stk], bias=EPS, scale=1.0 / 256.0)
                    ACT(st[:, 8:12], st[:, 4:8], AF.Exp, r=[stk], w=[stk], scale=-0.5)
                    for h in range(4):
                        STT(osum[:, h * 256:(h + 1) * 256], osum[:, h * 256:(h + 1) * 256], st[:, 8 + h:9 + h], ggt[:, :],
                            ALU.mult, ALU.mult, r=[osk, stk, "ggt"], w=[osk])
                    ab, abk = abr.next()
                    for half in range(2):
                        pj, pjk = ppj.next()
                        for kc in range(8):
                            MM(pj[:, :], hT[:, kc, :], Wrg[:, kc, half * 512:(half + 1) * 512], start=(kc == 0), stop=(kc == 7),
                               r=["Wrg", hTk], w=[pjk])
                        sr, srk = srr.next()
                        ACT(sr[:, :], pj[:, :], AF.Silu, r=[pjk], w=[srk])
                        TT("dve", ab[:, half * 512:(half + 1) * 512], osum[:, half * 512:(half + 1) * 512], sr[:, :], ALU.mult,
                           r=[osk, srk], w=[abk])
                    tp, tpk = pg.next()
                    tpb = tp[:, :].bitcast(BF16)
                    for kc in range(8):
                        TR(tpb[:, kc * 128:(kc + 1) * 128], ab[:, kc * 128:(kc + 1) * 128], identb, r=[abk] + CK, w=[tpk])
                    at, atk = atr.next()
                    CP("act", at[:, :, :], tpb[:, 0:1024].rearrange("p (k t) -> p k t", k=8), r=[tpk], w=[atk])
                    DMA(STORE_Q, AT_d[:, i * 128:(i + 1) * 128].rearrange("(kc p) t -> p kc t", p=128), at[:, :, :],
                        r=[atk], w=["AT_d"])

            ctx = stageA(tiles[0])
            for n_, i in enumerate(tiles):
                nxt = stageA(tiles[n_ + 1]) if n_ + 1 < len(tiles) else None
                stageB(i, ctx)
                ctx = nxt
            if d == 1:
                DMA(STORE_Q, HT_d[:, 0:OWN_N * 128].rearrange("(kc p) t -> p kc t", p=128), HT_own[:, :, 0:OWN_N * 128], r=["HT_own"], w=["HT_d"])
        if stop in (2, 3):
            S.waitall("sp", ["OF_d", "AT_d", "HT_d"])
        S.flush()
    if stop in (2, 3):
        return nc
    es_ht.close()

    es5 = ExitStack()
    stage5 = Pool_(es5, "wstage5", 2, [128, 8, 128], F32)
    Wa = sb(es5, "Wa", [128, 8, D], BF16)
    Wb = sb(es5, "Wb", [128, 8, D], BF16)
    Wo = sb(es5, "Wo", [128, 8, D], BF16)
    Wgg = sb(es5, "Wgg", [128, 8, D], BF16)
    Wgd = sb(es5, "Wgd", [128, 8, D], BF16)

    def prefetch_p5():
        wst["pool"] = stage5
        wst["n"] = 128
        load_w(Wa, "Wa", w_bra, 0, D, engs=("pool", "dve"))
        load_w(Wb, "Wb", w_brb, 0, D, engs=("pool", "dve"))
        load_w(Wo, "Wo", w_out, 0, D, engs=("pool", "dve"))
        load_w(Wgg, "Wgg", w_in, C_GG, D, engs=("pool", "dve"))
        load_w(Wgd, "Wgd", w_in, C_GD, D, engs=("pool", "dve"))

    with ExitStack() as es:
        ktp = Pool_(es, "ktp", 2, [128, NALL], BF16)
        vtp = Pool_(es, "vtp", 2, [128, 64, 129], BF16)
        qtp = Pool_(es, "qtp", 2, [128, NOWN], BF16)
        ptr = [Pool_(es, "ptr%d" % m, 3, [128, 512], BF16) for m in range(2)]
        bthr = Pool_(es, "bthr", 2, [128, NOWN], BF16)
        lv = sb(es, "lv", [128, 4, 64], F32)
        lsm = sb(es, "lsm", [128, 16], F32)
        gdt = bcast_load(es, "gdt", diff_g, 128)
        fin = Pool_(es, "fin", 8, [128, 16], F32)
        t1r = Pool_(es, "t1r", 2, [128, 128], F32)
        o1r = Pool_(es, "o1r", 8, [128, 128], F32)
        jr = Pool_(es, "jr", 2, [128, 128], F32)
        bbr = Pool_(es, "bbr", 2, [128, 128], BF16)
        accs = Pool_(es, "accs", 6, [128, 388], F32)
        pst = [Pool_(es, "pst%d" % m, 2, [128, 512], F32, psum=True) for m in range(2)]
        pacc = Pool_(es, "pacc", 3, [128, 512], F32, psum=True)
        ptp = Pool_(es, "ptp4", 1, [128, 512], F32, psum=True)
        for j, v in enumerate((lq1, lk1, lq2, lk2)):
            DMA("sp", lv[:, j, :], v.partition_broadcast(128), w=["lv"])
        junk, junkk = jr.next()
        STT(junk[:, 0:64], lv[:, 0, :], 1.0, lv[:, 1, :], ALU.mult, ALU.mult, r=["lv"], w=[junkk, "lsm"], accum_out=lsm[:, 0:1])
        STT(junk[:, 0:64], lv[:, 2, :], 1.0, lv[:, 3, :], ALU.mult, ALU.mult, r=["lv"], w=[junkk, "lsm"], accum_out=lsm[:, 1:2])
        ACT(lsm[:, 2:4], lsm[:, 0:2], AF.Exp, r=["lsm"], w=["lsm"])
        TT("dve", lsm[:, 4:5], lsm[:, 3:4], lsm[:, 2:3], ALU.subtract, r=["lsm"], w=["lsm"])
        TS_("dve", lsm[:, 5:6], lsm[:, 4:5], -LAM_INIT, None, ALU.add, r=["lsm"], w=["lsm"])
        TS_("dve", gdt[:, :], gdt[:, :], 1.0 - LAM_INIT, None, ALU.mult, r=["gdt"], w=["gdt"])
        for t, tk in vtp.t:
            MEMSET("pool", t[:, :, 128:129], 1.0, w=[tk + "_q%d" % q4 for q4 in range(4)])
        accpos = {}
        n = 0
        for j in range(4):
            for m in range(2):
                accpos[(j, m)] = (n // 3, (n % 3) * 129)
                n += 1
        deferred = [None, None]

        def make_finalize(h, qb, sacc, bth, bthk, last):
            st = {}

            def part1():
                for j in range(4):
                    b0_, c00 = accpos[(j, 0)]
                    b1_, c01 = accpos[(j, 1)]
                    a0, a0k = sacc[b0_]
                    a1, a1k = sacc[b1_]
                    f, fk = fin.next()
                    S.add("dve", lambda e, f=f, a0=a0, c00=c00: e.reciprocal(f[:, 0:1], a0[:, c00 + 128:c00 + 129]), [a0k], [fk])
                    S.add("dve", lambda e, f=f, a1=a1, c01=c01: e.reciprocal(f[:, 1:2], a1[:, c01 + 128:c01 + 129]), [a1k], [fk])
                    TT("dve", f[:, 2:3], f[:, 1:2], lsm[:, 5:6], ALU.mult, r=[fk, "lsm"], w=[fk])
                    t1, t1k = t1r.next()
                    TS_("dve", t1[:, :], a1[:, c01:c01 + 128], f[:, 2:3], None, ALU.mult, r=[a1k, fk], w=[t1k])
                    o1, o1k = o1r.next()
                    STT(o1[:, :], a0[:, c00:c00 + 128], f[:, 0:1], t1[:, :], ALU.mult, ALU.add, r=[a0k, fk, t1k], w=[o1k])
                    junk, junkk = jr.next()
                    STT(junk[:, :], o1[:, :], 1.0, o1[:, :], ALU.mult, ALU.mult, r=[o1k], w=[junkk, fk], accum_out=f[:, 3:4])
                    st[j] = (f, fk, o1, o1k)

            def part2():
                for j in range(4):
                    f, fk, o1, o1k = st[j]
                    ACT(f[:, 4:5], f[:, 3:4], AF.Ln, r=[fk], w=[fk], bias=EPS, scale=1.0 / 128.0)
                    ACT(f[:, 5:6], f[:, 4:5], AF.Exp, r=[fk], w=[fk], scale=-0.5)
                for j in range(4):
                    f, fk, o1, o1k = st[j]
                    bb, bbk = bbr.next()
                    STT(bb[:, :], o1[:, :], f[:, 5:6], gdt[:, :], ALU.mult, ALU.mult, r=[o1k, fk, "gdt"], w=[bbk])
                    tp, tpk = ptp.t[0]
                    tpb = tp[:, :].bitcast(BF16)
                    TR(tpb[:, 0:128], bb[:, :], identb, r=[bbk] + CK, w=[tpk])
                    tok0 = qb * 512 + j * 128
                    CP("dve", bth[:, tok0:tok0 + 128], tpb[:, 0:128], r=[tpk], w=[bthk])
                if last:
                    DMA(STORE_Q, BT_d[h * 128:(h + 1) * 128, 0:P4_QB * 512], bth[:, 0:P4_QB * 512], r=[bthk], w=["BT_d"])
            return part1, part2

        for h in range(P4_H):
            kT, kTk = ktp.next()
            vt, vtk = vtp.next()
            qT, qTk = qtp.next()
            DMA("sp", kT[:, :], KT_d[h, :, :], r=["KT_d"], w=[kTk])
            DMA("sp", qT[:, :], QT_d[h, :, :], r=["QT_d"], w=[qTk])
            for q4 in range(4):
                DMA("sp", vt[:, q4 * 16:(q4 + 1) * 16, 0:128],
                    V_d[q4 * 2048:(q4 + 1) * 2048, h * 128:(h + 1) * 128].rearrange("(kt p) c -> p kt c", p=128),
                    r=["V_d"], w=[vtk + "_q%d" % q4])
            if h == 0:
                prefetch_p5()
            bth, bthk = bthr.next()
            for qb in range(P4_QB):
                banks = [pacc.t[b] for b in range(3)]
                started = [False, False, False]
                pts_of = {}

                def qk_exp(kt_, m):
                    st_, stk_ = pst[m].next()
                    MM(st_[:, :], kT[m * 64:(m + 1) * 64, kt_ * 128:(kt_ + 1) * 128],
                       qT[m * 64:(m + 1) * 64, qb * 512:(qb + 1) * 512], r=[kTk, qTk], w=[stk_])
                    pt, ptk = ptr[m].next()
                    ACT(pt[:, :], st_[:, :], AF.Exp, r=[stk_], w=[ptk], scale=0.125)
                    pts_of[(kt_, m)] = (pt, ptk)

                for k0 in range(2):
                    for m in range(2):
                        qk_exp(k0, m)
                for kt_ in range(P4_KT):
                    if kt_ == 2 and deferred[0] is not None:
                        deferred[0]()
                    if kt_ == 12 and deferred[1] is not None:
                        deferred[1]()
                        deferred[0] = deferred[1] = None
                    for m in range(2):
                        pt, ptk = pts_of.pop((kt_, m))
                        for j in range(4):
                            b, c0 = accpos[(j, m)]
                            bt_, btk_ = banks[b]
                            first = (kt_ == 0 and not started[b])
                            if kt_ == 0:
                                started[b] = True
                            MM(bt_[:, c0:c0 + 129], pt[:, j * 128:(j + 1) * 128], vt[:, kt_, :],
                               start=first, stop=(kt_ == P4_KT - 1), r=[ptk, vtk + "_q%d" % (kt_ // 16)], w=[btk_])
                    if kt_ + 2 < P4_KT:
                        qk_exp(kt_ + 2, 0)
                        qk_exp(kt_ + 2, 1)
                sacc = []
                for b in range(3):
                    a_, ak_ = accs.next()
                    ncol = 387 if b < 2 else 258
                    CP("dve", a_[:, 0:ncol], banks[b][0][:, 0:ncol], r=[banks[b][1]], w=[ak_])
                    sacc.append((a_, ak_))
                p1_, p2_ = make_finalize(h, qb, sacc, bth, bthk, qb == P4_QB - 1)
                deferred[0], deferred[1] = p1_, p2_
        if deferred[0] is not None:
            deferred[0]()
            deferred[1]()
        if stop == 4:
            S.waitall("sp", ["BT_d"])
        S.flush()
    if stop == 4:
        return nc

    with ExitStack() as es:
        Wr = sb(es, "Wr", [128, 8, NE], F32)
        DMA("sp", Wr[:, :, :], router_w.rearrange("(kc p) n -> p kc n", p=128), w=["Wr"])
        g1t = bcast_load(es, "g1t", ln1_g, D)
        b1t = bcast_load(es, "b1t", ln1_b, D)
        rbt = bcast_load(es, "rbt", router_b, NE)
        cum = sb(es, "cum", [128, NE], F32)
        MEMSET("dve", cum[:, :], 0.0, w=["cum"])
        blk = Pool_(es, "blk", 3, [128, 8, 512], BF16)
        mTt = sb(es, "mTt", [128, 8, 512], BF16)
        sgr = Pool_(es, "sgr", 4, [128, 512], F32)
        hr = Pool_(es, "hr5", 2, [128, D], F32)
        rr = Pool_(es, "rr5", 2, [128, D], F32)
        h1r = Pool_(es, "h1r", 2, [128, D], F32)
        h1br = Pool_(es, "h1br", 2, [128, D], BF16)
        h1Tr = Pool_(es, "h1Tr", 2, [128, 8, 128], F32)
        small = Pool_(es, "small5", 3, [128, 16], F32)
        rt = Pool_(es, "rt5", 2, [128, 8, 32], F32)
        mkb = Pool_(es, "mkb5", 2, [128, 32], BF16)
        pm = Pool_(es, "pm5", 4, [128, 512], F32, psum=True)
        pt2 = Pool_(es, "pt5", 2, [128, 512], F32, psum=True)
        pr = Pool_(es, "pr5", 2, [128, 512], F32, psum=True)
        for tb in range(P5_TB):
            at, atk = blk.next()
            bt, btk = blk.next()
            ht, htk = blk.next()
            cols = slice(tb * 512, (tb + 1) * 512)
            DMA("sp", at[:, :, :], AT_d[:, cols].rearrange("(kc p) t -> p kc t", p=128), r=["AT_d"], w=[atk])
            DMA("sp", bt[:, :, :], BT_d[:, cols].rearrange("(kc p) t -> p kc t", p=128), r=["BT_d"], w=[btk])
            DMA("sp", ht[:, :, :], HT_d[:, cols].rearrange("(kc p) t -> p kc t", p=128), r=["HT_d"], w=[htk])
            for oc in range(8):
                ocs = slice(oc * 128, (oc + 1) * 128)
                pa, pak = pm.next(); pga, pgak = pm.next(); pb, pbk = pm.next(); pgb, pgbk = pm.next()
                for (po_, pok_, W_, wk_, x_, xk_) in ((pa, pak, Wa, "Wa", at, atk), (pga, pgak, Wgg, "Wgg", ht, htk),
                                                    (pb, pbk, Wb, "Wb", bt, btk), (pgb, pgbk, Wgd, "Wgd", ht, htk)):
                    for kc in range(8):
                        MM(po_[:, :], W_[:, kc, ocs], x_[:, kc, :], start=(kc == 0), stop=(kc == 7), r=[wk_, xk_], w=[pok_])
                sa, sak = sgr.next(); sb_, sbk = sgr.next()
                ACT(sa[:, :], pga[:, :], AF.Sigmoid, r=[pgak], w=[sak])
                ACT(sb_[:, :], pgb[:, :], AF.Sigmoid, r=[pgbk], w=[sbk])
                TT("dve", sa[:, :], pa[:, :], sa[:, :], ALU.mult, r=[pak, sak], w=[sak])
                TT("dve", sb_[:, :], pb[:, :], sb_[:, :], ALU.mult, r=[pbk, sbk], w=[sbk])
                TT("dve", mTt[:, oc, :], sa[:, :], sb_[:, :], ALU.add, r=[sak, sbk], w=["mTt"])
            for tt in range(P5_TT):
                ti = tb * 4 + tt
                h0, h0k = hr.next()
                DMA("sp", h0[:, :], H_d[ti * 128:(ti + 1) * 128, :], r=["H_d"], w=[h0k])
                r_, rk_ = rr.next()
                for half in range(2):
                    po_, pok_ = pm.next()
                    for kc in range(8):
                        MM(po_[:, :], mTt[:, kc, tt * 128:(tt + 1) * 128], Wo[:, kc, half * 512:(half + 1) * 512],
                           start=(kc == 0), stop=(kc == 7), r=["mTt", "Wo"], w=[pok_])
                    STT(r_[:, half * 512:(half + 1) * 512], h0[:, half * 512:(half + 1) * 512], DN_ALPHA, po_[:, :],
                        ALU.mult, ALU.add, r=[h0k, pok_], w=[rk_])
                h1, h1k = h1r.next()
                layernorm(es, "ln1", r_, rk_, g1t, "g1t", b1t, "b1t", h1, h1k, small)
                DMA(STORE_Q, H1_d[ti * 128:(ti + 1) * 128, :], h1[:, :], r=[h1k], w=["H1_d"])
                h1b, h1bk = h1br.next()
                CP("act", h1b[:, :], h1[:, :], r=[h1k], w=[h1bk])
                h1T, h1Tk = h1Tr.next()
                for half in range(2):
                    tp, tpk = pt2.next()
                    for q in range(4):
                        kc = half * 4 + q
                        TR(tp[:, q * 128:(q + 1) * 128], h1[:, kc * 128:(kc + 1) * 128], identf, r=[h1k, "cstf"], w=[tpk])
                    CP("act", h1T[:, half * 4:(half + 1) * 4, :], tp[:, :].rearrange("p (k t) -> p k t", k=4), r=[tpk], w=[h1Tk])
                pl, plk = pr.next()
                for kc in range(8):
                    MM(pl[:, 0:NE], h1T[:, kc, :], Wr[:, kc, :], start=(kc == 0), stop=(kc == 7), r=[h1Tk, "Wr"], w=[plk])
                R, Rk = rt.next()
                sm, smk = small.next()
                TT("dve", R[:, 0, :], pl[:, 0:NE], rbt[:, :], ALU.add, r=[plk, "rbt"], w=[Rk])
                S.add("dve", lambda e, sm=sm, R=R: e.max(sm[:, 0:8], R[:, 0, :]), [Rk], [smk])
                TS_("dve", R[:, 1, :], R[:, 0, :], sm[:, 3:4], None, ALU.is_ge, r=[Rk, smk], w=[Rk])
                TS_("dve", sm[:, 8:9], sm[:, 0:1], -1.0, None, ALU.mult, r=[smk], w=[smk])
                ACT(R[:, 2, :], R[:, 0, :], AF.Exp, r=[Rk, smk], w=[Rk], bias=sm[:, 8:9])
                STT(R[:, 2, :], R[:, 2, :], 1.0, R[:, 1, :], ALU.mult, ALU.mult, r=[Rk], w=[Rk, smk], accum_out=sm[:, 9:10])
                S.add("dve", lambda e, sm=sm: e.reciprocal(sm[:, 10:11], sm[:, 9:10]), [smk], [smk])
                TS_("dve", R[:, 3, :], R[:, 2, :], sm[:, 10:11], None, ALU.mult, r=[Rk, smk], w=[Rk])
                mb, mbk = mkb.next()
                CP("dve", mb[:, :], R[:, 1, :], r=[Rk], w=[mbk])
                pc, pck = pr.next()
                MM(pc[:, 0:NE], LTb, mb[:, :], r=CK + [mbk], w=[pck])
                MM(pc[:, 64:64 + NE], ONEb, mb[:, :], r=CK + [mbk], w=[pck])
                TT("dve", R[:, 4, :], pc[:, 0:NE], cum[:, :], ALU.add, r=[pck, "cum"], w=[Rk])
                TS_("dve", R[:, 4, :], R[:, 4, :], float(CAP - 1), None, ALU.min, r=[Rk], w=[Rk])
                TT("dve", R[:, 4, :], R[:, 4, :], ioe[:, :], ALU.add, r=[Rk, "ioe"], w=[Rk])
                TT("dve", cum[:, :], cum[:, :], pc[:, 64:64 + NE], ALU.add, r=[pck, "cum"], w=["cum"])
                for k in range(4):
                    TS_("dve", R[:, 5, :], R[:, 0, :], sm[:, k:k + 1], None, ALU.is_equal, r=[Rk, smk], w=[Rk])
                    STT(R[:, 6, :], R[:, 5, :], 1.0, R[:, 4, :], ALU.mult, ALU.mult, r=[Rk], w=[Rk, smk],
                        accum_out=sm[:, 11 + k:12 + k])
                    STT(R[:, 6, :], R[:, 5, :], 1.0, R[:, 3, :], ALU.mult, ALU.mult, r=[Rk], w=[Rk, "GATE"],
                        accum_out=GATE[:, ti, k:k + 1])
                CP("dve", IDX[:, ti, :], sm[:, 11:15], r=[smk], w=["IDX"])
                for k in range(4):
                    S.add("pool", lambda e, h1b=h1b, ti=ti, k=k: e.indirect_dma_start(
                        out=XG_d[:, :], out_offset=bass.IndirectOffsetOnAxis(ap=IDX[:, ti, k:k + 1], axis=0),
                        in_=h1b[:, :], in_offset=None), r=[h1bk, "IDX"], w=["XG_d"], dma=True)
        if stop == 5:
            S.waitall("sp", ["XG_d", "H1_d"])
        S.flush()
    es5.close()
    if stop == 5:
        return nc

    with ExitStack() as es:
        new_stage(es, 4, 256)
        wgp = Pool_(es, "wgp", 2, [128, 8, D], BF16)
        wup = Pool_(es, "wup", 2, [128, 8, D], BF16)
        wdp = Pool_(es, "wdp", 2, [128, 8, D], BF16)
        bcol = sb(es, "bcol", [128, 2, 256], F32)
        bld = Pool_(es, "bld", 2, [128, 128], F32)
        bdf = Pool_(es, "bdf", 2, [1, D], F32)
        bdb = Pool_(es, "bdb", 2, [1, D], BF16)
        xgp = Pool_(es, "xgp", 2, [128, D], BF16)
        xTp = Pool_(es, "xTp", 2, [128, 8, CAP], BF16)
        aTp = Pool_(es, "aTp", 2, [128, 8, CAP], BF16)
        gr = Pool_(es, "gr7", 2, [128, CAP], F32)
        ur = Pool_(es, "ur7", 2, [128, CAP], F32)
        sr7 = Pool_(es, "sr7", 2, [128, CAP], F32)
        yr = Pool_(es, "yr7", 2, [128, D], F32)
        pgu = Pool_(es, "pgu7", 4, [128, 512], F32, psum=True)
        py = Pool_(es, "py7", 2, [128, 512], F32, psum=True)
        ptp = Pool_(es, "ptp7", 2, [128, 512], F32, psum=True)
        for gi, bsrc in enumerate((eb_g, eb_u)):
            bv = bsrc.rearrange("e (fc p) -> (e fc) p", p=128)
            for half in range(2):
                bl_, blk_ = bld.next()
                DMA("sp", bl_[:, :], bv[half * 128:(half + 1) * 128, :], w=[blk_])
                tp, tpk = ptp.next()
                TR(tp[:, 0:128], bl_[:, :], identf, r=[blk_, "cstf"], w=[tpk])
                CP("act", bcol[:, gi, half * 128:(half + 1) * 128], tp[:, 0:128], r=[tpk], w=["bcol"])
        CE = ("act", "dve", "act", "dve", "pool", "act", "dve", "act", "dve", "pool", "act", "dve")

        def expert_w(e_):
            wg, wgk = wgp.next(); wu, wuk = wup.next(); wd, wdk = wdp.next()

            def gen():
                n = 0
                for (dst, dkey, src) in ((wg, wgk, ew_g[e_]), (wu, wuk, ew_u[e_]), (wd, wdk, ew_d[e_])):
                    for kc0 in range(0, 8, 2):
                        st, sk = wst["pool"].next()
                        stv = st[:, :, :].rearrange("p a b -> p (a b)").rearrange("p (k n) -> p k n", k=2)
                        DMA("sp", stv, src[kc0 * 128:(kc0 + 2) * 128, :].rearrange("(kc p) n -> p kc n", p=128), w=[sk])
                        CP(CE[n % len(CE)], dst[:, kc0:kc0 + 2, :], stv, r=[sk], w=[dkey])
                        n += 1
                        yield
            return (wg, wgk, wu, wuk, wd, wdk), gen()

        wcur, g0 = expert_w(0)
        for _ in g0:
            pass

        def x_transposes(e_):
            xT, xTk = xTp.next()
            for st_ in range(3):
                xg, xgk = xgp.next()
                r0 = e_ * CAP + st_ * 128
                DMA("sp", xg[:, :], XG_d[r0:r0 + 128, :], r=["XG_d"], w=[xgk])
                tp, tpk = ptp.next()
                tpb = tp[:, :].bitcast(BF16)
                for kc in range(8):
                    TR(tpb[:, kc * 128:(kc + 1) * 128], xg[:, kc * 128:(kc + 1) * 128], identb, r=[xgk] + CK, w=[tpk])
                CP("dve", xT[:, :, st_ * 128:(st_ + 1) * 128], tpb[:, 0:1024].rearrange("p (k t) -> p k t", k=8),
                   r=[tpk], w=[xTk])
            return xT, xTk

        def gu(e_, fc, wg, wgk, wu, wuk, xT, xTk, aT, aTk):
            fcs = slice(fc * 128, (fc + 1) * 128)
            pgt, pgtk = pgu.next(); put, putk = pgu.next()
            for kc in range(8):
                MM(pgt[:, 0:CAP], wg[:, kc, fcs], xT[:, kc, :], start=(kc == 0), stop=(kc == 7), r=[wgk, xTk], w=[pgtk])
            for kc in range(8):
                MM(put[:, 0:CAP], wu[:, kc, fcs], xT[:, kc, :], start=(kc == 0), stop=(kc == 7), r=[wuk, xTk], w=[putk])
            bi = e_ * 8 + fc
            g_, gk_ = gr.next(); u_, uk_ = ur.next(); s_, sk_ = sr7.next()
            TS_("dve", g_[:, :], pgt[:, 0:CAP], bcol[:, 0, bi:bi + 1], 7.0, ALU.add, ALU.min, r=[pgtk, "bcol"], w=[gk_])
            TS_("dve", u_[:, :], put[:, 0:CAP], bcol[:, 1, bi:bi + 1], 7.0, ALU.add, ALU.min, r=[putk, "bcol"], w=[uk_])
            TS_("dve", u_[:, :], u_[:, :], -7.0, 1.0, ALU.max, ALU.add, r=[uk_], w=[uk_])
            ACT(s_[:, :], g_[:, :], AF.Sigmoid, r=[gk_], w=[sk_], scale=1.702)
            TT("dve", g_[:, :], g_[:, :], s_[:, :], ALU.mult, r=[gk_, sk_], w=[gk_])
            TT("dve", aT[:, fc, :], g_[:, :], u_[:, :], ALU.mult, r=[gk_, uk_], w=[aTk])

        def down(e_, aT, aTk, wd, wdk, bb_, bbk, gnext):
            for st_ in range(3):
                y_, yk_ = yr.next()
                for half in range(2):
                    pyt, pytk = py.next()
                    for fc in range(8):
                        MM(pyt[:, :], aT[:, fc, st_ * 128:(st_ + 1) * 128], wd[:, fc, half * 512:(half + 1) * 512],
                           start=(fc == 0), stop=False, r=[aTk, wdk], w=[pytk])
                    MM(pyt[:, :], ONEb[0:1, :], bb_[0:1, half * 512:(half + 1) * 512], start=False, stop=True,
                       r=CK + [bbk], w=[pytk])
                    CP("act", y_[:, half * 512:(half + 1) * 512], pyt[:, :], r=[pytk], w=[yk_])
                r0 = e_ * CAP + st_ * 128
                DMA(STORE_Q, YG_d[r0:r0 + 128, :], y_[:, :], r=[yk_], w=["YG_d"])
                next(gnext, None)
                next(gnext, None)

        xcur = x_transposes(0)
        prev = None
        for e_ in range(P7_E):
            wg, wgk, wu, wuk, wd, wdk = wcur
            if e_ + 1 < P7_E:
                wnext, gnext = expert_w(e_ + 1)
            else:
                wnext, gnext = None, iter(())
            bf_, bfk = bdf.next(); bb_, bbk = bdb.next()
            DMA("sp", bf_[:, :], eb_d[e_:e_ + 1, :], w=[bfk])
            CP("dve", bb_[:, :], bf_[:, :], r=[bfk], w=[bbk])
            xT, xTk = xcur
            aT, aTk = aTp.next()
            for fc in range(2):
                gu(e_, fc, wg, wgk, wu, wuk, xT, xTk, aT, aTk)
                next(gnext, None)
            if prev is not None:
                down(*prev, gnext)
            if e_ + 1 < P7_E:
                xcur = x_transposes(e_ + 1)
            for fc in range(2, 8):
                gu(e_, fc, wg, wgk, wu, wuk, xT, xTk, aT, aTk)
                next(gnext, None)
            for _ in gnext:
                pass
            prev = (e_, aT, aTk, wd, wdk, bb_, bbk)
            wcur = wnext
        down(*prev, iter(()))
        if stop == 7:
            S.waitall("sp", ["YG_d"])
        S.flush()
    if stop == 7:
        return nc

    with ExitStack() as es:
        g2t = bcast_load(es, "g2t", ln2_g, D)
        b2t = bcast_load(es, "b2t", ln2_b, D)
        h1r = Pool_(es, "h1r8", 2, [128, D], F32)
        ykr = Pool_(es, "ykr8", 4, [128, D], F32)
        accr = Pool_(es, "accr8", 2, [128, D], F32)
        outr = Pool_(es, "outr8", 2, [128, D], F32)
        small = Pool_(es, "small8", 2, [128, 16], F32)
        outk = []
        for ti in range(P8_T):
            h1, h1k = h1r.next()
            DMA("sp", h1[:, :], H1_d[ti * 128:(ti + 1) * 128, :], r=["H1_d"], w=[h1k])
            acc, acck = accr.next()
            TS_("dve", acc[:, :], h1[:, :], DN_ALPHA, None, ALU.mult, r=[h1k], w=[acck])
            for k in range(4):
                yk, ykk = ykr.next()
                S.add("pool", lambda e, yk=yk, ti=ti, k=k: e.indirect_dma_start(
                    out=yk[:, :], out_offset=None, in_=YG_d[:, :],
                    in_offset=bass.IndirectOffsetOnAxis(ap=IDX[:, ti, k:k + 1], axis=0)),
                    r=["YG_d", "IDX"], w=[ykk], dma=True)
                STT(acc[:, :], yk[:, :], GATE[:, ti, k:k + 1], acc[:, :], ALU.mult, ALU.add, r=[ykk, "GATE", acck], w=[acck])
            o_, ok_ = outr.next()
            layernorm(es, "ln2", acc, acck, g2t, "g2t", b2t, "b2t", o_, ok_, small)
            kk = "out%d" % ti
            DMA("sp", out[ti * 128:(ti + 1) * 128, :], o_[:, :], r=[ok_], w=[kk])
            outk.append(kk)
        S.waitall("sp", outk)
        S.flush()
    es0.close()
    return nc


def _consts():
    c = np.zeros((128, 8, 128), np.float32)
    idx = np.arange(128)
    same = np.ones((128, 128), bool)
    c[:, 0, :] = np.eye(128, dtype=np.float32)
    c[:, 1, :] = (same & (idx[:, None] <= idx[None, :])).astype(np.float32)
    c[:, 2, :] = (same & (idx[:, None] >= idx[None, :])).astype(np.float32)
    c[:, 3, :] = (same & (idx[:, None] > idx[None, :])).astype(np.float32)
    c[:, 4, :] = (same & (idx[:, None] < idx[None, :])).astype(np.float32)
    c[:, 5, :] = (idx[:, None] < idx[None, :]).astype(np.float32)
    c[:, 6, :] = 1.0
    c[:, 7, 0] = (idx < 64).astype(np.float32)
    c[:, 7, 1] = (idx >= 64).astype(np.float32)
    c2 = np.tile((np.arange(32, dtype=np.float32) * CAP)[None, :], (128, 1)).astype(np.float32)
    return c, c2


_NC_CACHE = {}


def kernel(**inp):
    x = np.asarray(inp["x"], np.float32)
    if "nc" not in _NC_CACHE:
        _NC_CACHE["nc"] = build()
    nc = _NC_CACHE["nc"]
    cst, cst2 = _consts()
    pos = np.arange(8192, dtype=np.float32)
    inv_freq = np.power(np.float32(500000.0), -np.arange(0, 16, 2, dtype=np.float32) / np.float32(16)).astype(np.float32)
    ang = (pos[:, None] * inv_freq[None, :]).astype(np.float32)
    cs_full = np.concatenate([np.tile(np.cos(ang), (1, 8)), np.tile(np.sin(ang), (1, 8))], axis=1).astype(np.float32)

    def sq(name):
        a = np.asarray(inp[name], np.float32)
        return np.ascontiguousarray(a[0]) if a.shape[0] == 1 and name not in ("ln0_g", "ln0_b") else np.ascontiguousarray(a)

    shared = {
        "cst": cst, "cst2": cst2,
        "ln0_g": np.ascontiguousarray(inp["ln0_g"], np.float32), "ln0_b": np.ascontiguousarray(inp["ln0_b"], np.float32),
    }
    for nm in ("w_in", "gla_wf_fwd", "gla_bf_fwd", "gla_wf_bwd", "gla_bf_bwd", "gla_norm_g", "diff_lq1", "diff_lk1",
               "diff_lq2", "diff_lk2", "diff_norm_g", "w_br_gla", "w_br_diff", "w_out", "ln1_g", "ln1_b", "router_w",
               "router_b", "exp_w_gate", "exp_b_gate", "exp_w_up", "exp_b_up", "exp_w_down", "exp_b_down", "ln2_g", "ln2_b"):
        shared[nm] = np.ascontiguousarray(np.asarray(inp[nm], np.float32)[0])
    in_maps = []
    for c in range(8):
        b, qd = c // 4, c % 4
        own = np.arange(qd * 2048, (qd + 1) * 2048)
        oth = np.concatenate([np.arange(0, qd * 2048), np.arange((qd + 1) * 2048, 8192)])
        order = np.concatenate([oth, own])
        mk = np.zeros((8192, 2), np.float32)
        mk[:6144, 0] = (oth < qd * 2048).astype(np.float32)
        mk[:6144, 1] = (oth >= (qd + 1) * 2048).astype(np.float32)
        m = dict(shared)
        m["xa"] = np.ascontiguousarray(x[b][order])
        m["csa"] = np.ascontiguousarray(np.concatenate([cs_full[order], mk], axis=1))
        in_maps.append(m)
    res = run_bass_kernel_spmd(nc, in_maps, core_ids=list(range(8)))
    _NC_CACHE["last"] = res
    outp = np.zeros((2, 8192, 1024), np.float32)
    for c in range(8):
        b, qd = c // 4, c % 4
        outp[b, qd * 2048:(qd + 1) * 2048] = res.results[c]["out"]
    return outp
```

```python
import numpy as np
from contextlib import ExitStack
import concourse.bass as bass
import concourse.mybir as mybir
from concourse.bass_utils import run_bass_kernel_spmd

F32 = mybir.dt.float32
BF16 = mybir.dt.bfloat16
I32 = mybir.dt.int32
AF = mybir.ActivationFunctionType
ALU = mybir.AluOpType

D = 1024
NOWN = 2048
NOTH = 6144
NALL = 8192
CAP = 384
NE = 32
EPS = 1e-5
DN_ALPHA = 2.0 ** 0.25
LAM_INIT = 0.2
SAME_ENG_SYNC = True
P1_TILES = list(range(64))
P1_LEVEL = 9
OWN_N = 16
P4_H = 8
P4_QB = 4
P4_KT = 64
P5_TB = 4
P5_TT = 4
P7_E = 32
P8_T = 16
STORE_Q = 'pool'
NO_ROPE = False
DEBUG = False

C_QG, C_KG, C_VG, C_RG, C_LRF, C_LRB, C_QD, C_KD, C_VD, C_GG, C_GD = (
    0, 512, 1024, 2048, 3072, 3088, 3104, 4128, 5152, 6176, 7200)

ENG_ATTR = {"pe": "tensor", "act": "scalar", "dve": "vector", "pool": "gpsimd", "sp": "sync"}
NRING = 12


class Op:
    __slots__ = ("eng", "fn", "deps", "signal", "sem", "val", "dma", "real")


class Sched:
    def __init__(self, nc, es):
        self.nc = nc
        self.engs = ["pe", "act", "dve", "pool", "sp"]
        self.csem = {e: es.enter_context(nc.semaphore("c_" + e)) for e in ["pe", "act", "dve", "pool"]}
        self.ccnt = {e: 0 for e in self.csem}
        self.ring = {q: [es.enter_context(nc.semaphore("d_%s%d" % (q, i))) for i in range(NRING)]
                     for q in ["sp", "pool"]}
        self.rcnt = {q: 0 for q in self.ring}
        self.rlast = {q: [None] * NRING for q in self.ring}
        self.ops = {e: [] for e in self.engs}
        self.lw = {}
        self.rd = {}
        self.seen = {e: {} for e in self.engs}
        self.nops = 0

    def add(self, eng, fn, r=(), w=(), dma=False):
        op = Op()
        op.eng = eng; op.fn = fn; op.dma = dma; op.signal = False; op.deps = []
        op.sem = None; op.val = None; op.real = True
        deps = []
        for k in r:
            o = self.lw.get(k)
            if o is not None:
                deps.append(o)
        for k in w:
            o = self.lw.get(k)
            if o is not None:
                deps.append(o)
            deps.extend(self.rd.get(k, ()))
        if dma:
            q = eng
            i = self.rcnt[q]
            slot = i % NRING
            prev = self.rlast[q][slot]
            if prev is not None:
                deps.append(prev)
            op.sem = self.ring[q][slot]
            op.val = 16 * (i // NRING + 1)
            self.rcnt[q] = i + 1
            self.rlast[q][slot] = op
        sd = set()
        for d in deps:
            if id(d) in sd:
                continue
            sd.add(id(d))
            if (not d.dma) and (not dma) and d.eng == eng:
                if eng == "pe" or not SAME_ENG_SYNC:
                    continue
            if not d.dma:
                d.signal = True
            op.deps.append(d)
        for k in r:
            self.rd.setdefault(k, []).append(op)
        for k in w:
            self.lw[k] = op
            self.rd[k] = []
        self.ops[eng].append(op)
        self.nops += 1
        return op

    def waitall(self, eng, keys):
        op = Op()
        op.eng = eng; op.fn = (lambda e: None); op.dma = False; op.signal = False; op.sem = None; op.val = None
        op.deps = []
        op.real = False
        for k in keys:
            o = self.lw.get(k)
            if o is not None:
                if not o.dma:
                    o.signal = True
                op.deps.append(o)
        self.ops[eng].append(op)

    def barrier(self):
        lasts = []
        for eng in self.engs:
            real = [o for o in self.ops[eng] if not o.dma and o.deps is not None and getattr(o, "real", True)]
            if real:
                o = real[-1]
                o.signal = True
                lasts.append(o)
        dmas = []
        for q in self.ring:
            for o in self.rlast[q]:
                if o is not None:
                    dmas.append(o)
        for eng in self.engs:
            op = Op()
            op.eng = eng; op.fn = (lambda e: None); op.dma = False; op.signal = False; op.sem = None; op.val = None
            op.deps = [o for o in lasts if o.eng != eng] + dmas
            self.ops[eng].append(op)

    def flush(self):
        nc = self.nc
        self.barrier()
        for o in self.lw.values():
            if not o.dma and o.val is None:
                o.signal = True
        for lst in self.rd.values():
            for o in lst:
                if not o.dma and o.val is None:
                    o.signal = True
        for eng in self.engs:
            for op in self.ops[eng]:
                if not op.dma and op.signal:
                    self.ccnt[eng] += 1
                    op.val = self.ccnt[eng]
                    op.sem = self.csem[eng]
                elif not op.dma:
                    op.val = -1
        with nc.Block() as blk:
            for eng in self.engs:
                ops = self.ops[eng]
                if not ops:
                    continue

                def body(e, ops=ops, eng=eng):
                    seen = self.seen[eng]
                    for op in ops:
                        for d in op.deps:
                            assert d.val is not None and d.val > 0, "dep not signalled"
                            key = id(d.sem)
                            if seen.get(key, 0) < d.val:
                                e.wait_ge(d.sem, d.val)
                                seen[key] = d.val
                        ins = op.fn(e)
                        if ins is None:
                            continue
                        if op.dma:
                            ins.then_inc(op.sem, 16)
                        elif op.signal:
                            ins.then_inc(op.sem, 1)

                getattr(blk, ENG_ATTR[eng])(body)
        self.ops = {e: [] for e in self.engs}


def build(stop=99, debug=False):
    global DEBUG
    DEBUG = debug
    nc = bass.Bass("TRN2", target_bir_lowering=False)

    def din(name, shape, dt=F32):
        return nc.dram_tensor(name, list(shape), dt, kind="ExternalInput").ap()

    def dscr(name, shape, dt):
        kind = "ExternalOutput" if DEBUG else "Internal"
        return nc.dram_tensor(name, list(shape), dt, kind=kind).ap()

    xa = din("xa", [NALL, D])
    csa = din("csa", [NALL, 130])
    cst = din("cst", [128, 8, 128])
    cst2 = din("cst2", [128, 32])
    ln0_g = din("ln0_g", [D]); ln0_b = din("ln0_b", [D])
    w_in = din("w_in", [D, 8224])
    wf_f = din("gla_wf_fwd", [16, 512]); bf_f = din("gla_bf_fwd", [512])
    wf_b = din("gla_wf_bwd", [16, 512]); bf_b = din("gla_bf_bwd", [512])
    gla_g = din("gla_norm_g", [256])
    lq1 = din("diff_lq1", [64]); lk1 = din("diff_lk1", [64])
    lq2 = din("diff_lq2", [64]); lk2 = din("diff_lk2", [64])
    diff_g = din("diff_norm_g", [128])
    w_bra = din("w_br_gla", [D, D]); w_brb = din("w_br_diff", [D, D]); w_out = din("w_out", [D, D])
    ln1_g = din("ln1_g", [D]); ln1_b = din("ln1_b", [D])
    router_w = din("router_w", [D, NE]); router_b = din("router_b", [NE])
    ew_g = din("exp_w_gate", [NE, D, D]); eb_g = din("exp_b_gate", [NE, D])
    ew_u = din("exp_w_up", [NE, D, D]); eb_u = din("exp_b_up", [NE, D])
    ew_d = din("exp_w_down", [NE, D, D]); eb_d = din("exp_b_down", [NE, D])
    ln2_g = din("ln2_g", [D]); ln2_b = din("ln2_b", [D])
    out = nc.dram_tensor("out", [NOWN, D], F32, kind="ExternalOutput").ap()

    KT_d = dscr("KT_d", [8, 128, NALL], BF16)
    V_d = dscr("V_d", [NALL, D], BF16)
    QT_d = dscr("QT_d", [8, 128, NOWN], BF16)
    H_d = dscr("H_d", [NOWN, D], F32)
    HT_d = dscr("HT_d", [D, NOWN], BF16)
    OF_d = dscr("OF_d", [NOWN, D], F32)
    AT_d = dscr("AT_d", [D, NOWN], BF16)
    BT_d = dscr("BT_d", [D, NOWN], BF16)
    H1_d = dscr("H1_d", [NOWN, D], F32)
    XG_d = dscr("XG_d", [NE * CAP, D], BF16)
    YG_d = dscr("YG_d", [NE * CAP, D], F32)

    es0 = ExitStack()
    S = Sched(nc, es0)

    def MM(out_, lhsT, rhs, start=True, stop=True, r=(), w=()):
        return S.add("pe", lambda e: e.matmul(out_, lhsT, rhs, start=start, stop=stop, skip_group_check=True), r, w)

    def TR(out_, in_, ident, r=(), w=()):
        return S.add("pe", lambda e: e.transpose(out_, in_, ident), r, w)

    def ACT(out_, in_, func, r=(), w=(), bias=0.0, scale=1.0, accum_out=None):
        if accum_out is None:
            return S.add("act", lambda e: e.activation(out_, in_, func, bias=bias, scale=scale), r, w)
        return S.add("act", lambda e: e.activation(out_, in_, func, bias=bias, scale=scale, accum_out=accum_out), r, w)

    def TS_(eng, out_, in0, s1, s2, op0, op1=None, r=(), w=(), accum_out=None):
        if op1 is None:
            return S.add(eng, lambda e: e.tensor_scalar(out_, in0, s1, None, op0), r, w)
        if accum_out is not None:
            return S.add(eng, lambda e: e.tensor_scalar(out_, in0, s1, s2, op0, op1, accum_out), r, w)
        return S.add(eng, lambda e: e.tensor_scalar(out_, in0, s1, s2, op0, op1), r, w)

    def TT(eng, out_, in0, in1, op, r=(), w=()):
        return S.add(eng, lambda e: e.tensor_tensor(out_, in0, in1, op), r, w)

    def STT(out_, in0, scalar, in1, op0, op1, r=(), w=(), accum_out=None):
        if accum_out is None:
            return S.add("dve", lambda e: e.scalar_tensor_tensor(out_, in0, scalar, in1, op0, op1), r, w)
        return S.add("dve", lambda e: e.scalar_tensor_tensor(out_, in0, scalar, in1, op0, op1, accum_out), r, w)

    def CP(eng, out_, in_, r=(), w=()):
        if eng == "act":
            return S.add("act", lambda e: e.activation(out_, in_, AF.Copy), r, w)
        return S.add(eng, lambda e: e.tensor_copy(out_, in_), r, w)

    def MEMSET(eng, ap, val, w=()):
        return S.add(eng, lambda e: e.memset(ap, val), (), w)

    def DMA(q, out_, in_, r=(), w=(), slow=False):
        if slow:
            return S.add(q, lambda e: e.dma_start(out=out_, in_=in_, allow_slow_non_contiguous=True), r, w, dma=True)
        return S.add(q, lambda e: e.dma_start(out=out_, in_=in_), r, w, dma=True)

    uid = [0]

    class Pool_:
        def __init__(self, es, name, n, shape, dt, psum=False):
            self.t = []
            uid[0] += 1
            for i in range(n):
                nm = "%s_%d_u%d" % (name, i, uid[0])
                if psum:
                    self.t.append((es.enter_context(nc.psum_tensor(nm, shape, dt)), nm))
                else:
                    self.t.append((es.enter_context(nc.sbuf_tensor(nm, shape, dt)), nm))
            self.i = 0

        def next(self):
            t = self.t[self.i % len(self.t)]
            self.i += 1
            return t

    def sb(es, name, shape, dt):
        uid[0] += 1
        return es.enter_context(nc.sbuf_tensor("%s_u%d" % (name, uid[0]), shape, dt))

    cstf = sb(es0, "cstf", [128, 8, 128], F32)
    cstb = sb(es0, "cstb", [128, 8, 128], BF16)
    ioe = sb(es0, "ioe", [128, 32], F32)
    Sst = sb(es0, "Sst", [128, 2, 4, 256], F32)
    Pb = sb(es0, "Pb", [128, 4], F32)
    IDX = sb(es0, "IDX", [128, 16, 4], I32)
    GATE = sb(es0, "GATE", [128, 16, 4], F32)
    DMA("sp", cstf[:, :, :], cst, w=["cstf"])
    DMA("sp", ioe[:, :], cst2, w=["ioe"])
    CP("dve", cstb[:, :, :], cstf[:, :, :], r=["cstf"], w=["cstb"])
    MEMSET("dve", Sst[:, :, :, :], 0.0, w=["Sf0", "Sf1", "Sf2", "Sf3", "Sb0", "Sb1", "Sb2", "Sb3"])
    MEMSET("dve", Pb[:, :], 1.0, w=["Pb"])
    identb = cstb[:, 0, :]
    identf = cstf[:, 0, :]
    TIb, TItb, TSb, TPb, LTb, ONEb = (cstb[:, j, :] for j in range(1, 7))
    CIb = cstb[:, 7, 0:2]
    CK = ["cstb"]

    stg_es = ExitStack()
    wst = {"pool": None, "n": 128}

    def new_stage(es, n, cols):
        wst["pool"] = Pool_(es, "wstage", n, [128, 8, cols], F32)
        wst["n"] = cols
    cast_rr = [0]
    es_ht = ExitStack()
    HT_own = sb(es_ht, "HT_own", [128, 8, NOWN], BF16)
    Wkv = sb(es_ht, "Wkv", [128, 8, 1536], BF16)

    def load_w(dst, dkey, wd, c0, ncols, dcol0=0, engs=("pool", "act")):
        PW = wst["n"]
        for p0 in range(0, ncols, PW):
            n = min(PW, ncols - p0)
            st, sk = wst["pool"].next()
            DMA("sp", st[:, :, 0:n], wd[:, c0 + p0:c0 + p0 + n].rearrange("(kc p) n -> p kc n", p=128), w=[sk])
            eng = engs[cast_rr[0] % len(engs)]
            cast_rr[0] += 1
            CP(eng, dst[:, :, dcol0 + p0:dcol0 + p0 + n], st[:, :, 0:n], r=[sk], w=[dkey])

    def load_w_rows(dst, dkey, wd, engs):
        for kc0 in range(0, 8, 2):
            st, sk = wst["pool"].next()
            stv = st[:, :, :].rearrange("p a b -> p (a b)").rearrange("p (k n) -> p k n", k=2)
            DMA("sp", stv, wd[kc0 * 128:(kc0 + 2) * 128, :].rearrange("(kc p) n -> p kc n", p=128), w=[sk])
            eng = engs[cast_rr[0] % len(engs)]
            cast_rr[0] += 1
            CP(eng, dst[:, kc0:kc0 + 2, :], stv, r=[sk], w=[dkey])

    def bcast_load(es, name, vec, n):
        t = sb(es, name, [128, n], F32)
        DMA("sp", t[:, :], vec.partition_broadcast(128), w=[name])
        return t

    def layernorm(es_tmp, pref, x_t, xkey, g_t, gkey, b_t, bkey, out_t, okey, small):
        st, stk = small.next()
        S.add("dve", lambda e: e.bn_stats(st[:, 0:6], x_t[:, 0:512]), [xkey], [stk])
        S.add("dve", lambda e: e.bn_stats(st[:, 6:12], x_t[:, 512:1024]), [xkey], [stk])
        S.add("dve", lambda e: e.bn_aggr(st[:, 12:14], st[:, 0:12]), [stk], [stk])
        ACT(st[:, 14:15], st[:, 13:14], AF.Ln, r=[stk], w=[stk], bias=EPS)
        ACT(st[:, 15:16], st[:, 14:15], AF.Exp, r=[stk], w=[stk], scale=-0.5)
        TS_("dve", out_t[:, :], x_t[:, :], st[:, 12:13], st[:, 15:16], ALU.subtract, ALU.mult, r=[xkey, stk], w=[okey])
        TT("dve", out_t[:, :], out_t[:, :], g_t[:, :], ALU.mult, r=[okey, gkey], w=[okey])
        TT("dve", out_t[:, :], out_t[:, :], b_t[:, :], ALU.add, r=[okey, bkey], w=[okey])

    NW1 = 3072
    with ExitStack() as es:
        new_stage(es, 2, 128)
        W1 = sb(es, "W1", [128, 8, NW1], BF16)
        Wlr = sb(es, "Wlr", [128, 8, 32], BF16)
        wfs = sb(es, "wfs", [17, 2, 512], F32)
        wfa = sb(es, "wfa", [17, 2, 512], BF16)
        Lt = sb(es, "Lt", [32, 2, 128], BF16)
        g0t = bcast_load(es, "g0t", ln0_g, D)
        b0t = bcast_load(es, "b0t", ln0_b, D)
        load_w(W1, "W1", w_in, C_KD, 1024, 0)
        load_w(W1, "W1", w_in, C_VD, 1024, 1024)
        load_w(W1, "W1", w_in, C_QD, 1024, 2048)
        load_w(Wkv, "Wkv", w_in, C_KG, 512, 0)
        load_w(Wkv, "Wkv", w_in, C_VG, 1024, 512)
        load_w(Wlr, "Wlr", w_in, C_LRF, 32, 0)
        DMA("sp", wfs[0:16, 0, :], wf_f, w=["wfs"])
        DMA("sp", wfs[16:17, 0, :], bf_f.rearrange("(a n) -> a n", a=1), w=["wfs"])
        DMA("sp", wfs[0:16, 1, :], wf_b, w=["wfs"])
        DMA("sp", wfs[16:17, 1, :], bf_b.rearrange("(a n) -> a n", a=1), w=["wfs"])
        CP("dve", wfa[:, :, :], wfs[:, :, :], r=["wfs"], w=["wfa"])
        MEMSET("dve", Lt[:, :, :], 1.0, w=["Lt0", "Lt1"])

        xr = Pool_(es, "xr", 2, [128, D], F32)
        csr = Pool_(es, "csr", 2, [128, 130], F32)
        small = Pool_(es, "small", 3, [128, 16], F32)
        hfr = Pool_(es, "hfr", 2, [128, D], F32)
        hbr = Pool_(es, "hbr", 2, [128, D], BF16)
        hTr = Pool_(es, "hTr", 2, [128, 8, 128], BF16)
        kbr = Pool_(es, "kbr", 2, [128, D], BF16)
        ktr = Pool_(es, "ktr", 2, [128, 8, 128], BF16)
        vbr = Pool_(es, "vbr", 2, [128, D], BF16)
        ropet = Pool_(es, "ropet", 2, [128, 4, 8, 8], F32)
        kgr = Pool_(es, "kgr", 2, [128, 512], BF16)
        vgr = Pool_(es, "vgr", 2, [128, D], BF16)
        f32r = Pool_(es, "f32r", 4, [128, 512], F32)
        lfr = Pool_(es, "lfr", 4, [128, 512], BF16)
        kstr = Pool_(es, "kstr", 4, [128, 512], BF16)
        decr = Pool_(es, "decr", 2, [128, 16], F32)
        nmr = Pool_(es, "nmr", 2, [128, 2], F32)
        ptp = Pool_(es, "ptp", 1, [128, 512], F32, psum=True)
        ppj = Pool_(es, "ppj", 2, [128, 512], F32, psum=True)
        pg = Pool_(es, "pg", 2, [128, 512], F32, psum=True)
        pu = Pool_(es, "pu", 2, [128, 512], F32, psum=True)
        psm = Pool_(es, "psm", 1, [128, 512], F32, psum=True)
        uslot = [0]

        def rope_and_store(pj, pjk, kb, kbk, cs, csk, half):
            dst = kb[:, half * 512:(half + 1) * 512]
            CP("act", dst, pj[:, :], r=[pjk], w=[kbk])
            if NO_ROPE:
                return
            d3 = dst.rearrange("p (g d) -> p g d", d=64)
            x1 = d3[:, :, 0:8]; x2 = d3[:, :, 8:16]
            cosb = cs[:, 0:64].rearrange("p (g d) -> p g d", d=8)
            sinb = cs[:, 64:128].rearrange("p (g d) -> p g d", d=8)
            rt, rk = ropet.next()
            TT("dve", rt[:, 0], x1, cosb, ALU.mult, r=[kbk, csk], w=[rk])
            TT("dve", rt[:, 1], x2, sinb, ALU.mult, r=[kbk, csk], w=[rk])
            TT("dve", rt[:, 2], x2, cosb, ALU.mult, r=[kbk, csk], w=[rk])
            TT("dve", rt[:, 3], x1, sinb, ALU.mult, r=[kbk, csk], w=[rk])
            TT("dve", d3[:, :, 0:8], rt[:, 0], rt[:, 1], ALU.subtract, r=[rk], w=[kbk])
            TT("dve", d3[:, :, 8:16], rt[:, 2], rt[:, 3], ALU.add, r=[rk], w=[kbk])

        pend = [iter(())]

        def proj_tok(hT, hTk, W, wkey, c0, pj, pjk):
            for kc in range(8):
                MM(pj[:, :], hT[:, kc, :], W[:, kc, c0:c0 + 512], start=(kc == 0), stop=(kc == 7),
                   r=[hTk, wkey], w=[pjk])
            next(pend[0], None)
            next(pend[0], None)

        def p1A(i):
                own = i >= 48
                xt, xk = xr.next()
                cs, csk = csr.next()
                DMA("sp", xt[:, :], xa[i * 128:(i + 1) * 128, :], w=[xk])
                DMA("sp", cs[:, :], csa[i * 128:(i + 1) * 128, :], w=[csk])
                if P1_LEVEL < 2:
                    return None
                hf, hfk = hfr.next()
                layernorm(es, "ln0", xt, xk, g0t, "g0t", b0t, "b0t", hf, hfk, small)
                hb, hbk = hbr.next()
                CP("act", hb[:, :], hf[:, :], r=[hfk], w=[hbk])
                if own:
                    DMA(STORE_Q, H_d[(i - 48) * 128:(i - 47) * 128, :], hf[:, :], r=[hfk], w=["H_d"])
                tp, tpk = ptp.next()
                tpb = tp[:, :].bitcast(BF16)
                for kc in range(8):
                    TR(tpb[:, kc * 128:(kc + 1) * 128], hb[:, kc * 128:(kc + 1) * 128], identb, r=[hbk] + CK, w=[tpk])
                hT, hTk = hTr.next()
                CP("act", hT[:, :, :], tpb[:, 0:1024].rearrange("p (k t) -> p k t", k=8), r=[tpk], w=[hTk])
                if own:
                    CP("pool", HT_own[:, :, (i - 48) * 128:(i - 47) * 128], hT[:, :, :], r=[hTk], w=["HT_own"])
                return dict(own=own, cs=cs, csk=csk, hT=hT, hTk=hTk)

        def p1B(i, c):
                if c is None:
                    return
                own = c["own"]; cs = c["cs"]; csk = c["csk"]; hT = c["hT"]; hTk = c["hTk"]
                if P1_LEVEL < 3:
                    return
                gla = (not own) and P1_LEVEL >= 4
                if gla:
                    kg, kgk = kgr.next()
                    pj, pjk = ppj.next()
                    proj_tok(hT, hTk, Wkv, "Wkv", 0, pj, pjk)
                    CP("act", kg[:, :], pj[:, :], r=[pjk], w=[kgk])
                    nm, nmk = nmr.next()
                    TS_("dve", nm[:, :], cs[:, 128:130], -1.0 / 16.0, None, ALU.mult, r=[csk], w=[nmk])
                    sm, smk = psm.next()
                    for d in range(2):
                        for kc in range(8):
                            MM(sm[0:16, d * 128:(d + 1) * 128], Wlr[:, kc, d * 16:(d + 1) * 16], hT[:, kc, :],
                               start=(kc == 0), stop=(kc == 7), r=["Wlr", hTk], w=[smk])
                    for d in range(2):
                        CP("act", Lt[0:16, d, :], sm[0:16, d * 128:(d + 1) * 128], r=[smk], w=["Lt%d" % d])
                    e1s = []
                    for d in range(2):
                        g1, g1k = pg.next()
                        MM(g1[:, :], Lt[0:17, d, :], wfa[0:17, d, :], r=["Lt%d" % d, "wfa"], w=[g1k])
                        e1, e1k = f32r.next()
                        ACT(e1[:, :], g1[:, :], AF.Exp, r=[g1k], w=[e1k], scale=-1.0)
                        e1s.append((e1, e1k))
                    lfs = []
                    for d in range(2):
                        e1, e1k = e1s[d]
                        ACT(e1[:, :], e1[:, :], AF.Ln, r=[e1k], w=[e1k], bias=1.0)
                        lf, lfk = lfr.next()
                        TS_("dve", lf[:, :], e1[:, :], nm[:, d:d + 1], None, ALU.mult, r=[e1k, nmk], w=[lfk])
                        lfs.append((lf, lfk))
                    vg, vgk = vgr.next()
                    for half in range(2):
                        pj, pjk = ppj.next()
                        proj_tok(hT, hTk, Wkv, "Wkv", 512 + half * 512, pj, pjk)
                        CP("act", vg[:, half * 512:(half + 1) * 512], pj[:, :], r=[pjk], w=[vgk])
                kb, kbk = kbr.next()
                for half in range(2):
                    pj, pjk = ppj.next()
                    proj_tok(hT, hTk, W1, "W1", half * 512, pj, pjk)
                    rope_and_store(pj, pjk, kb, kbk, cs, csk, half)
                if gla:
                    ksts = []
                    e2s = []
                    for d in range(2):
                        lf, lfk = lfs[d]
                        g2, g2k = pg.next()
                        MM(g2[:, :], (TSb if d == 0 else TPb), lf[:, :], r=CK + [lfk], w=[g2k])
                        e2, e2k = f32r.next()
                        ACT(e2[:, :], g2[:, :], AF.Exp, r=[g2k], w=[e2k])
                        e2s.append((e2, e2k))
                    for d in range(2):
                        e2, e2k = e2s[d]
                        kst, kstk = kstr.next()
                        STT(kst[:, :], e2[:, :], cs[:, 128 + d:129 + d], kg[:, :], ALU.mult, ALU.mult, r=[e2k, csk, kgk], w=[kstk])
                        ksts.append((kst, kstk))
                tp, tpk = ptp.next()
                tpb = tp[:, :].bitcast(BF16)
                for h in range(8):
                    TR(tpb[:, h * 128:(h + 1) * 128], kb[:, h * 128:(h + 1) * 128], identb, r=[kbk] + CK, w=[tpk])
                kt, ktk = ktr.next()
                CP("dve", kt[:, :, :], tpb[:, 0:1024].rearrange("p (k t) -> p k t", k=8), r=[tpk], w=[ktk])
                DMA(STORE_Q, KT_d[:, :, i * 128:(i + 1) * 128].rearrange("h p t -> p h t"), kt[:, :, :], r=[ktk], w=["KT_d"])
                vb, vbk = vbr.next()
                for half in range(2):
                    pj, pjk = ppj.next()
                    proj_tok(hT, hTk, W1, "W1", 1024 + half * 512, pj, pjk)
                    CP("act", vb[:, half * 512:(half + 1) * 512], pj[:, :], r=[pjk], w=[vbk])
                DMA(STORE_Q, V_d[i * 128:(i + 1) * 128, :], vb[:, :], r=[vbk], w=["V_d"])
                if own:
                    kb, kbk = kbr.next()
                    for half in range(2):
                        pj, pjk = ppj.next()
                        proj_tok(hT, hTk, W1, "W1", 2048 + half * 512, pj, pjk)
                        rope_and_store(pj, pjk, kb, kbk, cs, csk, half)
                    tp, tpk = ptp.next()
                    tpb = tp[:, :].bitcast(BF16)
                    for h in range(8):
                        TR(tpb[:, h * 128:(h + 1) * 128], kb[:, h * 128:(h + 1) * 128], identb, r=[kbk] + CK, w=[tpk])
                    kt, ktk = ktr.next()
                    CP("dve", kt[:, :, :], tpb[:, 0:1024].rearrange("p (k t) -> p k t", k=8), r=[tpk], w=[ktk])
                    DMA(STORE_Q, QT_d[:, :, (i - 48) * 128:(i - 47) * 128].rearrange("h p t -> p h t"), kt[:, :, :],
                        r=[ktk], w=["QT_d"])
                    return
                if not gla:
                    return
                for d in range(2):
                    lf, lfk = lfs[d]
                    for h in range(4):
                        col = 256 + d * 4 + h
                        MM(sm[:, col:col + 1], lf[:, h * 128:(h + 1) * 128], ONEb[:, 0:1], r=[lfk] + CK, w=[smk])
                dec, deck = decr.next()
                ACT(dec[:, 0:8], sm[:, 256:264], AF.Exp, r=[smk], w=[deck])

                def cgen(ksts=ksts, vg=vg, vgk=vgk, dec=dec, deck=deck):
                    for d in range(2):
                        kst, kstk = ksts[d]
                        for h in range(4):
                            ub, ubk = pu.next()
                            uap = ub[:, 0:256]
                            MM(uap, kst[:, h * 128:(h + 1) * 128], vg[:, h * 256:(h + 1) * 256], r=[kstk, vgk], w=[ubk])
                            if d == 0:
                                sk_ = "Sf%d" % h
                                STT(Sst[:, 0, h, :], Sst[:, 0, h, :], dec[:, h:h + 1], uap, ALU.mult, ALU.add,
                                    r=[sk_, deck, ubk], w=[sk_])
                            else:
                                sk_ = "Sb%d" % h
                                STT(Sst[:, 1, h, :], uap, Pb[:, h:h + 1], Sst[:, 1, h, :], ALU.mult, ALU.add,
                                    r=[sk_, "Pb", ubk], w=[sk_])
                            yield
                    TT("dve", Pb[:, :], Pb[:, :], dec[:, 4:8], ALU.mult, r=["Pb", deck], w=["Pb"])
                    yield
                for _ in pend[0]:
                    pass
                pend[0] = cgen()

        p1tiles = (P1_TILES if P1_LEVEL >= 1 else [])
        ctx = p1A(p1tiles[0]) if p1tiles else None
        for n_, i in enumerate(p1tiles):
            nxt = p1A(p1tiles[n_ + 1]) if n_ + 1 < len(p1tiles) else None
            p1B(i, ctx)
            ctx = nxt
        for _ in pend[0]:
            pass
        if stop == 1:
            DMA("sp", out[0:128, 0:1024].rearrange("p (a b c) -> p a b c", a=2, b=4), Sst[:, :, :, 0:128], r=["Sf0", "Sf1", "Sf2", "Sf3", "Sb0", "Sb1", "Sb2", "Sb3"], w=["dbg"])
            S.waitall("sp", ["dbg", "KT_d", "V_d", "QT_d", "H_d"])
        S.flush()
    if stop == 1:
        return nc

    NW2 = 2048
    with ExitStack() as es:
        for d in range(2):
            if d == 0:
                new_stage(es, 2, 128)
                DMA(STORE_Q, HT_d[:, 0:OWN_N * 128].rearrange("(kc p) t -> p kc t", p=128), HT_own[:, :, 0:OWN_N * 128], r=["HT_own"], w=["HT_d"])
                Wq = sb(es, "Wq", [128, 8, 512], BF16)
                load_w(Wq, "Wq", w_in, C_QG, 512, 0)
                Wrg = sb(es, "Wrg", [128, 8, 1024], BF16)
                load_w(Wrg, "Wrg", w_in, C_RG, 1024, 0, engs=("pool",))
                ggt = bcast_load(es, "ggt", gla_g, 256)
            Wl2 = sb(es, "Wl2_%d" % d, [128, 8, 16], BF16)
            wfs2 = sb(es, "wfs2_%d" % d, [17, 512], F32)
            wfa2 = sb(es, "wfa2_%d" % d, [17, 512], BF16)
            Lt2 = sb(es, "Lt2_%d" % d, [32, 128], BF16)
            mask4 = sb(es, "mask4_%d" % d, [128, 4, 128], F32)
            load_w(Wl2, "Wl2", w_in, C_LRF if d == 0 else C_LRB, 16, 0)
            DMA("sp", wfs2[0:16, :], wf_f if d == 0 else wf_b, w=["wfs2"])
            DMA("sp", wfs2[16:17, :], (bf_f if d == 0 else bf_b).rearrange("(a n) -> a n", a=1), w=["wfs2"])
            CP("dve", wfa2[:, :], wfs2[:, :], r=["wfs2"], w=["wfa2"])
            MEMSET("dve", Lt2[:, :], 1.0, w=["Lt2"])
            for h in range(4):
                CP("dve", mask4[:, h, :], cstf[:, 1 if d == 0 else 2, :], r=["cstf"], w=["mask4"])
            if d == 0:
                ofr = Pool_(es, "ofr", 2, [128, D], F32)
                osr = Pool_(es, "osr", 2, [128, D], F32)
                abr = Pool_(es, "abr", 2, [128, D], BF16)
                atr = Pool_(es, "atr", 2, [128, 8, 128], BF16)
                srr = Pool_(es, "srr", 2, [128, 512], F32)
                small = Pool_(es, "small3", 2, [128, 16], F32)
                kgr = Pool_(es, "kgr2", 2, [128, 512], BF16)
                vgr = Pool_(es, "vgr2", 2, [128, D], BF16)
                f32r = Pool_(es, "f32r2", 6, [128, 512], F32)
                lfr = Pool_(es, "lfr2", 2, [128, 512], BF16)
                kstr = Pool_(es, "kstr2", 2, [128, 512], BF16)
                eqr = Pool_(es, "eqr", 3, [128, 512], F32)
                qinr = Pool_(es, "qinr", 2, [128, 512], BF16)
                kinr = Pool_(es, "kinr", 2, [128, 512], BF16)
                attr = Pool_(es, "attr", 2, [128, 512], BF16)
                ppj = Pool_(es, "ppj2", 1, [128, 512], F32, psum=True)
                pg = Pool_(es, "pg2", 3, [128, 512], F32, psum=True)
                pu = Pool_(es, "pu2", 2, [128, 512], F32, psum=True)
                po = Pool_(es, "po2", 2, [128, 512], F32, psum=True)
            Sbf = [Pool_(es, "Sbf%d_%d" % (h, d), 2, [128, 256], BF16) for h in range(4)]
            uslot = [0]
            oslot = [0]
            skey = ["S%s%d" % ("f" if d == 0 else "b", h) for h in range(4)]
            cur = []
            for h in range(4):
                t, tk = Sbf[h].next()
                CP("pool", t[:, :], Sst[:, d, h, :], r=[skey[h]], w=[tk])
                cur.append((t, tk))
            tiles = list(range(OWN_N)) if d == 0 else list(range(OWN_N - 1, -1, -1))
            chunks = (0,)
            def stageA(i):
                    hT = HT_own[:, :, i * 128:(i + 1) * 128]
                    hTk = "HT_own"
                    kg, kgk = kgr.next()
                    pj, pjk = ppj.next()
                    for kc in range(8):
                        MM(pj[:, :], hT[:, kc, :], Wkv[:, kc, 0:512], start=(kc == 0), stop=(kc == 7), r=["Wkv", hTk], w=[pjk])
                    CP("act", kg[:, :], pj[:, :], r=[pjk], w=[kgk])
                    vg, vgk = vgr.next()
                    for half in range(2):
                        pj, pjk = ppj.next()
                        for kc in range(8):
                            MM(pj[:, :], hT[:, kc, :], Wkv[:, kc, 512 + half * 512:1024 + half * 512],
                               start=(kc == 0), stop=(kc == 7), r=["Wkv", hTk], w=[pjk])
                        CP("act", vg[:, half * 512:(half + 1) * 512], pj[:, :], r=[pjk], w=[vgk])
                    g1, g1k = pg.next()
                    for kc in range(8):
                        MM(g1[0:16, 0:128], Wl2[:, kc, 0:16], hT[:, kc, :], start=(kc == 0), stop=(kc == 7),
                           r=["Wl2", hTk], w=[g1k])
                    CP("act", Lt2[0:16, :], g1[0:16, 0:128], r=[g1k], w=["Lt2"])
                    MM(g1[:, :], Lt2[0:17, :], wfa2[0:17, :], r=["Lt2", "wfa2"], w=[g1k])
                    e1, e1k = f32r.next()
                    ACT(e1[:, :], g1[:, :], AF.Exp, r=[g1k], w=[e1k], scale=-1.0)
                    ACT(e1[:, :], e1[:, :], AF.Ln, r=[e1k], w=[e1k], bias=1.0)
                    lf, lfk = lfr.next()
                    TS_("dve", lf[:, :], e1[:, :], -1.0 / 16.0, None, ALU.mult, r=[e1k], w=[lfk])
                    gb, gbk = pg.next()
                    for h in range(4):
                        MM(gb[:, h * 128:(h + 1) * 128], lf[:, h * 128:(h + 1) * 128], (TIb if d == 0 else TItb),
                           r=[lfk] + CK, w=[gbk])
                    Eq, Eqk = eqr.next()
                    Ek, Ekk = f32r.next()
                    ACT(Eq[:, :], gb[:, :], AF.Exp, r=[gbk], w=[Eqk])
                    ACT(Ek[:, :], gb[:, :], AF.Exp, r=[gbk], w=[Ekk], scale=-1.0)
                    g2, g2k = pg.next()
                    MM(g2[:, :], (TSb if d == 0 else TPb), lf[:, :], r=CK + [lfk], w=[g2k])
                    e2, e2k = f32r.next()
                    ACT(e2[:, :], g2[:, :], AF.Exp, r=[g2k], w=[e2k])
                    kst, kstk = kstr.next()
                    TT("dve", kst[:, :], e2[:, :], kg[:, :], ALU.mult, r=[e2k, kgk], w=[kstk])
                    gq, gqk = pg.next()
                    gk, gkk = pg.next()
                    for h in range(4):
                        for kc in range(8):
                            MM(gq[:, h * 128:(h + 1) * 128], Wq[:, kc, h * 128:(h + 1) * 128], hT[:, kc, :],
                               start=(kc == 0), stop=(kc == 7), r=["Wq", hTk], w=[gqk])
                    for h in range(4):
                        for kc in range(8):
                            MM(gk[:, h * 128:(h + 1) * 128], Wkv[:, kc, h * 128:(h + 1) * 128], hT[:, kc, :],
                               start=(kc == 0), stop=(kc == 7), r=["Wkv", hTk], w=[gkk])
                    qin, qink = qinr.next()
                    kin, kink = kinr.next()
                    STT(qin[:, :], gq[:, :], 128.0 ** -0.5, Eq[:, :], ALU.mult, ALU.mult, r=[gqk, Eqk], w=[qink])
                    TT("dve", kin[:, :], gk[:, :], Ek[:, :], ALU.mult, r=[gkk, Ekk], w=[kink])
                    ga, gak = pg.next()
                    for h in range(4):
                        MM(ga[:, h * 128:(h + 1) * 128], kin[:, h * 128:(h + 1) * 128], qin[:, h * 128:(h + 1) * 128],
                           r=[kink, qink], w=[gak])
                    att, attk = attr.next()
                    TT("dve", att[:, :], ga[:, :], mask4[:, :, :].rearrange("p h t -> p (h t)"), ALU.mult,
                       r=[gak, "mask4"], w=[attk])
                    if d == 1:
                        of, ofk = ofr.next()
                        DMA("sp", of[:, :], OF_d[i * 128:(i + 1) * 128, :], r=["OF_d"], w=[ofk])
                        osum, osk = osr.next()
                    else:
                        of, ofk = ofr.next()
                    return dict(hT=hT, hTk=hTk, vg=vg, vgk=vgk, kst=kst, kstk=kstk, qin=qin, qink=qink, att=att, attk=attk,
                                Eq=Eq, Eqk=Eqk, of=of, ofk=ofk, osum=(osum if d == 1 else None), osk=(osk if d == 1 else None))

            def stageB(i, c):
                    hT = c["hT"]; hTk = c["hTk"]; vg = c["vg"]; vgk = c["vgk"]; kst = c["kst"]; kstk = c["kstk"]
                    qin = c["qin"]; qink = c["qink"]; att = c["att"]; attk = c["attk"]; Eq = c["Eq"]; Eqk = c["Eqk"]
                    of = c["of"]; ofk = c["ofk"]; osum = c["osum"]; osk = c["osk"]
                    obs = []
                    for h in range(4):
                        ob_, obk_ = po.t[h % 2]
                        c0 = (h // 2) * 256
                        obs.append((ob_, obk_, c0))
                        MM(ob_[:, c0:c0 + 256], att[:, h * 128:(h + 1) * 128], vg[:, h * 256:(h + 1) * 256],
                           start=(h < 2), stop=False, r=[attk, vgk], w=[obk_])
                    for ci, ch in enumerate(chunks):
                        r0 = ch * 64
                        for h in range(4):
                            ob_, obk_, c0 = obs[h]
                            st_bf, st_k = cur[h]
                            MM(ob_[:, c0:c0 + 256], qin[:, h * 128:(h + 1) * 128], st_bf[:, :],
                               start=False, stop=True, r=[qink, st_k], w=[obk_])
                        for h in range(4):
                            ub, ubk = pu.t[h % 2]
                            uc0 = (h // 2) * 256
                            uap = ub[:, uc0:uc0 + 256]
                            MM(uap, kst[:, h * 128:(h + 1) * 128], vg[:, h * 256:(h + 1) * 256],
                               r=[kstk, vgk], w=[ubk])
                            if d == 0:
                                dcol = h * 128 + 127
                            else:
                                dcol = h * 128
                            STT(Sst[:, d, h, :], Sst[:, d, h, :], Eq[:, dcol:dcol + 1], uap, ALU.mult, ALU.add,
                                r=[skey[h], Eqk, ubk], w=[skey[h]])
                            t, tk = Sbf[h].next()
                            CP("pool", t[:, :], Sst[:, d, h, :], r=[skey[h]], w=[tk])
                            cur[h] = (t, tk)
                    for h in range(4):
                        ob_, obk_, c0 = obs[h]
                        ob = ob_[:, c0:c0 + 256]
                        if d == 0:
                            CP("act", of[:, h * 256:(h + 1) * 256], ob, r=[obk_], w=[ofk])
                        else:
                            TT("dve", osum[:, h * 256:(h + 1) * 256], ob, of[:, h * 256:(h + 1) * 256], ALU.add,
                               r=[obk_, ofk], w=[osk])
                    if d == 0:
                        DMA(STORE_Q, OF_d[i * 128:(i + 1) * 128, :], of[:, :], r=[ofk], w=["OF_d"])
                        return
                    st, stk = small.next()
                    junk, junkk = f32r.next()
                    for h in range(4):
                        STT(junk[:, 0:256], osum[:, h * 256:(h + 1) * 256], 1.0, osum[:, h * 256:(h + 1) * 256],
                            ALU.mult, ALU.mult, r=[osk], w=[junkk, stk], accum_out=st[:, h:h + 1])
                    ACT(st[:, 4:8], st[:, 0:4], AF.Ln, r=[stk], w=[stk], bias=EPS, scale=1.0 / 256.0)
                    ACT(st[:, 8:12], st[:, 4:8], AF.Exp, r=[stk], w=[stk], scale=-0.5)
                    for h in range(4):
                        STT(osum[:, h * 256:(h + 1) * 256], osum[:, h * 256:(h + 1) * 256], st[:, 8 + h:9 + h], ggt[:, :],
                            ALU.mult, ALU.mult, r=[osk, stk, "ggt"], w=[osk])
                    ab, abk = abr.next()
                    for half in range(2):
                        pj, pjk = ppj.next()
                        for kc in range(8):
                            MM(pj[:, :], hT[:, kc, :], Wrg[:, kc, half * 512:(half + 1) * 512], start=(kc == 0), stop=(kc == 7),
                               r=["Wrg", hTk], w=[pjk])
                        sr, srk = srr.next()
                        ACT(sr[:, :], pj[:, :], AF.Silu, r=[pjk], w=[srk])
                        TT("dve", ab[:, half * 512:(half + 1) * 512], osum[:, half * 512:(half + 1) * 512], sr[:, :], ALU.mult,
                           r=[osk, srk], w=[abk])
                    tp, tpk = pg.next()
                    tpb = tp[:, :].bitcast(BF16)
                    for kc in range(8):
                        TR(tpb[:, kc * 128:(kc + 1) * 128], ab[:, kc * 128:(kc + 1) * 128], identb, r=[abk] + CK, w=[tpk])
                    at, atk = atr.next()
                    CP("act", at[:, :, :], tpb[:, 0:1024].rearrange("p (k t) -> p k t", k=8), r=[tpk], w=[atk])
                    DMA(STORE_Q, AT_d[:, i * 128:(i + 1) * 128].rearrange("(kc p) t -> p kc t", p=128), at[:, :, :],
                        r=[atk], w=["AT_d"])

            ctx = stageA(tiles[0])
            for n_, i in enumerate(tiles):
                nxt = stageA(tiles[n_ + 1]) if n_ + 1 < len(tiles) else None
                stageB(i, ctx)
                ctx = nxt
        if stop in (2, 3):
            S.waitall("sp", ["OF_d", "AT_d", "HT_d"])
        S.flush()
    if stop in (2, 3):
        return nc
    es_ht.close()

    es5 = ExitStack()
    stage5 = Pool_(es5, "wstage5", 2, [128, 8, 128], F32)
    Wa = sb(es5, "Wa", [128, 8, D], BF16)
    Wb = sb(es5, "Wb", [128, 8, D], BF16)
    Wo = sb(es5, "Wo", [128, 8, D], BF16)
    Wgg = sb(es5, "Wgg", [128, 8, D], BF16)
    Wgd = sb(es5, "Wgd", [128, 8, D], BF16)

    def prefetch_p5():
        wst["pool"] = stage5
        wst["n"] = 128
        load_w(Wa, "Wa", w_bra, 0, D, engs=("pool", "dve"))
        load_w(Wb, "Wb", w_brb, 0, D, engs=("pool", "dve"))
        load_w(Wo, "Wo", w_out, 0, D, engs=("pool", "dve"))
        load_w(Wgg, "Wgg", w_in, C_GG, D, engs=("pool", "dve"))
        load_w(Wgd, "Wgd", w_in, C_GD, D, engs=("pool", "dve"))

    with ExitStack() as es:
        ktp = Pool_(es, "ktp", 2, [128, NALL], BF16)
        vtp = Pool_(es, "vtp", 2, [128, 64, 129], BF16)
        qtp = Pool_(es, "qtp", 2, [128, NOWN], BF16)
        ptr = [Pool_(es, "ptr%d" % m, 3, [128, 512], BF16) for m in range(2)]
        bthr = Pool_(es, "bthr", 2, [128, NOWN], BF16)
        lv = sb(es, "lv", [128, 4, 64], F32)
        lsm = sb(es, "lsm", [128, 16], F32)
        gdt = bcast_load(es, "gdt", diff_g, 128)
        fin = Pool_(es, "fin", 8, [128, 16], F32)
        t1r = Pool_(es, "t1r", 2, [128, 128], F32)
        o1r = Pool_(es, "o1r", 8, [128, 128], F32)
        jr = Pool_(es, "jr", 2, [128, 128], F32)
        bbr = Pool_(es, "bbr", 2, [128, 128], BF16)
        accs = Pool_(es, "accs", 6, [128, 388], F32)
        pst = [Pool_(es, "pst%d" % m, 2, [128, 512], F32, psum=True) for m in range(2)]
        pacc = Pool_(es, "pacc", 3, [128, 512], F32, psum=True)
        ptp = Pool_(es, "ptp4", 1, [128, 512], F32, psum=True)
        for j, v in enumerate((lq1, lk1, lq2, lk2)):
            DMA("sp", lv[:, j, :], v.partition_broadcast(128), w=["lv"])
        junk, junkk = jr.next()
        STT(junk[:, 0:64], lv[:, 0, :], 1.0, lv[:, 1, :], ALU.mult, ALU.mult, r=["lv"], w=[junkk, "lsm"], accum_out=lsm[:, 0:1])
        STT(junk[:, 0:64], lv[:, 2, :], 1.0, lv[:, 3, :], ALU.mult, ALU.mult, r=["lv"], w=[junkk, "lsm"], accum_out=lsm[:, 1:2])
        ACT(lsm[:, 2:4], lsm[:, 0:2], AF.Exp, r=["lsm"], w=["lsm"])
        TT("dve", lsm[:, 4:5], lsm[:, 3:4], lsm[:, 2:3], ALU.subtract, r=["lsm"], w=["lsm"])
        TS_("dve", lsm[:, 5:6], lsm[:, 4:5], -LAM_INIT, None, ALU.add, r=["lsm"], w=["lsm"])
        TS_("dve", gdt[:, :], gdt[:, :], 1.0 - LAM_INIT, None, ALU.mult, r=["gdt"], w=["gdt"])
        for t, tk in vtp.t:
            MEMSET("pool", t[:, :, 128:129], 1.0, w=[tk + "_q%d" % q4 for q4 in range(4)])
        accpos = {}
        n = 0
        for j in range(4):
            for m in range(2):
                accpos[(j, m)] = (n // 3, (n % 3) * 129)
                n += 1
        deferred = [None, None]

        def make_finalize(h, qb, sacc, bth, bthk, last):
            st = {}

            def part1():
                for j in range(4):
                    b0_, c00 = accpos[(j, 0)]
                    b1_, c01 = accpos[(j, 1)]
                    a0, a0k = sacc[b0_]
                    a1, a1k = sacc[b1_]
                    f, fk = fin.next()
                    S.add("dve", lambda e, f=f, a0=a0, c00=c00: e.reciprocal(f[:, 0:1], a0[:, c00 + 128:c00 + 129]), [a0k], [fk])
                    S.add("dve", lambda e, f=f, a1=a1, c01=c01: e.reciprocal(f[:, 1:2], a1[:, c01 + 128:c01 + 129]), [a1k], [fk])
                    TT("dve", f[:, 2:3], f[:, 1:2], lsm[:, 5:6], ALU.mult, r=[fk, "lsm"], w=[fk])
                    t1, t1k = t1r.next()
                    TS_("dve", t1[:, :], a1[:, c01:c01 + 128], f[:, 2:3], None, ALU.mult, r=[a1k, fk], w=[t1k])
                    o1, o1k = o1r.next()
                    STT(o1[:, :], a0[:, c00:c00 + 128], f[:, 0:1], t1[:, :], ALU.mult, ALU.add, r=[a0k, fk, t1k], w=[o1k])
                    junk, junkk = jr.next()
                    STT(junk[:, :], o1[:, :], 1.0, o1[:, :], ALU.mult, ALU.mult, r=[o1k], w=[junkk, fk], accum_out=f[:, 3:4])
                    st[j] = (f, fk, o1, o1k)

            def part2():
                for j in range(4):
                    f, fk, o1, o1k = st[j]
                    ACT(f[:, 4:5], f[:, 3:4], AF.Ln, r=[fk], w=[fk], bias=EPS, scale=1.0 / 128.0)
                    ACT(f[:, 5:6], f[:, 4:5], AF.Exp, r=[fk], w=[fk], scale=-0.5)
                for j in range(4):
                    f, fk, o1, o1k = st[j]
                    bb, bbk = bbr.next()
                    STT(bb[:, :], o1[:, :], f[:, 5:6], gdt[:, :], ALU.mult, ALU.mult, r=[o1k, fk, "gdt"], w=[bbk])
                    tp, tpk = ptp.t[0]
                    tpb = tp[:, :].bitcast(BF16)
                    TR(tpb[:, 0:128], bb[:, :], identb, r=[bbk] + CK, w=[tpk])
                    tok0 = qb * 512 + j * 128
                    CP("dve", bth[:, tok0:tok0 + 128], tpb[:, 0:128], r=[tpk], w=[bthk])
                if last:
                    DMA(STORE_Q, BT_d[h * 128:(h + 1) * 128, 0:P4_QB * 512], bth[:, 0:P4_QB * 512], r=[bthk], w=["BT_d"])
            return part1, part2

        for h in range(P4_H):
            kT, kTk = ktp.next()
            vt, vtk = vtp.next()
            qT, qTk = qtp.next()
            DMA("sp", kT[:, :], KT_d[h, :, :], r=["KT_d"], w=[kTk])
            DMA("sp", qT[:, :], QT_d[h, :, :], r=["QT_d"], w=[qTk])
            for q4 in range(4):
                DMA("sp", vt[:, q4 * 16:(q4 + 1) * 16, 0:128],
                    V_d[q4 * 2048:(q4 + 1) * 2048, h * 128:(h + 1) * 128].rearrange("(kt p) c -> p kt c", p=128),
                    r=["V_d"], w=[vtk + "_q%d" % q4])
            if h == 0:
                prefetch_p5()
            bth, bthk = bthr.next()
            for qb in range(P4_QB):
                banks = [pacc.t[b] for b in range(3)]
                started = [False, False, False]
                pts_of = {}

                def qk_exp(kt_, m):
                    st_, stk_ = pst[m].next()
                    MM(st_[:, :], kT[m * 64:(m + 1) * 64, kt_ * 128:(kt_ + 1) * 128],
                       qT[m * 64:(m + 1) * 64, qb * 512:(qb + 1) * 512], r=[kTk, qTk], w=[stk_])
                    pt, ptk = ptr[m].next()
                    ACT(pt[:, :], st_[:, :], AF.Exp, r=[stk_], w=[ptk], scale=0.125)
                    pts_of[(kt_, m)] = (pt, ptk)

                for k0 in range(2):
                    for m in range(2):
                        qk_exp(k0, m)
                for kt_ in range(P4_KT):
                    if kt_ == 2 and deferred[0] is not None:
                        deferred[0]()
                    if kt_ == 12 and deferred[1] is not None:
                        deferred[1]()
                        deferred[0] = deferred[1] = None
                    for m in range(2):
                        pt, ptk = pts_of.pop((kt_, m))
                        for j in range(4):
                            b, c0 = accpos[(j, m)]
                            bt_, btk_ = banks[b]
                            first = (kt_ == 0 and not started[b])
                            if kt_ == 0:
                                started[b] = True
                            MM(bt_[:, c0:c0 + 129], pt[:, j * 128:(j + 1) * 128], vt[:, kt_, :],
                               start=first, stop=(kt_ == P4_KT - 1), r=[ptk, vtk + "_q%d" % (kt_ // 16)], w=[btk_])
                    if kt_ + 2 < P4_KT:
                        qk_exp(kt_ + 2, 0)
                        qk_exp(kt_ + 2, 1)
                sacc = []
                for b in range(3):
                    a_, ak_ = accs.next()
                    ncol = 387 if b < 2 else 258
                    CP("dve", a_[:, 0:ncol], banks[b][0][:, 0:ncol], r=[banks[b][1]], w=[ak_])
                    sacc.append((a_, ak_))
                p1_, p2_ = make_finalize(h, qb, sacc, bth, bthk, qb == P4_QB - 1)
                deferred[0], deferred[1] = p1_, p2_
        if deferred[0] is not None:
            deferred[0]()
            deferred[1]()
        if stop == 4:
            S.waitall("sp", ["BT_d"])
        S.flush()
    if stop == 4:
        return nc

    with ExitStack() as es:
        Wr = sb(es, "Wr", [128, 8, NE], F32)
        DMA("sp", Wr[:, :, :], router_w.rearrange("(kc p) n -> p kc n", p=128), w=["Wr"])
        g1t = bcast_load(es, "g1t", ln1_g, D)
        b1t = bcast_load(es, "b1t", ln1_b, D)
        rbt = bcast_load(es, "rbt", router_b, NE)
        cum = sb(es, "cum", [128, NE], F32)
        MEMSET("dve", cum[:, :], 0.0, w=["cum"])
        blk = Pool_(es, "blk", 3, [128, 8, 512], BF16)
        mTt = sb(es, "mTt", [128, 8, 512], BF16)
        sgr = Pool_(es, "sgr", 4, [128, 512], F32)
        hr = Pool_(es, "hr5", 2, [128, D], F32)
        rr = Pool_(es, "rr5", 2, [128, D], F32)
        h1r = Pool_(es, "h1r", 2, [128, D], F32)
        h1br = Pool_(es, "h1br", 2, [128, D], BF16)
        h1Tr = Pool_(es, "h1Tr", 2, [128, 8, 128], F32)
        small = Pool_(es, "small5", 3, [128, 16], F32)
        rt = Pool_(es, "rt5", 2, [128, 8, 32], F32)
        mkb = Pool_(es, "mkb5", 2, [128, 32], BF16)
        pm = Pool_(es, "pm5", 4, [128, 512], F32, psum=True)
        pt2 = Pool_(es, "pt5", 2, [128, 512], F32, psum=True)
        pr = Pool_(es, "pr5", 2, [128, 512], F32, psum=True)
        for tb in range(P5_TB):
            at, atk = blk.next()
            bt, btk = blk.next()
            ht, htk = blk.next()
            cols = slice(tb * 512, (tb + 1) * 512)
            DMA("sp", at[:, :, :], AT_d[:, cols].rearrange("(kc p) t -> p kc t", p=128), r=["AT_d"], w=[atk])
            DMA("sp", bt[:, :, :], BT_d[:, cols].rearrange("(kc p) t -> p kc t", p=128), r=["BT_d"], w=[btk])
            DMA("sp", ht[:, :, :], HT_d[:, cols].rearrange("(kc p) t -> p kc t", p=128), r=["HT_d"], w=[htk])
            for oc in range(8):
                ocs = slice(oc * 128, (oc + 1) * 128)
                pa, pak = pm.next(); pga, pgak = pm.next(); pb, pbk = pm.next(); pgb, pgbk = pm.next()
                for (po_, pok_, W_, wk_, x_, xk_) in ((pa, pak, Wa, "Wa", at, atk), (pga, pgak, Wgg, "Wgg", ht, htk),
                                                    (pb, pbk, Wb, "Wb", bt, btk), (pgb, pgbk, Wgd, "Wgd", ht, htk)):
                    for kc in range(8):
                        MM(po_[:, :], W_[:, kc, ocs], x_[:, kc, :], start=(kc == 0), stop=(kc == 7), r=[wk_, xk_], w=[pok_])
                sa, sak = sgr.next(); sb_, sbk = sgr.next()
                ACT(sa[:, :], pga[:, :], AF.Sigmoid, r=[pgak], w=[sak])
                ACT(sb_[:, :], pgb[:, :], AF.Sigmoid, r=[pgbk], w=[sbk])
                TT("dve", sa[:, :], pa[:, :], sa[:, :], ALU.mult, r=[pak, sak], w=[sak])
                TT("dve", sb_[:, :], pb[:, :], sb_[:, :], ALU.mult, r=[pbk, sbk], w=[sbk])
                TT("dve", mTt[:, oc, :], sa[:, :], sb_[:, :], ALU.add, r=[sak, sbk], w=["mTt"])
            for tt in range(P5_TT):
                ti = tb * 4 + tt
                h0, h0k = hr.next()
                DMA("sp", h0[:, :], H_d[ti * 128:(ti + 1) * 128, :], r=["H_d"], w=[h0k])
                r_, rk_ = rr.next()
                for half in range(2):
                    po_, pok_ = pm.next()
                    for kc in range(8):
                        MM(po_[:, :], mTt[:, kc, tt * 128:(tt + 1) * 128], Wo[:, kc, half * 512:(half + 1) * 512],
                           start=(kc == 0), stop=(kc == 7), r=["mTt", "Wo"], w=[pok_])
                    STT(r_[:, half * 512:(half + 1) * 512], h0[:, half * 512:(half + 1) * 512], DN_ALPHA, po_[:, :],
                        ALU.mult, ALU.add, r=[h0k, pok_], w=[rk_])
                h1, h1k = h1r.next()
                layernorm(es, "ln1", r_, rk_, g1t, "g1t", b1t, "b1t", h1, h1k, small)
                DMA(STORE_Q, H1_d[ti * 128:(ti + 1) * 128, :], h1[:, :], r=[h1k], w=["H1_d"])
                h1b, h1bk = h1br.next()
                CP("act", h1b[:, :], h1[:, :], r=[h1k], w=[h1bk])
                h1T, h1Tk = h1Tr.next()
                for half in range(2):
                    tp, tpk = pt2.next()
                    for q in range(4):
                        kc = half * 4 + q
                        TR(tp[:, q * 128:(q + 1) * 128], h1[:, kc * 128:(kc + 1) * 128], identf, r=[h1k, "cstf"], w=[tpk])
                    CP("act", h1T[:, half * 4:(half + 1) * 4, :], tp[:, :].rearrange("p (k t) -> p k t", k=4), r=[tpk], w=[h1Tk])
                pl, plk = pr.next()
                for kc in range(8):
                    MM(pl[:, 0:NE], h1T[:, kc, :], Wr[:, kc, :], start=(kc == 0), stop=(kc == 7), r=[h1Tk, "Wr"], w=[plk])
                R, Rk = rt.next()
                sm, smk = small.next()
                TT("dve", R[:, 0, :], pl[:, 0:NE], rbt[:, :], ALU.add, r=[plk, "rbt"], w=[Rk])
                S.add("dve", lambda e, sm=sm, R=R: e.max(sm[:, 0:8], R[:, 0, :]), [Rk], [smk])
                TS_("dve", R[:, 1, :], R[:, 0, :], sm[:, 3:4], None, ALU.is_ge, r=[Rk, smk], w=[Rk])
                TS_("dve", sm[:, 8:9], sm[:, 0:1], -1.0, None, ALU.mult, r=[smk], w=[smk])
                ACT(R[:, 2, :], R[:, 0, :], AF.Exp, r=[Rk, smk], w=[Rk], bias=sm[:, 8:9])
                STT(R[:, 2, :], R[:, 2, :], 1.0, R[:, 1, :], ALU.mult, ALU.mult, r=[Rk], w=[Rk, smk], accum_out=sm[:, 9:10])
                S.add("dve", lambda e, sm=sm: e.reciprocal(sm[:, 10:11], sm[:, 9:10]), [smk], [smk])
                TS_("dve", R[:, 3, :], R[:, 2, :], sm[:, 10:11], None, ALU.mult, r=[Rk, smk], w=[Rk])
                mb, mbk = mkb.next()
                CP("dve", mb[:, :], R[:, 1, :], r=[Rk], w=[mbk])
                pc, pck = pr.next()
                MM(pc[:, 0:NE], LTb, mb[:, :], r=CK + [mbk], w=[pck])
                MM(pc[:, 64:64 + NE], ONEb, mb[:, :], r=CK + [mbk], w=[pck])
                TT("dve", R[:, 4, :], pc[:, 0:NE], cum[:, :], ALU.add, r=[pck, "cum"], w=[Rk])
                TS_("dve", R[:, 4, :], R[:, 4, :], float(CAP - 1), None, ALU.min, r=[Rk], w=[Rk])
                TT("dve", R[:, 4, :], R[:, 4, :], ioe[:, :], ALU.add, r=[Rk, "ioe"], w=[Rk])
                TT("dve", cum[:, :], cum[:, :], pc[:, 64:64 + NE], ALU.add, r=[pck, "cum"], w=["cum"])
                for k in range(4):
                    TS_("dve", R[:, 5, :], R[:, 0, :], sm[:, k:k + 1], None, ALU.is_equal, r=[Rk, smk], w=[Rk])
                    STT(R[:, 6, :], R[:, 5, :], 1.0, R[:, 4, :], ALU.mult, ALU.mult, r=[Rk], w=[Rk, smk],
                        accum_out=sm[:, 11 + k:12 + k])
                    STT(R[:, 6, :], R[:, 5, :], 1.0, R[:, 3, :], ALU.mult, ALU.mult, r=[Rk], w=[Rk, "GATE"],
                        accum_out=GATE[:, ti, k:k + 1])
                CP("dve", IDX[:, ti, :], sm[:, 11:15], r=[smk], w=["IDX"])
                for k in range(4):
                    S.add("pool", lambda e, h1b=h1b, ti=ti, k=k: e.indirect_dma_start(
                        out=XG_d[:, :], out_offset=bass.IndirectOffsetOnAxis(ap=IDX[:, ti, k:k + 1], axis=0),
                        in_=h1b[:, :], in_offset=None), r=[h1bk, "IDX"], w=["XG_d"], dma=True)
        if stop == 5:
            S.waitall("sp", ["XG_d", "H1_d"])
        S.flush()
    es5.close()
    if stop == 5:
        return nc

    with ExitStack() as es:
        new_stage(es, 4, 256)
        wgp = Pool_(es, "wgp", 2, [128, 8, D], BF16)
        wup = Pool_(es, "wup", 2, [128, 8, D], BF16)
        wdp = Pool_(es, "wdp", 2, [128, 8, D], BF16)
        bcol = sb(es, "bcol", [128, 2, 256], F32)
        bld = Pool_(es, "bld", 2, [128, 128], F32)
        bdf = Pool_(es, "bdf", 2, [1, D], F32)
        bdb = Pool_(es, "bdb", 2, [1, D], BF16)
        xgp = Pool_(es, "xgp", 2, [128, D], BF16)
        xTp = Pool_(es, "xTp", 2, [128, 8, CAP], BF16)
        aTp = Pool_(es, "aTp", 2, [128, 8, CAP], BF16)
        gr = Pool_(es, "gr7", 2, [128, CAP], F32)
        ur = Pool_(es, "ur7", 2, [128, CAP], F32)
        sr7 = Pool_(es, "sr7", 2, [128, CAP], F32)
        yr = Pool_(es, "yr7", 2, [128, D], F32)
        pgu = Pool_(es, "pgu7", 4, [128, 512], F32, psum=True)
        py = Pool_(es, "py7", 2, [128, 512], F32, psum=True)
        ptp = Pool_(es, "ptp7", 2, [128, 512], F32, psum=True)
        for gi, bsrc in enumerate((eb_g, eb_u)):
            bv = bsrc.rearrange("e (fc p) -> (e fc) p", p=128)
            for half in range(2):
                bl_, blk_ = bld.next()
                DMA("sp", bl_[:, :], bv[half * 128:(half + 1) * 128, :], w=[blk_])
                tp, tpk = ptp.next()
                TR(tp[:, 0:128], bl_[:, :], identf, r=[blk_, "cstf"], w=[tpk])
                CP("act", bcol[:, gi, half * 128:(half + 1) * 128], tp[:, 0:128], r=[tpk], w=["bcol"])
        CE = ("act", "dve", "act", "dve", "pool", "act", "dve", "act", "dve", "pool", "act", "dve")

        def expert_w(e_):
            wg, wgk = wgp.next(); wu, wuk = wup.next(); wd, wdk = wdp.next()

            def gen():
                n = 0
                for (dst, dkey, src) in ((wg, wgk, ew_g[e_]), (wu, wuk, ew_u[e_]), (wd, wdk, ew_d[e_])):
                    for kc0 in range(0, 8, 2):
                        st, sk = wst["pool"].next()
                        stv = st[:, :, :].rearrange("p a b -> p (a b)").rearrange("p (k n) -> p k n", k=2)
                        DMA("sp", stv, src[kc0 * 128:(kc0 + 2) * 128, :].rearrange("(kc p) n -> p kc n", p=128), w=[sk])
                        CP(CE[n % len(CE)], dst[:, kc0:kc0 + 2, :], stv, r=[sk], w=[dkey])
                        n += 1
                        yield
            return (wg, wgk, wu, wuk, wd, wdk), gen()

        wcur, g0 = expert_w(0)
        for _ in g0:
            pass

        def x_transposes(e_):
            xT, xTk = xTp.next()
            for st_ in range(3):
                xg, xgk = xgp.next()
                r0 = e_ * CAP + st_ * 128
                DMA("sp", xg[:, :], XG_d[r0:r0 + 128, :], r=["XG_d"], w=[xgk])
                tp, tpk = ptp.next()
                tpb = tp[:, :].bitcast(BF16)
                for kc in range(8):
                    TR(tpb[:, kc * 128:(kc + 1) * 128], xg[:, kc * 128:(kc + 1) * 128], identb, r=[xgk] + CK, w=[tpk])
                CP("dve", xT[:, :, st_ * 128:(st_ + 1) * 128], tpb[:, 0:1024].rearrange("p (k t) -> p k t", k=8),
                   r=[tpk], w=[xTk])
            return xT, xTk

        def gu(e_, fc, wg, wgk, wu, wuk, xT, xTk, aT, aTk):
            fcs = slice(fc * 128, (fc + 1) * 128)
            pgt, pgtk = pgu.next(); put, putk = pgu.next()
            for kc in range(8):
                MM(pgt[:, 0:CAP], wg[:, kc, fcs], xT[:, kc, :], start=(kc == 0), stop=(kc == 7), r=[wgk, xTk], w=[pgtk])
            for kc in range(8):
                MM(put[:, 0:CAP], wu[:, kc, fcs], xT[:, kc, :], start=(kc == 0), stop=(kc == 7), r=[wuk, xTk], w=[putk])
            bi = e_ * 8 + fc
            g_, gk_ = gr.next(); u_, uk_ = ur.next(); s_, sk_ = sr7.next()
            TS_("dve", g_[:, :], pgt[:, 0:CAP], bcol[:, 0, bi:bi + 1], 7.0, ALU.add, ALU.min, r=[pgtk, "bcol"], w=[gk_])
            TS_("dve", u_[:, :], put[:, 0:CAP], bcol[:, 1, bi:bi + 1], 7.0, ALU.add, ALU.min, r=[putk, "bcol"], w=[uk_])
            TS_("dve", u_[:, :], u_[:, :], -7.0, 1.0, ALU.max, ALU.add, r=[uk_], w=[uk_])
            ACT(s_[:, :], g_[:, :], AF.Sigmoid, r=[gk_], w=[sk_], scale=1.702)
            TT("dve", g_[:, :], g_[:, :], s_[:, :], ALU.mult, r=[gk_, sk_], w=[gk_])
            TT("dve", aT[:, fc, :], g_[:, :], u_[:, :], ALU.mult, r=[gk_, uk_], w=[aTk])

        def down(e_, aT, aTk, wd, wdk, bb_, bbk, gnext):
            for st_ in range(3):
                y_, yk_ = yr.next()
                for half in range(2):
                    pyt, pytk = py.next()
                    for fc in range(8):
                        MM(pyt[:, :], aT[:, fc, st_ * 128:(st_ + 1) * 128], wd[:, fc, half * 512:(half + 1) * 512],
                           start=(fc == 0), stop=False, r=[aTk, wdk], w=[pytk])
                    MM(pyt[:, :], ONEb[0:1, :], bb_[0:1, half * 512:(half + 1) * 512], start=False, stop=True,
                       r=CK + [bbk], w=[pytk])
                    CP("act", y_[:, half * 512:(half + 1) * 512], pyt[:, :], r=[pytk], w=[yk_])
                r0 = e_ * CAP + st_ * 128
                DMA(STORE_Q, YG_d[r0:r0 + 128, :], y_[:, :], r=[yk_], w=["YG_d"])
                next(gnext, None)
                next(gnext, None)

        xcur = x_transposes(0)
        prev = None
        for e_ in range(P7_E):
            wg, wgk, wu, wuk, wd, wdk = wcur
            if e_ + 1 < P7_E:
                wnext, gnext = expert_w(e_ + 1)
            else:
                wnext, gnext = None, iter(())
            bf_, bfk = bdf.next(); bb_, bbk = bdb.next()
            DMA("sp", bf_[:, :], eb_d[e_:e_ + 1, :], w=[bfk])
            CP("dve", bb_[:, :], bf_[:, :], r=[bfk], w=[bbk])
            xT, xTk = xcur
            aT, aTk = aTp.next()
            for fc in range(2):
                gu(e_, fc, wg, wgk, wu, wuk, xT, xTk, aT, aTk)
                next(gnext, None)
            if prev is not None:
                down(*prev, gnext)
            if e_ + 1 < P7_E:
                xcur = x_transposes(e_ + 1)
            for fc in range(2, 8):
                gu(e_, fc, wg, wgk, wu, wuk, xT, xTk, aT, aTk)
                next(gnext, None)
            for _ in gnext:
                pass
            prev = (e_, aT, aTk, wd, wdk, bb_, bbk)
            wcur = wnext
        down(*prev, iter(()))
        if stop == 7:
            S.waitall("sp", ["YG_d"])
        S.flush()
    if stop == 7:
        return nc

    with ExitStack() as es:
        g2t = bcast_load(es, "g2t", ln2_g, D)
        b2t = bcast_load(es, "b2t", ln2_b, D)
        h1r = Pool_(es, "h1r8", 2, [128, D], F32)
        ykr = Pool_(es, "ykr8", 4, [128, D], F32)
        accr = Pool_(es, "accr8", 2, [128, D], F32)
        outr = Pool_(es, "outr8", 2, [128, D], F32)
        small = Pool_(es, "small8", 2, [128, 16], F32)
        outk = []
        for ti in range(P8_T):
            h1, h1k = h1r.next()
            DMA("sp", h1[:, :], H1_d[ti * 128:(ti + 1) * 128, :], r=["H1_d"], w=[h1k])
            acc, acck = accr.next()
            TS_("dve", acc[:, :], h1[:, :], DN_ALPHA, None, ALU.mult, r=[h1k], w=[acck])
            for k in range(4):
                yk, ykk = ykr.next()
                S.add("pool", lambda e, yk=yk, ti=ti, k=k: e.indirect_dma_start(
                    out=yk[:, :], out_offset=None, in_=YG_d[:, :],
                    in_offset=bass.IndirectOffsetOnAxis(ap=IDX[:, ti, k:k + 1], axis=0)),
                    r=["YG_d", "IDX"], w=[ykk], dma=True)
                STT(acc[:, :], yk[:, :], GATE[:, ti, k:k + 1], acc[:, :], ALU.mult, ALU.add, r=[ykk, "GATE", acck], w=[acck])
            o_, ok_ = outr.next()
            layernorm(es, "ln2", acc, acck, g2t, "g2t", b2t, "b2t", o_, ok_, small)
            kk = "out%d" % ti
            DMA("sp", out[ti * 128:(ti + 1) * 128, :], o_[:, :], r=[ok_], w=[kk])
            outk.append(kk)
        S.waitall("sp", outk)
        S.flush()
    es0.close()
    return nc


def _consts():
    c = np.zeros((128, 8, 128), np.float32)
    idx = np.arange(128)
    same = np.ones((128, 128), bool)
    c[:, 0, :] = np.eye(128, dtype=np.float32)
    c[:, 1, :] = (same & (idx[:, None] <= idx[None, :])).astype(np.float32)
    c[:, 2, :] = (same & (idx[:, None] >= idx[None, :])).astype(np.float32)
    c[:, 3, :] = (same & (idx[:, None] > idx[None, :])).astype(np.float32)
    c[:, 4, :] = (same & (idx[:, None] < idx[None, :])).astype(np.float32)
    c[:, 5, :] = (idx[:, None] < idx[None, :]).astype(np.float32)
    c[:, 6, :] = 1.0
    c[:, 7, 0] = (idx < 64).astype(np.float32)
    c[:, 7, 1] = (idx >= 64).astype(np.float32)
    c2 = np.tile((np.arange(32, dtype=np.float32) * CAP)[None, :], (128, 1)).astype(np.float32)
    return c, c2


_NC_CACHE = {}


def kernel(**inp):
    x = np.asarray(inp["x"], np.float32)
    if "nc" not in _NC_CACHE:
        _NC_CACHE["nc"] = build()
    nc = _NC_CACHE["nc"]
    cst, cst2 = _consts()
    pos = np.arange(8192, dtype=np.float32)
    inv_freq = np.power(np.float32(500000.0), -np.arange(0, 16, 2, dtype=np.float32) / np.float32(16)).astype(np.float32)
    ang = (pos[:, None] * inv_freq[None, :]).astype(np.float32)
    cs_full = np.concatenate([np.tile(np.cos(ang), (1, 8)), np.tile(np.sin(ang), (1, 8))], axis=1).astype(np.float32)

    def sq(name):
        a = np.asarray(inp[name], np.float32)
        return np.ascontiguousarray(a[0]) if a.shape[0] == 1 and name not in ("ln0_g", "ln0_b") else np.ascontiguousarray(a)

    shared = {
        "cst": cst, "cst2": cst2,
        "ln0_g": np.ascontiguousarray(inp["ln0_g"], np.float32), "ln0_b": np.ascontiguousarray(inp["ln0_b"], np.float32),
    }
    for nm in ("w_in", "gla_wf_fwd", "gla_bf_fwd", "gla_wf_bwd", "gla_bf_bwd", "gla_norm_g", "diff_lq1", "diff_lk1",
               "diff_lq2", "diff_lk2", "diff_norm_g", "w_br_gla", "w_br_diff", "w_out", "ln1_g", "ln1_b", "router_w",
               "router_b", "exp_w_gate", "exp_b_gate", "exp_w_up", "exp_b_up", "exp_w_down", "exp_b_down", "ln2_g", "ln2_b"):
        shared[nm] = np.ascontiguousarray(np.asarray(inp[nm], np.float32)[0])
    in_maps = []
    for c in range(8):
        b, qd = c // 4, c % 4
        own = np.arange(qd * 2048, (qd + 1) * 2048)
        oth = np.concatenate([np.arange(0, qd * 2048), np.arange((qd + 1) * 2048, 8192)])
        order = np.concatenate([oth, own])
        mk = np.zeros((8192, 2), np.float32)
        mk[:6144, 0] = (oth < qd * 2048).astype(np.float32)
        mk[:6144, 1] = (oth >= (qd + 1) * 2048).astype(np.float32)
        m = dict(shared)
        m["xa"] = np.ascontiguousarray(x[b][order])
        m["csa"] = np.ascontiguousarray(np.concatenate([cs_full[order], mk], axis=1))
        in_maps.append(m)
    res = run_bass_kernel_spmd(nc, in_maps, core_ids=list(range(8)))
    _NC_CACHE["last"] = res
    outp = np.zeros((2, 8192, 1024), np.float32)
    for c in range(8):
        b, qd = c // 4, c % 4
        outp[b, qd * 2048:(qd + 1) * 2048] = res.results[c]["out"]
    return outp
```
